# Optimizing a Trainium2 kernel written in Bass

```python
import math
import jax, jax.numpy as jnp
from jax import lax
import numpy as np

D_MODEL = 2048
BATCH = 8
SEQ = 2048
DEPTH = 2

N_MIXERS = 2
N_MLA_LAYERS = (DEPTH + 1) // 2
N_GDN_LAYERS = DEPTH // 2
RMS_EPS = 1e-6

MLA_HEADS = D_MODEL // 128
Q_LORA = D_MODEL // 4
KV_LORA = D_MODEL // 4
QK_NOPE = 128
QK_ROPE = 64
V_HEAD = 128
ROPE_THETA = 10000.0
Q_BLOCK = 128
MLA_IN = Q_LORA + KV_LORA + QK_ROPE

GDN_HEADS = D_MODEL // 128
GDN_DK = 128
GDN_DV = 128
CONV_WIDTH = 4
CHUNK = 64
QKV_DIM = GDN_HEADS * (2 * GDN_DK + GDN_DV)
GDN_VAL = GDN_HEADS * GDN_DV
GDN_IN = QKV_DIM + GDN_VAL + 2 * GDN_HEADS

D_FF = 7 * D_MODEL // 2
N_EXPERTS = 8
TOP_K = 2
D_FF_EXPERT = 7 * D_MODEL // 2
MOE_BLOCK = 256

kernel_name = 'mla_gdn_interleaved_moe_trunk'


def rms_norm(x, w):
    xf = x.astype(jnp.float32)
    y = xf * lax.rsqrt(jnp.mean(xf * xf, axis=-1, keepdims=True) + RMS_EPS)
    return (y * w.astype(jnp.float32)).astype(x.dtype)


def l2norm(x):
    xf = x.astype(jnp.float32)
    return xf * lax.rsqrt(jnp.sum(xf * xf, axis=-1, keepdims=True) + RMS_EPS)


def swiglu(x, w_gate, w_up, w_down):
    return (jax.nn.silu(x @ w_gate) * (x @ w_up)) @ w_down


def rope_angles(positions, dim):
    inv_freq = ROPE_THETA ** (-jnp.arange(0, dim, 2, dtype=jnp.float32) / dim)
    ang = positions.astype(jnp.float32)[..., None] * inv_freq
    return jnp.cos(ang), jnp.sin(ang)


def apply_rope(x, cos, sin):
    x1, x2 = jnp.split(x, 2, axis=-1)
    return jnp.concatenate([x1 * cos - x2 * sin, x2 * cos + x1 * sin], axis=-1).astype(x.dtype)


def mla_attention(h, positions, w_in, q_norm, w_qb, kv_norm, w_kvb, w_o):
    B, S, _ = h.shape
    proj = h @ w_in
    c_q, c_kv, k_rope = jnp.split(proj, [Q_LORA, Q_LORA + KV_LORA], axis=-1)
    q = (rms_norm(c_q, q_norm) @ w_qb).reshape(B, S, MLA_HEADS, QK_NOPE + QK_ROPE)
    kv = (rms_norm(c_kv, kv_norm) @ w_kvb).reshape(B, S, MLA_HEADS, QK_NOPE + V_HEAD)
    q_nope, q_pe = jnp.split(q, [QK_NOPE], axis=-1)
    k_nope, v = jnp.split(kv, [QK_NOPE], axis=-1)
    cos, sin = rope_angles(positions, QK_ROPE)
    q_pe = apply_rope(q_pe, cos[:, :, None, :], sin[:, :, None, :])
    k_pe = apply_rope(k_rope, cos, sin)
    q_nope, q_pe, k_nope, v = (t.transpose(0, 2, 1, 3) for t in (q_nope, q_pe, k_nope, v))
    scale = (QK_NOPE + QK_ROPE) ** -0.5
    outs = []
    for blk in range(S // Q_BLOCK):
        s0 = blk * Q_BLOCK
        s1 = s0 + Q_BLOCK
        sc = (jnp.einsum('bhqd,bhkd->bhqk', q_nope[:, :, s0:s1], k_nope[:, :, :s1],
                         preferred_element_type=jnp.float32)
              + jnp.einsum('bhqd,bkd->bhqk', q_pe[:, :, s0:s1], k_pe[:, :s1],
                           preferred_element_type=jnp.float32)) * scale
        causal = jnp.arange(s1)[None, :] <= jnp.arange(s0, s1)[:, None]
        p = jax.nn.softmax(jnp.where(causal, sc, -jnp.inf), axis=-1).astype(v.dtype)
        outs.append(jnp.einsum('bhqk,bhkd->bhqd', p, v[:, :, :s1]))
    o = jnp.concatenate(outs, axis=2).transpose(0, 2, 1, 3).reshape(B, S, MLA_HEADS * V_HEAD)
    return o @ w_o


def causal_depthwise_conv(x, w):
    K, C = w.shape
    return lax.conv_general_dilated(x, w[:, None, :].astype(x.dtype), window_strides=(1,),
                                    padding=[(K - 1, 0)], dimension_numbers=('NWC', 'WIO', 'NWC'),
                                    feature_group_count=C)


def chunked_gated_delta_rule(q, k, v, g, beta):
    B, S, H, DK = q.shape
    DV = v.shape[-1]
    N = S // CHUNK

    def chunks(t):
        return jnp.moveaxis(t.reshape(B, N, CHUNK, H, *t.shape[3:]), 3, 2)

    q, k, v, g, beta = (chunks(t) for t in (q, k, v, g, beta))
    g = jnp.cumsum(g, axis=-1)
    g_last = g[..., -1]
    incl = jnp.tril(jnp.ones((CHUNK, CHUNK), dtype=bool))
    strict = jnp.tril(jnp.ones((CHUNK, CHUNK), dtype=bool), -1)
    diff = g[..., :, None] - g[..., None, :]
    decay = jnp.where(incl, jnp.exp(jnp.where(incl, diff, 0.0)), 0.0)
    k_beta = k * beta[..., None]
    v_beta = v * beta[..., None]
    lower = jnp.where(strict, jnp.einsum('bnhid,bnhjd->bnhij', k_beta, k) * decay, 0.0)
    eye = jnp.eye(CHUNK, dtype=q.dtype)
    t_inv = lax.linalg.triangular_solve(eye + lower, jnp.broadcast_to(eye, lower.shape),
                                        left_side=True, lower=True, unit_diagonal=True)
    u = t_inv @ v_beta
    w = t_inv @ (k_beta * jnp.exp(g)[..., None])
    a_intra = jnp.where(incl, jnp.einsum('bnhid,bnhjd->bnhij', q, k) * decay, 0.0)
    q_dec = q * jnp.exp(g)[..., None]
    k_dec = k * jnp.exp(g_last[..., None] - g)[..., None]

    def step(state, xs):
        w_c, u_c, q_c, k_c, a_c, gl_c = xs
        v_new = u_c - w_c @ state
        o_c = q_c @ state + a_c @ v_new
        state = state * jnp.exp(gl_c)[..., None, None] + jnp.swapaxes(k_c, -1, -2) @ v_new
        return state, o_c

    xs = tuple(jnp.moveaxis(t, 1, 0) for t in (w, u, q_dec, k_dec, a_intra, g_last))
    state0 = jnp.zeros((B, H, DK, DV), q.dtype)
    _, o = lax.scan(step, state0, xs)
    return jnp.moveaxis(o, 0, 1).transpose(0, 1, 3, 2, 4).reshape(B, S, H, DV)


def gated_deltanet(h, w_in, conv_w, a_log, dt_bias, norm_w, w_o):
    B, S, _ = h.shape
    proj = h @ w_in
    qkv, z, b_logit, a_in = jnp.split(proj, [QKV_DIM, QKV_DIM + GDN_VAL, QKV_DIM + GDN_VAL + GDN_HEADS], axis=-1)
    qkv = jax.nn.silu(causal_depthwise_conv(qkv, conv_w))
    q, k, v = jnp.split(qkv, [GDN_HEADS * GDN_DK, 2 * GDN_HEADS * GDN_DK], axis=-1)
    q = l2norm(q.reshape(B, S, GDN_HEADS, GDN_DK)) * (GDN_DK ** -0.5)
    k = l2norm(k.reshape(B, S, GDN_HEADS, GDN_DK))
    v = v.reshape(B, S, GDN_HEADS, GDN_DV).astype(jnp.float32)
    beta = jax.nn.sigmoid(b_logit.astype(jnp.float32))
    g = -jnp.exp(a_log.astype(jnp.float32)) * jax.nn.softplus(a_in.astype(jnp.float32) + dt_bias.astype(jnp.float32))
    o = chunked_gated_delta_rule(q, k, v, g, beta)
    o = rms_norm(o, norm_w) * jax.nn.silu(z.reshape(B, S, GDN_HEADS, GDN_DV).astype(jnp.float32))
    return o.reshape(B, S, GDN_VAL).astype(h.dtype) @ w_o


def moe_swiglu(h, router_w, router_b, w_gate, w_up, w_down):
    B, S, D = h.shape
    T = B * S
    A = T * TOP_K
    NB = -(-A // MOE_BLOCK) + N_EXPERTS
    xt = h.reshape(T, D)
    logits = jnp.einsum('td,de->te', xt, router_w, preferred_element_type=jnp.float32) + router_b.astype(jnp.float32)
    top_logit, top_idx = lax.top_k(logits, TOP_K)
    gates = jax.nn.softmax(top_logit, axis=-1).astype(h.dtype)
    flat_e = top_idx.reshape(A)
    flat_tok = jnp.arange(A, dtype=jnp.int32) // TOP_K
    order = jnp.argsort(flat_e)
    e_sorted = flat_e[order]
    tok_sorted = flat_tok[order]
    g_sorted = gates.reshape(A)[order]
    counts = jnp.bincount(flat_e, length=N_EXPERTS)
    padded = (counts + MOE_BLOCK - 1) // MOE_BLOCK * MOE_BLOCK
    pad_end = jnp.cumsum(padded)
    pad_start = pad_end - padded
    grp_start = jnp.cumsum(counts) - counts
    dest = pad_start[e_sorted] + (jnp.arange(A, dtype=jnp.int32) - grp_start[e_sorted])
    slot_tok = jnp.zeros((NB * MOE_BLOCK,), jnp.int32).at[dest].set(tok_sorted)
    slot_gate = jnp.zeros((NB * MOE_BLOCK,), gates.dtype).at[dest].set(g_sorted)
    block_expert = jnp.minimum(jnp.searchsorted(pad_end, jnp.arange(NB) * MOE_BLOCK, side='right'), N_EXPERTS - 1)
    xb = xt[slot_tok].reshape(NB, MOE_BLOCK, D)

    def expert_block(args):
        xe, e = args
        return swiglu(xe, w_gate[e], w_up[e], w_down[e])

    yb = lax.map(expert_block, (xb, block_expert)).reshape(NB * MOE_BLOCK, D)
    y = jax.ops.segment_sum(yb * slot_gate[:, None], slot_tok, num_segments=T)
    return y.reshape(B, S, D)


def _normal(key, shape, fan_in):
    return jax.random.normal(key, shape, jnp.float32) * (fan_in ** -0.5)


def _gain(key, shape):
    return 1.0 + 0.02 * jax.random.normal(key, shape, jnp.float32)


def setup_inputs(seed: int = 0) -> dict:
    key = jax.random.key(seed)
    ks = jax.random.split(key, 32)
    D = D_MODEL
    Lm, Lg = N_MLA_LAYERS, N_GDN_LAYERS
    dt = jax.random.uniform(ks[17], (Lg, GDN_HEADS), jnp.float32, minval=0.001, maxval=0.1)
    return {
        'x': jax.random.normal(ks[0], (BATCH, SEQ, D), jnp.float32),
        'positions': jnp.arange(SEQ, dtype=jnp.int32)[None, :] + jax.random.randint(ks[1], (BATCH, 1), 0, 4096, dtype=jnp.int32),
        'ln_mix_mla': _gain(ks[2], (Lm, D)),
        'mla_w_in': _normal(ks[3], (Lm, D, MLA_IN), D),
        'mla_q_norm': _gain(ks[4], (Lm, Q_LORA)),
        'mla_w_qb': _normal(ks[5], (Lm, Q_LORA, MLA_HEADS * (QK_NOPE + QK_ROPE)), Q_LORA),
        'mla_kv_norm': _gain(ks[6], (Lm, KV_LORA)),
        'mla_w_kvb': _normal(ks[7], (Lm, KV_LORA, MLA_HEADS * (QK_NOPE + V_HEAD)), KV_LORA),
        'mla_w_o': _normal(ks[8], (Lm, MLA_HEADS * V_HEAD, D), MLA_HEADS * V_HEAD),
        'ln_ffn_dense': _gain(ks[9], (Lm, D)),
        'ffn_w_gate': _normal(ks[10], (Lm, D, D_FF), D),
        'ffn_w_up': _normal(ks[11], (Lm, D, D_FF), D),
        'ffn_w_down': _normal(ks[12], (Lm, D_FF, D), D_FF),
        'ln_mix_gdn': _gain(ks[13], (Lg, D)),
        'gdn_w_in': _normal(ks[14], (Lg, D, GDN_IN), D),
        'gdn_conv_w': _normal(ks[15], (Lg, CONV_WIDTH, QKV_DIM), CONV_WIDTH),
        'gdn_a_log': jnp.log(jax.random.uniform(ks[16], (Lg, GDN_HEADS), jnp.float32, minval=1.0, maxval=16.0)),
        'gdn_dt_bias': jnp.log(jnp.expm1(dt)),
        'gdn_norm': _gain(ks[18], (Lg, GDN_DV)),
        'gdn_w_o': _normal(ks[19], (Lg, GDN_VAL, D), GDN_VAL),
        'ln_ffn_moe': _gain(ks[20], (Lg, D)),
        'moe_router': _normal(ks[21], (Lg, D, N_EXPERTS), D),
        'moe_router_bias': 0.01 * jax.random.normal(ks[22], (Lg, N_EXPERTS), jnp.float32),
        'moe_w_gate': _normal(ks[23], (Lg, N_EXPERTS, D, D_FF_EXPERT), D),
        'moe_w_up': _normal(ks[24], (Lg, N_EXPERTS, D, D_FF_EXPERT), D),
        'moe_w_down': _normal(ks[25], (Lg, N_EXPERTS, D_FF_EXPERT, D), D_FF_EXPERT),
        'final_norm': _gain(ks[26], (D,)),
    }


def reference(x, positions, ln_mix_mla, mla_w_in, mla_q_norm, mla_w_qb, mla_kv_norm, mla_w_kvb, mla_w_o,
              ln_ffn_dense, ffn_w_gate, ffn_w_up, ffn_w_down,
              ln_mix_gdn, gdn_w_in, gdn_conv_w, gdn_a_log, gdn_dt_bias, gdn_norm, gdn_w_o,
              ln_ffn_moe, moe_router, moe_router_bias, moe_w_gate, moe_w_up, moe_w_down, final_norm):
    h = x
    for i in range(DEPTH):
        j = i // N_MIXERS
        if i % N_MIXERS == 0:
            h = h + mla_attention(rms_norm(h, ln_mix_mla[j]), positions, mla_w_in[j], mla_q_norm[j],
                                  mla_w_qb[j], mla_kv_norm[j], mla_w_kvb[j], mla_w_o[j])
            h = h + swiglu(rms_norm(h, ln_ffn_dense[j]), ffn_w_gate[j], ffn_w_up[j], ffn_w_down[j])
        else:
            h = h + gated_deltanet(rms_norm(h, ln_mix_gdn[j]), gdn_w_in[j], gdn_conv_w[j], gdn_a_log[j],
                                   gdn_dt_bias[j], gdn_norm[j], gdn_w_o[j])
            h = h + moe_swiglu(rms_norm(h, ln_ffn_moe[j]), moe_router[j], moe_router_bias[j],
                               moe_w_gate[j], moe_w_up[j], moe_w_down[j])
    return rms_norm(h, final_norm)
```

```python
import numpy as np
from contextlib import ExitStack
import concourse.bass as bass
import concourse.mybir as mybir
from concourse.bass_utils import run_bass_kernel_spmd

F32 = mybir.dt.float32
BF16 = mybir.dt.bfloat16
F32R = mybir.dt.float32r
AF = mybir.ActivationFunctionType
ALU = mybir.AluOpType
AX = mybir.AxisListType

D = 2048
T = 2048
KT = 16
DFF = 7168
EPS = 1e-6


class Buf:
    __slots__ = ("w", "r")

    def __init__(self):
        self.w = None
        self.r = {}


class Sched:
    NDS = 8

    def __init__(self, nc, stack):
        self.nc = nc
        self.engs = {"pe": nc.tensor, "act": nc.scalar, "dve": nc.vector,
                     "pool": nc.gpsimd, "sp": nc.sync}
        self.sem = {k: stack.enter_context(nc.semaphore("s_" + k)) for k in self.engs}
        self.cnt = {k: 0 for k in self.engs}
        self.known = {k: {} for k in self.engs}
        self.dsem, self.dval, self.dnext = {}, {}, {}
        for q in ("sp", "pool"):
            self.dsem[q] = [stack.enter_context(nc.semaphore("d_%s%d" % (q, i))) for i in range(self.NDS)]
            self.dval[q] = [0] * self.NDS
            self.dnext[q] = 0
        self.n_ins = 0
        self.n_wait = 0

    def _semh(self, key):
        if key[0] == "E":
            return self.sem[key[1]]
        return self.dsem[key[1]][key[2]]

    def _wait(self, eng, toks, defer=False):
        need = {}
        for (key, val) in toks:
            if val > need.get(key, 0):
                need[key] = val
        kn = self.known[eng]
        lst = [(key, val) for key, val in need.items() if kn.get(key, 0) < val]
        pend = None
        if defer and lst and eng != "pe":
            pend = lst.pop()
        for key, val in lst:
            self.engs[eng].wait_ge(self._semh(key), val)
            self.n_wait += 1
            kn[key] = val
        if pend is not None:
            kn[pend[0]] = pend[1]
        return pend

    def _deps(self, eng, reads, writes):
        toks = []
        own = ("E", eng)
        for b in reads:
            if b.w is not None:
                if b.w[0] == own and eng == "pe":
                    continue
                toks.append(b.w)
        for b in writes:
            if b.w is not None and b.w[0] != own:
                toks.append(b.w)
            for key, val in b.r.items():
                if key != own:
                    toks.append((key, val))
        return toks

    def _mark(self, tok, reads, writes):
        key, val = tok
        for b in reads:
            b.r[key] = val
        for b in writes:
            b.w = tok
            b.r = {}

    def op(self, eng, fn, reads=(), writes=(), inc=True):
        pend = self._wait(eng, self._deps(eng, reads, writes), defer=True)
        ins = fn(self.engs[eng])
        if pend is not None:
            ins._wait_ge(self._semh(pend[0]), pend[1])
        self.n_ins += 1
        if not inc:
            self._mark((("E", eng), self.cnt[eng] + 1), reads, writes)
            return ins
        self.cnt[eng] += 1
        ins.then_inc(self.sem[eng], 1)
        self._mark((("E", eng), self.cnt[eng]), reads, writes)
        return ins

    def dma(self, q, out, in_, reads=(), writes=(), **kw):
        self._wait(q, self._deps(q, reads, writes))
        i = self.dnext[q]
        self.dnext[q] = (i + 1) % self.NDS
        key = ("D", q, i)
        if self.dval[q][i] > 0:
            self._wait(q, [(key, self.dval[q][i])])
        self.dval[q][i] += 16
        self.engs[q].dma_start(out=out, in_=in_, **kw).then_inc(self.dsem[q][i], 16)
        self.n_ins += 1
        tok = (key, self.dval[q][i])
        self._mark(tok, reads, writes)
        return tok

    def barrier(self):
        toks = [(("E", k), v) for k, v in self.cnt.items() if v > 0]
        for q in self.dsem:
            for i in range(self.NDS):
                if self.dval[q][i] > 0:
                    toks.append((("D", q, i), self.dval[q][i]))
        for e in self.engs:
            self._wait(e, [t for t in toks if t[0] != ("E", e)])


class KB:
    def __init__(self):
        self.nc = bass.Bass("TRN2", target_bir_lowering=False)
        self.st = ExitStack()
        self.S = Sched(self.nc, self.st)
        self.uid = 0
        self.dram_bufs = {}

    def name(self, p="t"):
        self.uid += 1
        return "%s%d" % (p, self.uid)

    def sb(self, stack, shape, dt):
        return stack.enter_context(self.nc.sbuf_tensor(self.name("sb"), list(shape), dt))

    def ps(self, stack, shape=(128, 512), dt=F32):
        return stack.enter_context(self.nc.psum_tensor(self.name("ps"), list(shape), dt))

    def dram(self, name, shape, dt, kind="Internal"):
        t = self.nc.dram_tensor(name, list(shape), dt, kind=kind).ap()
        self.dram_bufs[name] = Buf()
        return t

    def dbuf(self, name):
        return self.dram_bufs[name]


def load_consts(K, stack, cst_dram):
    S = K.S
    c = {}
    cf = K.sb(stack, [128, 1024], F32)
    B = Buf()
    S.dma("sp", cf[:], cst_dram, reads=[K.dbuf("cst")], writes=[B])
    cb = K.sb(stack, [128, 1024], BF16)
    Bb = Buf()
    S.op("dve", lambda e: e.tensor_copy(cb[:], cf[:]), reads=[B], writes=[Bb])
    c["f"], c["b"], c["Bf"], c["Bb"] = cf, cb, B, Bb
    c["ident_f"] = cf[:, 0:128]
    c["ones_f"] = cf[:, 128:256]
    c["ident_b"] = cb[:, 0:128]
    c["ones_b"] = cb[:, 128:256]
    c["U_b"] = cb[:, 512:640]
    return c


def norm_T(K, C, src, Bsrc, dst, Bdst, lnw, Blnw, ntok, tmp, ps_bank, Bps, dst32=None, nk=KT, dim=D, dcol=0):
    S = K.S
    for c0 in range(0, ntok, 512):
        n = min(512, ntok - c0)
        for k in range(nk):
            sq, Bsq = tmp["sq"][k % 2]
            S.op("act", lambda e: e.activation(sq[:, :n], src[:, k, c0:c0 + n], AF.Square), reads=[Bsrc[k]], writes=[Bsq])
            S.op("pe", lambda e: e.matmul(ps_bank[:, :n], C["ones_f"], sq[:, :n], start=(k == 0), stop=(k == nk - 1)),
                 reads=[Bsq, C["Bf"]], writes=[Bps], inc=True)
        rs, Brs = tmp["rs"]
        S.op("act", lambda e: e.activation(rs[:, :n], ps_bank[:, :n], AF.Sqrt, bias=tmp["eps"][:, 0:1], scale=1.0 / dim), reads=[Bps, tmp["Beps"]], writes=[Brs])
        S.op("dve", lambda e: e.reciprocal(rs[:, :n], rs[:, :n]), reads=[Brs], writes=[Brs])
        for k in range(nk):
            if dst32 is None:
                S.op("dve", lambda e: e.scalar_tensor_tensor(dst[:, k, dcol + c0:dcol + c0 + n], src[:, k, c0:c0 + n], lnw[:, k:k + 1], rs[:, :n], ALU.mult, ALU.mult),
                     reads=[Bsrc[k], Blnw, Brs], writes=[Bdst])
            else:
                d32, Bd32 = dst32
                S.op("dve", lambda e: e.scalar_tensor_tensor(d32[:, k, c0:c0 + n], src[:, k, c0:c0 + n], lnw[:, k:k + 1], rs[:, :n], ALU.mult, ALU.mult),
                     reads=[Bsrc[k], Blnw, Brs], writes=[Bd32])
                S.op("pool", lambda e: e.tensor_copy(dst[:, k, dcol + c0:dcol + c0 + n], d32[:, k, c0:c0 + n]), reads=[Bd32], writes=[Bdst])


def norm_tmp(K, stack):
    S = K.S
    tmp = {"sq": [], "rs": None}
    for i in range(2):
        tmp["sq"].append((K.sb(stack, [128, 512], F32), Buf()))
    tmp["rs"] = (K.sb(stack, [128, 512], F32), Buf())
    eps = K.sb(stack, [128, 1], F32)
    tmp["eps"] = eps
    tmp["Beps"] = Buf()
    S.op("dve", lambda e: e.memset(eps[:], EPS), writes=[tmp["Beps"]])
    return tmp


def phase_ffn(K, C, hin, Bhin, hout, Bhout, lnw_d, wg, wu, wd, Bw):
    S = K.S
    TH = 1024
    FC = 256
    NFC = DFF // FC
    NFT = FC // 128
    hin_v = hin.rearrange("(k p) t -> p k t", p=128)
    hout_v = hout.rearrange("(k p) t -> p k t", p=128)
    wg_v = wg.rearrange("(k p) f -> p k f", p=128)
    wu_v = wu.rearrange("(k p) f -> p k f", p=128)
    wd_v = wd.rearrange("(j p) d -> p j d", p=128)
    with ExitStack() as st:
        xn = K.sb(st, [128, KT, TH], BF16); Bxn = Buf()
        yacc = K.sb(st, [128, KT, TH], F32); By = [Buf() for _ in range(KT)]
        lnw = K.sb(st, [128, KT], F32); Blnw = Buf()
        S.dma("sp", lnw[:], lnw_d, reads=[Bw], writes=[Blnw])
        tmp = norm_tmp(K, st)
        wgs = [(K.sb(st, [128, KT, FC], BF16), Buf()) for _ in range(2)]
        wus = [(K.sb(st, [128, KT, FC], BF16), Buf()) for _ in range(2)]
        wds = [(K.sb(st, [128, NFT, D], BF16), Buf()) for _ in range(2)]
        hts = [(K.sb(st, [128, NFT, TH], BF16), Buf()) for _ in range(2)]
        sgs = [(K.sb(st, [128, 512], F32), Buf()) for _ in range(2)]
        pgs = [(K.ps(st), Buf()) for _ in range(2)]
        pus = [(K.ps(st), Buf()) for _ in range(2)]
        pos = [(K.ps(st), Buf()) for _ in range(2)]
        pn = (K.ps(st), Buf())

        def load_gu(fc):
            s = fc % 2
            S.dma("pool", wgs[s][0][:], wg_v[:, :, fc * FC:(fc + 1) * FC], reads=[Bw], writes=[wgs[s][1]])
            S.dma("pool", wus[s][0][:], wu_v[:, :, fc * FC:(fc + 1) * FC], reads=[Bw], writes=[wus[s][1]])

        def load_d(fc):
            s = fc % 2
            S.dma("pool", wds[s][0][:], wd_v[:, fc * NFT:(fc + 1) * NFT, :], reads=[Bw], writes=[wds[s][1]])

        cnt = [0]

        def gateup(fc):
            s = fc % 2
            wgt, Bwg = wgs[s]
            wut, Bwu = wus[s]
            ht, Bht = hts[s]
            for ft in range(NFT):
                for tc in range(TH // 512):
                    i = cnt[0] % 2
                    cnt[0] += 1
                    pg, Bpg = pgs[i]
                    pu, Bpu = pus[i]
                    sg, Bsg = sgs[i]
                    tsl = slice(tc * 512, (tc + 1) * 512)
                    for k in range(KT):
                        S.op("pe", lambda e: e.matmul(pg[:], wgt[:, k, ft * 128:(ft + 1) * 128], xn[:, k, tsl], start=(k == 0), stop=(k == KT - 1)),
                             reads=[Bwg, Bxn], writes=[Bpg], inc=(k == KT - 1))
                    for k in range(KT):
                        S.op("pe", lambda e: e.matmul(pu[:], wut[:, k, ft * 128:(ft + 1) * 128], xn[:, k, tsl], start=(k == 0), stop=(k == KT - 1)),
                             reads=[Bwu, Bxn], writes=[Bpu], inc=(k == KT - 1))
                    S.op("act", lambda e: e.activation(sg[:], pg[:], AF.Silu), reads=[Bpg], writes=[Bsg])
                    S.op("dve", lambda e: e.tensor_tensor(ht[:, ft, tsl], sg[:], pu[:], ALU.mult), reads=[Bsg, Bpu], writes=[Bht])

        ocnt = [0]

        def down(fc):
            s = fc % 2
            wdt, Bwd = wds[s]
            ht, Bht = hts[s]
            for dt in range(KT):
                for tc in range(TH // 512):
                    i = ocnt[0] % 2
                    ocnt[0] += 1
                    po, Bpo = pos[i]
                    tsl = slice(tc * 512, (tc + 1) * 512)
                    for ft in range(NFT):
                        S.op("pe", lambda e: e.matmul(po[:], wdt[:, ft, dt * 128:(dt + 1) * 128], ht[:, ft, tsl], start=(ft == 0), stop=(ft == NFT - 1)),
                             reads=[Bwd, Bht], writes=[Bpo], inc=(ft == NFT - 1))
                    S.op("dve", lambda e: e.tensor_tensor(yacc[:, dt, tsl], yacc[:, dt, tsl], po[:], ALU.add), reads=[Bpo, By[dt]], writes=[By[dt]])

        for half in range(T // TH):
            hs = slice(half * TH, (half + 1) * TH)
            S.dma("sp", yacc[:], hin_v[:, :, hs], reads=[Bhin], writes=By)
            load_gu(0)
            load_d(0)
            norm_T(K, C, yacc, By, xn, Bxn, lnw, Blnw, TH, tmp, pn[0], pn[1])
            for fc in range(NFC + 1):
                if fc + 1 < NFC:
                    load_gu(fc + 1)
                if fc < NFC:
                    gateup(fc)
                if fc >= 1:
                    down(fc - 1)
                if fc + 1 < NFC:
                    load_d(fc + 1)
            S.dma("sp", hout_v[:, :, hs], yacc[:], reads=By, writes=[Bhout])
    S.barrier()


I32 = mybir.dt.int32
NH = 16
TWO_PI = float(2 * np.pi)
CW1 = 6.28125
CW2 = float(2 * np.pi - 6.28125)


def rope_tables(K, C, st0, posb, Bpos):
    S = K.S
    cos2 = K.sb(st0, [64, T], F32)
    sin2 = K.sb(st0, [64, T], F32)
    Brope = Buf()
    invf = C["f"][0:64, 384:385]
    sign = C["f"][0:64, 385:386]
    with ExitStack() as st:
        pi_ = K.sb(st, [64, T], I32); Bpi = Buf()
        ang = K.sb(st, [64, T], F32); Bang = Buf()
        a = K.sb(st, [64, T], F32); Ba = Buf()
        ki = K.sb(st, [64, T], I32); Bki = Buf()
        kf = K.sb(st, [64, T], F32); Bkf = Buf()
        m = K.sb(st, [64, T], F32); Bm = Buf()
        negpi = None
        S.dma("sp", pi_[:], posb, reads=[Bpos], writes=[Bpi])
        S.op("dve", lambda e: e.tensor_copy(a[:], pi_[:]), reads=[Bpi], writes=[Ba])
        S.op("dve", lambda e: e.tensor_scalar(ang[:], a[:], invf, None, ALU.mult), reads=[Ba, C["Bf"]], writes=[Bang])
        for dst, shift in ((sin2, 0.0), (cos2, float(np.pi / 2))):
            S.op("dve", lambda e: e.tensor_scalar(a[:], ang[:], shift, None, ALU.add), reads=[Bang], writes=[Ba])
            S.op("dve", lambda e: e.tensor_scalar(ki[:], a[:], 1.0 / TWO_PI, None, ALU.mult), reads=[Ba], writes=[Bki])
            S.op("dve", lambda e: e.tensor_copy(kf[:], ki[:]), reads=[Bki], writes=[Bkf])
            S.op("dve", lambda e: e.scalar_tensor_tensor(a[:], kf[:], -CW1, a[:], ALU.mult, ALU.add), reads=[Bkf, Ba], writes=[Ba])
            S.op("dve", lambda e: e.scalar_tensor_tensor(a[:], kf[:], -CW2, a[:], ALU.mult, ALU.add), reads=[Bkf, Ba], writes=[Ba])
            S.op("dve", lambda e: e.tensor_scalar(m[:], a[:], float(np.pi), -TWO_PI, ALU.is_gt, ALU.mult), reads=[Ba], writes=[Bm])
            S.op("dve", lambda e: e.tensor_tensor(a[:], a[:], m[:], ALU.add), reads=[Ba, Bm], writes=[Ba])
            S.op("dve", lambda e: e.tensor_scalar(m[:], a[:], -float(np.pi), TWO_PI, ALU.is_lt, ALU.mult), reads=[Ba], writes=[Bm])
            S.op("dve", lambda e: e.tensor_tensor(a[:], a[:], m[:], ALU.add), reads=[Ba, Bm], writes=[Ba])
            S.op("act", lambda e: e.activation(dst[:], a[:], AF.Sin), reads=[Ba], writes=[Brope])
        S.op("dve", lambda e: e.tensor_scalar(sin2[:], sin2[:], sign, None, ALU.mult), reads=[Brope, C["Bf"]], writes=[Brope])
        S.barrier()
    return cos2, sin2, Brope


def phase_mla(K, C, hin, Bhin, hout, Bhout, W, Bw):
    S = K.S
    scale = float(192 ** -0.5)
    hin_v = hin.rearrange("(k p) t -> p k t", p=128)
    hout_v = hout.rearrange("(k p) t -> p k t", p=128)
    oT = W["oT"]
    BoT = K.dbuf("oT")
    oT_v = oT.rearrange("(h p) t -> p h t", p=128)
    win_v = W["win"].rearrange("(k p) f -> p k f", p=128)
    wqb_v = W["wqb"].rearrange("(k p) f -> p k f", p=128)
    wkvb_v = W["wkvb"].rearrange("(k p) f -> p k f", p=128)
    wo_v = W["wo"].rearrange("(k p) f -> p k f", p=128)
    maskb = C["f"][:, 256:384]
    with ExitStack() as st0:
        cqn = K.sb(st0, [128, 4, T], BF16); Bcqn = Buf()
        ckvn = K.sb(st0, [128, 4, T], BF16); Bckvn = Buf()
        kr = K.sb(st0, [64, T], BF16); Bkr = Buf()
        cos2, sin2, Brope = rope_tables(K, C, st0, W["posb"], Bw)

        with ExitStack() as st:
            win = K.sb(st, [128, KT, 1152], BF16); Bwin = Buf()
            for k0 in range(0, KT, 4):
                S.dma("pool", win[:, k0:k0 + 4, :], win_v[:, k0:k0 + 4, :], reads=[Bw], writes=[Bwin])
            lnw = K.sb(st, [128, KT], F32); Blnw = Buf()
            qnw = K.sb(st, [128, 4], F32); kvnw = K.sb(st, [128, 4], F32)
            S.dma("sp", lnw[:], W["lnw"], reads=[Bw], writes=[Blnw])
            S.dma("sp", qnw[:], W["qnw"], reads=[Bw], writes=[Blnw])
            S.dma("sp", kvnw[:], W["kvnw"], reads=[Bw], writes=[Blnw])
            tmp = norm_tmp(K, st)
            hch = K.sb(st, [128, KT, 512], F32); Bh = [Buf() for _ in range(KT)]
            xn = K.sb(st, [128, KT, 512], BF16); Bxn = Buf()
            cq32 = K.sb(st, [128, 4, 512], F32); Bcq = [Buf() for _ in range(4)]
            ckv32 = K.sb(st, [128, 4, 512], F32); Bckv = [Buf() for _ in range(4)]
            t1 = K.sb(st, [64, 512], F32); Bt1 = Buf()
            t2 = K.sb(st, [64, 512], F32); Bt2 = Buf()
            pn = (K.ps(st), Buf())
            pp = [(K.ps(st), Buf()) for _ in range(3)]
            pc = [0]

            def proj(col0, m, evac):
                p, Bp = pp[pc[0] % 3]
                pc[0] += 1
                for k in range(KT):
                    S.op("pe", lambda e: e.matmul(p[0:m, :], win[:, k, col0:col0 + m], xn[:, k, :], start=(k == 0), stop=(k == KT - 1)),
                         reads=[Bwin, Bxn], writes=[Bp], inc=(k == KT - 1))
                evac(p, Bp)

            for tc in range(T // 512):
                tsl = slice(tc * 512, (tc + 1) * 512)
                S.dma("sp", hch[:], hin_v[:, :, tsl], reads=[Bhin], writes=Bh)
                norm_T(K, C, hch, Bh, xn, Bxn, lnw, Blnw, 512, tmp, pn[0], pn[1])
                for m in range(4):
                    proj(m * 128, 128, lambda p, Bp: S.op("act", lambda e: e.copy(cq32[:, m, :], p[:]), reads=[Bp], writes=[Bcq[m]]))
                for m in range(4):
                    proj(512 + m * 128, 128, lambda p, Bp: S.op("act", lambda e: e.copy(ckv32[:, m, :], p[:]), reads=[Bp], writes=[Bckv[m]]))
                proj(1024, 64, lambda p, Bp: S.op("dve", lambda e: e.tensor_tensor(t1[:], p[0:64, :], cos2[:, tsl], ALU.mult), reads=[Bp, Brope], writes=[Bt1]))
                proj(1088, 64, lambda p, Bp: S.op("dve", lambda e: e.tensor_tensor(t2[:], p[0:64, :], sin2[:, tsl], ALU.mult), reads=[Bp, Brope], writes=[Bt2]))
                S.op("dve", lambda e: e.tensor_tensor(kr[:, tsl], t1[:], t2[:], ALU.add), reads=[Bt1, Bt2], writes=[Bkr])
                norm_T(K, C, cq32, Bcq, cqn, Bcqn, qnw, Blnw, 512, tmp, pn[0], pn[1], nk=4, dim=512, dcol=tc * 512)
                norm_T(K, C, ckv32, Bckv, ckvn, Bckvn, kvnw, Blnw, 512, tmp, pn[0], pn[1], nk=4, dim=512, dcol=tc * 512)
            S.barrier()

        with ExitStack() as st:
            wqb = K.sb(st, [128, 4, 4096], BF16); Bwqb = Buf()
            wkvb = K.sb(st, [128, 4, 4096], BF16); Bwkvb = Buf()
            for k in range(4):
                S.dma("pool", wqb[:, k, :], wqb_v[:, k, :], reads=[Bw], writes=[Bwqb])
                S.dma("pool", wkvb[:, k, :], wkvb_v[:, k, :], reads=[Bw], writes=[Bwkvb])
            qn = [(K.sb(st, [128, T], BF16), Buf()) for _ in range(2)]
            qr = [(K.sb(st, [64, T], BF16), Buf()) for _ in range(2)]
            kn = [(K.sb(st, [128, T], BF16), Buf()) for _ in range(2)]
            Vt = [(K.sb(st, [128, 16, 128], BF16), Buf()) for _ in range(2)]
            oTh = [(K.sb(st, [128, T], BF16), Buf()) for _ in range(2)]
            Pb = [(K.sb(st, [128, T], BF16), Buf()) for _ in range(2)]
            PTb = [(K.sb(st, [128, 16, 128], BF16), Buf()) for _ in range(2)]
            otm = [(K.sb(st, [128, 128], BF16), Buf()) for _ in range(2)]
            sm = [(K.sb(st, [128, 4], F32), Buf()) for _ in range(4)]
            t1 = K.sb(st, [64, 512], F32); Bt1 = Buf()
            t2 = K.sb(st, [64, 512], F32); Bt2 = Buf()
            Sps = K.ps(st, [128, 2560], F32); Bs = [Buf() for _ in range(5)]
            PTp = K.ps(st, [128, 1024], BF16); LPT = Buf()
            pob = K.ps(st); Lpo = Buf()
            ppj = [(K.ps(st), Buf()) for _ in range(1)]
            bc = [0]

            def run_chains(gens):
                gens = list(gens)
                while gens:
                    for g_ in list(gens):
                        try:
                            next(g_)
                        except StopIteration:
                            gens.remove(g_)

            def both(g1, g2):
                gens = [g1, g2]
                while gens:
                    for g_ in list(gens):
                        try:
                            next(g_)
                        except StopIteration:
                            gens.remove(g_)
                    yield

            def projB(wt, Bwt, col0, m, src, Bsrc_, tsl, evac):
                p, Bp = ppj[0]
                for k in range(4):
                    S.op("pe", lambda e: e.matmul(p[0:m, :], wt[:, k, col0:col0 + m], src[:, k, tsl], start=(k == 0), stop=(k == 3)),
                         reads=[Bwt, Bsrc_], writes=[Bp], inc=(k == 3))
                evac(p, Bp)

            def proj_gen(h):
                s = h % 2
                qnt, Bqn = qn[s]
                qrt, Bqr = qr[s]
                knt, Bkn = kn[s]
                vt, Bv = Vt[s]
                for tc in range(4):
                    tsl = slice(tc * 512, (tc + 1) * 512)
                    projB(wqb, Bwqb, h * 256, 128, cqn, Bcqn, tsl,
                          lambda p, Bp: S.op("act", lambda e: e.copy(qnt[:, tsl], p[:]), reads=[Bp], writes=[Bqn]))
                    yield
                    projB(wqb, Bwqb, h * 256 + 128, 64, cqn, Bcqn, tsl,
                          lambda p, Bp: S.op("dve", lambda e: e.tensor_tensor(t1[:], p[0:64, :], cos2[:, tsl], ALU.mult), reads=[Bp, Brope], writes=[Bt1]))
                    yield
                    projB(wqb, Bwqb, h * 256 + 192, 64, cqn, Bcqn, tsl,
                          lambda p, Bp: S.op("dve", lambda e: e.tensor_tensor(t2[:], p[0:64, :], sin2[:, tsl], ALU.mult), reads=[Bp, Brope], writes=[Bt2]))
                    S.op("dve", lambda e: e.tensor_tensor(qrt[:, tsl], t1[:], t2[:], ALU.add), reads=[Bt1, Bt2], writes=[Bqr])
                    yield
                    projB(wkvb, Bwkvb, h * 256, 128, ckvn, Bckvn, tsl,
                          lambda p, Bp: S.op("act", lambda e: e.copy(knt[:, tsl], p[:]), reads=[Bp], writes=[Bkn]))
                    yield
                for g in range(4):
                    p, Bp = ppj[0]
                    for j in range(4):
                        tt = g * 4 + j
                        for k in range(4):
                            S.op("pe", lambda e: e.matmul(p[:, j * 128:(j + 1) * 128], ckvn[:, k, tt * 128:(tt + 1) * 128],
                                                          wkvb[:, k, h * 256 + 128: h * 256 + 256], start=(k == 0), stop=(k == 3)),
                                 reads=[Bckvn, Bwkvb], writes=[Bp], inc=(k == 3 and j == 3))
                    S.op("act", lambda e: e.copy(vt[:, g * 4:(g + 1) * 4, :], p[:].rearrange("p (a c) -> p a c", a=4)), reads=[Bp], writes=[Bv])
                    yield

            def tile_chain(h, i, lane):
                s = h % 2
                qnt, Bqn = qn[s]
                qrt, Bqr = qr[s]
                knt, Bkn = kn[s]
                vt, Bv = Vt[s]
                oth, Both = oTh[s]
                s1 = (i + 1) * 128
                nb = (s1 + 511) // 512
                b0 = 0 if lane == 0 else 5 - nb
                sb = b0 * 512
                qsl = slice(i * 128, (i + 1) * 128)
                smt, Bsm = sm[lane * 2 + (i % 2)]
                P, BP = Pb[lane]
                PT, BPT = PTb[lane]
                ot, Bot = otm[lane]
                for kc in range(nb):
                    n = min(512, s1 - kc * 512)
                    S.op("pe", lambda e: e.matmul(Sps[:, sb + kc * 512: sb + kc * 512 + n], qnt[:, qsl], knt[:, kc * 512: kc * 512 + n], start=True, stop=False),
                         reads=[Bqn, Bkn], writes=[Bs[b0 + kc]], inc=False)
                    S.op("pe", lambda e: e.matmul(Sps[:, sb + kc * 512: sb + kc * 512 + n], qrt[:, qsl], kr[:, kc * 512: kc * 512 + n], start=False, stop=True),
                         reads=[Bqr, Bkr], writes=[Bs[b0 + kc]], inc=True)
                yield
                bd = b0 + i // 4
                dsl = slice(sb + i * 128, sb + (i + 1) * 128)
                S.op("dve", lambda e: e.tensor_tensor(Sps[:, dsl], Sps[:, dsl], maskb, ALU.add), reads=[Bs[bd], C["Bf"]], writes=[Bs[bd]])
                S.op("dve", lambda e: e.memset(smt[:], 0.0), writes=[Bsm])
                S.op("dve", lambda e: e.tensor_reduce(smt[:, 0:1], Sps[:, sb:sb + s1], AX.X, ALU.max), reads=Bs[b0:b0 + nb], writes=[Bsm])
                S.op("dve", lambda e: e.tensor_scalar(smt[:, 1:2], smt[:, 0:1], -scale, None, ALU.mult), reads=[Bsm], writes=[Bsm])
                yield
                S.op("act", lambda e: e.activation(P[:, 0:s1], Sps[:, sb:sb + s1], AF.Exp, bias=smt[:, 1:2], scale=scale, accum_out=smt[:, 2:3]),
                     reads=Bs[b0:b0 + nb] + [Bsm], writes=[BP, Bsm])
                S.op("dve", lambda e: e.reciprocal(smt[:, 3:4], smt[:, 2:3]), reads=[Bsm], writes=[Bsm])
                yield
                po_ = lane * 512
                for g0 in range(0, i + 1, 4):
                    g1 = min(i + 1, g0 + 4)
                    for kt in range(g0, g1):
                        S.op("pe", lambda e: e.transpose(PTp[:, po_ + (kt - g0) * 128: po_ + (kt - g0 + 1) * 128], P[:, kt * 128:(kt + 1) * 128], C["ident_b"]),
                             reads=[BP, C["Bb"]], writes=[LPT], inc=(kt == g1 - 1))
                    yield
                    src = PTp[:, po_: po_ + (g1 - g0) * 128].rearrange("p (a c) -> p a c", c=128)
                    if (g0 // 4 + lane) % 2 == 0:
                        S.op("act", lambda e: e.copy(PT[:, g0:g1, :], src), reads=[], writes=[BPT, LPT])
                    else:
                        S.op("dve", lambda e: e.tensor_copy(PT[:, g0:g1, :], src), reads=[], writes=[BPT, LPT])
                    yield
                oo = lane * 128
                for kt in range(i + 1):
                    S.op("pe", lambda e: e.matmul(pob[:, oo:oo + 128], PT[:, kt, :], vt[:, kt, :], start=(kt == 0), stop=(kt == i)),
                         reads=[BPT, Bv], writes=[Lpo], inc=(kt == i))
                yield
                S.op("dve", lambda e: e.tensor_scalar(ot[:], pob[:, oo:oo + 128], smt[:, 3:4], None, ALU.mult), reads=[Bsm], writes=[Bot, Lpo])
                yield
                oTv = pob[:, 256 + lane * 64: 320 + lane * 64].bitcast(BF16)
                S.op("pe", lambda e: e.transpose(oTv, ot[:], C["ident_b"]), reads=[Bot, C["Bb"]], writes=[Lpo])
                yield
                S.op("act", lambda e: e.copy(oth[:, qsl], oTv), reads=[], writes=[Both, Lpo])
                yield

            def attn_gen(h):
                for i in range(8):
                    yield from both(tile_chain(h, i, 0), tile_chain(h, 15 - i, 1))
                S.dma("sp", oT_v[:, h, :], oTh[h % 2][0][:], reads=[oTh[h % 2][1]], writes=[BoT])

            run_chains([proj_gen(0)])
            for h in range(NH):
                gens = [attn_gen(h)]
                if h + 1 < NH:
                    gens.append(proj_gen(h + 1))
                run_chains(gens)
            S.barrier()

    with ExitStack() as st:
        wo = K.sb(st, [128, KT, D], BF16); Bwo = Buf()
        for k0 in range(0, KT, 4):
            S.dma("pool", wo[:, k0:k0 + 4, :], wo_v[:, k0:k0 + 4, :], reads=[Bw], writes=[Bwo])
        och = [(K.sb(st, [128, KT, 512], BF16), Buf()) for _ in range(2)]
        hch = [(K.sb(st, [128, KT, 512], F32), [Buf() for _ in range(KT)]) for _ in range(2)]
        pcs = [(K.ps(st), Buf()) for _ in range(4)]
        n = 0
        for tc in range(T // 512):
            tsl = slice(tc * 512, (tc + 1) * 512)
            oc, Boc = och[tc % 2]
            hc, Bhc = hch[tc % 2]
            S.dma("sp", oc[:], oT_v[:, :, tsl], reads=[BoT], writes=[Boc])
            S.dma("sp", hc[:], hin_v[:, :, tsl], reads=[Bhin], writes=Bhc)
            for dt in range(KT):
                p, Bp = pcs[n % 4]
                n += 1
                for k in range(KT):
                    S.op("pe", lambda e: e.matmul(p[:], wo[:, k, dt * 128:(dt + 1) * 128], oc[:, k, :], start=(k == 0), stop=(k == KT - 1)),
                         reads=[Bwo, Boc], writes=[Bp], inc=(k == KT - 1))
                S.op("dve", lambda e: e.tensor_tensor(hc[:, dt, :], hc[:, dt, :], p[:], ALU.add), reads=[Bp, Bhc[dt]], writes=[Bhc[dt]])
            S.dma("sp", hout_v[:, :, tsl], hc[:], reads=Bhc, writes=[Bhout])
    S.barrier()


NE = 8
CAP = 768
CT = CAP // 128
CCH = CAP // 2


def phase_moe(K, C, hin, Bhin, outT, Bout, W, Bw):
    S = K.S
    hin_v = hin.rearrange("(k p) t -> p k t", p=128)
    out_v = outT.rearrange("(k p) t -> p k t", p=128)
    XeT = W["XeT"].rearrange("e (k p) c -> e p k c", p=128)
    PTs = W["PTs"].rearrange("(j p) t -> p j t", p=128)
    Ysd = W["Ys"].rearrange("(j p) d -> p j d", p=128)
    BXe, BPT, BYs = K.dbuf("XeT"), K.dbuf("PTs"), K.dbuf("Ys")
    with ExitStack() as stg:
        gs = K.sb(stg, [128, NE, CT], F32); Bgs = Buf()
        with ExitStack() as st1:
            xn_tm = K.sb(st1, [128, 16, D], BF16); Bxtm = Buf()
            logits = K.sb(st1, [128, 16, NE], F32); Blog = Buf()
            rw = K.sb(st1, [128, KT, NE], F32); Brw = Buf()
            rb = K.sb(st1, [128, NE], F32)
            lnw = K.sb(st1, [128, KT], F32); Blnw = Buf()
            S.dma("sp", rw[:], W["rw"], reads=[Bw], writes=[Brw])
            S.dma("sp", rb[:], W["rb"], reads=[Bw], writes=[Brw])
            S.dma("sp", lnw[:], W["lnw"], reads=[Bw], writes=[Blnw])
            with ExitStack() as st:
                tmp = norm_tmp(K, st)
                hch = K.sb(st, [128, KT, 512], F32); Bh = [Buf() for _ in range(KT)]
                xn = K.sb(st, [128, KT, 512], BF16); Bxn = Buf()
                xn32 = K.sb(st, [128, KT, 512], F32); Bxn32 = Buf()
                pn = (K.ps(st), Buf())
                pl = (K.ps(st), Buf())
                ptp = [(K.ps(st, [128, 1024], BF16), Buf()) for _ in range(2)]
                n = 0
                for tc in range(T // 512):
                    tsl = slice(tc * 512, (tc + 1) * 512)
                    S.dma("sp", hch[:], hin_v[:, :, tsl], reads=[Bhin], writes=Bh)
                    norm_T(K, C, hch, Bh, xn, Bxn, lnw, Blnw, 512, tmp, pn[0], pn[1], dst32=(xn32, Bxn32))
                    for j in range(4):
                        tt = tc * 4 + j
                        for k in range(KT):
                            S.op("pe", lambda e: e.matmul(pl[0][:, j * 8:(j + 1) * 8], xn32[:, k, j * 128:(j + 1) * 128], rw[:, k, :], start=(k == 0), stop=(k == KT - 1)),
                                 reads=[Bxn32, Brw], writes=[pl[1]], inc=(k == KT - 1))
                        for g in range(2):
                            p, Bp = ptp[n % 2]
                            n += 1
                            for k in range(8):
                                kk = g * 8 + k
                                S.op("pe", lambda e: e.transpose(p[:, k * 128:(k + 1) * 128], xn[:, kk, j * 128:(j + 1) * 128], C["ident_b"]),
                                     reads=[Bxn, C["Bb"]], writes=[Bp], inc=(k == 7))
                            if g == 0:
                                S.op("act", lambda e: e.copy(xn_tm[:, tt, g * 1024:(g + 1) * 1024], p[:]), reads=[Bp], writes=[Bxtm])
                            else:
                                S.op("dve", lambda e: e.tensor_copy(xn_tm[:, tt, g * 1024:(g + 1) * 1024], p[:]), reads=[Bp], writes=[Bxtm])
                    S.op("dve", lambda e: e.tensor_tensor(logits[:, tc * 4:(tc + 1) * 4, :], pl[0][:, 0:32].rearrange("p (a c) -> p a c", c=8),
                                                          rb[:].unsqueeze(1).to_broadcast([128, 4, NE]), ALU.add), reads=[pl[1], Brw], writes=[Blog])
                S.barrier()
            sh = [128, 16, NE]
            m1 = K.sb(st1, [128, 16], F32); m2 = K.sb(st1, [128, 16], F32)
            g1 = K.sb(st1, [128, 16], F32); g2 = K.sb(st1, [128, 16], F32)
            mk1 = K.sb(st1, sh, F32); mk2 = K.sb(st1, sh, F32); l2 = K.sb(st1, sh, F32)
            sel = K.sb(st1, sh, F32); selb = K.sb(st1, sh, BF16); gate = K.sb(st1, sh, F32)
            ghl = K.sb(st1, [128, 16, NE, 2], BF16); gt = K.sb(st1, sh, F32)
            pos = K.sb(st1, sh, F32)
            Br = Buf()
            bc = lambda a: a[:].unsqueeze(2).to_broadcast(sh)
            R = lambda fn: S.op("dve", fn, reads=[Br, Blog], writes=[Br])
            R(lambda e: e.tensor_reduce(m1[:], logits[:], AX.X, ALU.max))
            R(lambda e: e.tensor_tensor(mk1[:], logits[:], bc(m1), ALU.is_equal))
            R(lambda e: e.scalar_tensor_tensor(l2[:], mk1[:], -1e30, logits[:], ALU.mult, ALU.add))
            R(lambda e: e.tensor_reduce(m2[:], l2[:], AX.X, ALU.max))
            R(lambda e: e.tensor_tensor(mk2[:], l2[:], bc(m2), ALU.is_equal))
            R(lambda e: e.tensor_tensor(sel[:], mk1[:], mk2[:], ALU.add))
            R(lambda e: e.tensor_copy(selb[:], sel[:]))
            R(lambda e: e.tensor_tensor(g2[:], m2[:], m1[:], ALU.subtract))
            S.op("act", lambda e: e.activation(g2[:], g2[:], AF.Exp), reads=[Br], writes=[Br])
            R(lambda e: e.tensor_scalar(g1[:], g2[:], 1.0, None, ALU.add))
            R(lambda e: e.reciprocal(g1[:], g1[:]))
            R(lambda e: e.tensor_tensor(g2[:], g2[:], g1[:], ALU.mult))
            R(lambda e: e.tensor_tensor(gate[:], mk1[:], bc(g1), ALU.mult))
            R(lambda e: e.tensor_tensor(gt[:], mk2[:], bc(g2), ALU.mult))
            R(lambda e: e.tensor_tensor(gate[:], gate[:], gt[:], ALU.add))
            R(lambda e: e.tensor_copy(ghl[:, :, :, 0], gate[:]))
            R(lambda e: e.tensor_copy(gt[:], ghl[:, :, :, 0]))
            R(lambda e: e.tensor_tensor(gt[:], gate[:], gt[:], ALU.subtract))
            R(lambda e: e.tensor_copy(ghl[:, :, :, 1], gt[:]))
            with ExitStack() as st:
                pp = (K.ps(st), Buf())
                for tt in range(16):
                    for i in range(tt + 1):
                        lhs = C["ones_b"] if i < tt else C["U_b"]
                        S.op("pe", lambda e: e.matmul(pp[0][:, tt * 8:(tt + 1) * 8], lhs, selb[:, i, :], start=(i == 0), stop=(i == tt)),
                             reads=[Br, C["Bb"]], writes=[pp[1]], inc=(i == tt))
                S.op("dve", lambda e: e.tensor_copy(pos[:], pp[0][:, 0:128].rearrange("p (a c) -> p a c", c=8)), reads=[pp[1]], writes=[Br])
                S.barrier()
            with ExitStack() as st:
                iota = K.sb(st, [128, CAP], F32); Bio = Buf()
                S.dma("sp", iota[:], W["iota"], reads=[Bw], writes=[Bio])
                Pe = [(K.sb(st, [128, 16, CAP], BF16), Buf()) for _ in range(1)]
                PTst = (K.sb(st, [128, CT, T], BF16), Buf())
                Xst = (K.sb(st, [128, KT, CAP], BF16), Buf())
                ptp = [(K.ps(st, [128, 1024], BF16), Buf()) for _ in range(2)]
                pgs = (K.ps(st), Buf())
                gtmp = K.sb(st, [128, 2 * CT], F32); Bgtmp = Buf()
                pga = [(K.ps(st), Buf()) for _ in range(4)]
                n = 0
                m = 0
                for ex in range(NE):
                    P, BP = Pe[0]
                    for tt in range(16):
                        eng = "dve"
                        S.op(eng, lambda e: e.tensor_scalar(P[:, tt, :], iota[:], pos[:, tt, ex:ex + 1], sel[:, tt, ex:ex + 1], ALU.is_equal, ALU.mult),
                             reads=[Bio, Br], writes=[BP])
                    for ct in range(CT):
                        for g in range(2):
                            p, Bp = ptp[n % 2]
                            n += 1
                            for k in range(8):
                                tt = g * 8 + k
                                S.op("pe", lambda e: e.transpose(p[:, k * 128:(k + 1) * 128], P[:, tt, ct * 128:(ct + 1) * 128], C["ident_b"]),
                                     reads=[BP, C["Bb"]], writes=[Bp], inc=(k == 7))
                            if g == 0:
                                S.op("act", lambda e: e.copy(PTst[0][:, ct, g * 1024:(g + 1) * 1024], p[:]), reads=[Bp], writes=[PTst[1]])
                            else:
                                S.op("dve", lambda e: e.tensor_copy(PTst[0][:, ct, g * 1024:(g + 1) * 1024], p[:]), reads=[Bp], writes=[PTst[1]])
                    S.dma("sp", PTs[:, ex * CT:(ex + 1) * CT, :], PTst[0][:], reads=[PTst[1]], writes=[BPT])
                    for ct in range(CT):
                        for tt in range(16):
                            S.op("pe", lambda e: e.matmul(pgs[0][:, ct * 2:(ct + 1) * 2], P[:, tt, ct * 128:(ct + 1) * 128], ghl[:, tt, ex, :], start=(tt == 0), stop=(tt == 15)),
                                 reads=[BP, Br], writes=[pgs[1]], inc=(tt == 15))
                    S.op("act", lambda e: e.copy(gtmp[:], pgs[0][:, 0:2 * CT]), reads=[pgs[1]], writes=[Bgtmp])
                    pv = gtmp[:].rearrange("p (a c) -> p a c", c=2)
                    S.op("dve", lambda e: e.tensor_tensor(gs[:, ex, :], pv[:, :, 0], pv[:, :, 1], ALU.add), reads=[Bgtmp], writes=[Bgs])
                    for dt in range(KT):
                        for cc in range(2):
                            p, Bp = pga[m % 4]
                            m += 1
                            for tt in range(16):
                                S.op("pe", lambda e: e.matmul(p[:, 0:CCH], xn_tm[:, tt, dt * 128:(dt + 1) * 128], P[:, tt, cc * CCH:(cc + 1) * CCH], start=(tt == 0), stop=(tt == 15)),
                                     reads=[Bxtm, BP], writes=[Bp], inc=(tt == 15))
                            if m % 2 == 0:
                                S.op("act", lambda e: e.copy(Xst[0][:, dt, cc * CCH:(cc + 1) * CCH], p[:, 0:CCH]), reads=[Bp], writes=[Xst[1]])
                            else:
                                S.op("dve", lambda e: e.tensor_copy(Xst[0][:, dt, cc * CCH:(cc + 1) * CCH], p[:, 0:CCH]), reads=[Bp], writes=[Xst[1]])
                    S.dma("sp", XeT[ex], Xst[0][:], reads=[Xst[1]], writes=[BXe])
                S.barrier()

        FC = 256
        NFC = DFF // FC
        NFT = FC // 128
        with ExitStack() as st:
            Xe = [(K.sb(st, [128, KT, CAP], BF16), Buf()) for _ in range(2)]
            yacc = K.sb(st, [128, CT, D], F32); By = [Buf() for _ in range(CT)]
            Yst = (K.sb(st, [128, CT, D], BF16), Buf())
            wgs = [(K.sb(st, [128, KT, FC], BF16), Buf()) for _ in range(2)]
            wus = [(K.sb(st, [128, KT, FC], BF16), Buf()) for _ in range(2)]
            wds = [(K.sb(st, [128, NFT, D], BF16), Buf()) for _ in range(2)]
            hts = [(K.sb(st, [128, NFT, CAP], BF16), Buf()) for _ in range(2)]
            sgs = [(K.sb(st, [128, CCH], F32), Buf()) for _ in range(2)]
            pgs = [(K.ps(st), Buf()) for _ in range(2)]
            pus = [(K.ps(st), Buf()) for _ in range(2)]
            pos_ = [(K.ps(st), Buf()) for _ in range(4)]
            seq = [(ex, fc) for ex in range(NE) for fc in range(NFC)]

            def wview(w, ex):
                return w[ex].rearrange("(k p) f -> p k f", p=128)

            def load_gu(i):
                ex, fc = seq[i]
                s = i % 2
                S.dma("pool", wgs[s][0][:], wview(W["wg"], ex)[:, :, fc * FC:(fc + 1) * FC], reads=[Bw], writes=[wgs[s][1]])
                S.dma("pool", wus[s][0][:], wview(W["wu"], ex)[:, :, fc * FC:(fc + 1) * FC], reads=[Bw], writes=[wus[s][1]])

            def load_d(i):
                ex, fc = seq[i]
                s = i % 2
                S.dma("pool", wds[s][0][:], W["wd"][ex].rearrange("(j p) d -> p j d", p=128)[:, fc * NFT:(fc + 1) * NFT, :], reads=[Bw], writes=[wds[s][1]])

            def load_x(ex):
                S.dma("sp", Xe[ex % 2][0][:], XeT[ex], reads=[BXe], writes=[Xe[ex % 2][1]])

            cnt = [0]
            ocnt = [0]

            def gateup(i):
                ex, fc = seq[i]
                s = i % 2
                x, Bx = Xe[ex % 2]
                wgt, Bwg = wgs[s]
                wut, Bwu = wus[s]
                ht, Bht = hts[s]
                for ft in range(NFT):
                    for cc in range(2):
                        j = cnt[0] % 2
                        cnt[0] += 1
                        pg, Bpg = pgs[j]
                        pu, Bpu = pus[j]
                        sg, Bsg = sgs[j]
                        csl = slice(cc * CCH, (cc + 1) * CCH)
                        for k in range(KT):
                            S.op("pe", lambda e: e.matmul(pg[:, 0:CCH], wgt[:, k, ft * 128:(ft + 1) * 128], x[:, k, csl], start=(k == 0), stop=(k == KT - 1)),
                                 reads=[Bwg, Bx], writes=[Bpg], inc=(k == KT - 1))
                        for k in range(KT):
                            S.op("pe", lambda e: e.matmul(pu[:, 0:CCH], wut[:, k, ft * 128:(ft + 1) * 128], x[:, k, csl], start=(k == 0), stop=(k == KT - 1)),
                                 reads=[Bwu, Bx], writes=[Bpu], inc=(k == KT - 1))
                        S.op("act", lambda e: e.activation(sg[:], pg[:, 0:CCH], AF.Silu), reads=[Bpg], writes=[Bsg])
                        S.op("dve", lambda e: e.tensor_tensor(ht[:, ft, csl], sg[:], pu[:, 0:CCH], ALU.mult), reads=[Bsg, Bpu], writes=[Bht])

            def down(i):
                ex, fc = seq[i]
                s = i % 2
                wdt, Bwd = wds[s]
                ht, Bht = hts[s]
                for ct in range(CT):
                    for dc in range(4):
                        j = ocnt[0] % 4
                        ocnt[0] += 1
                        po, Bpo = pos_[j]
                        dsl = slice(dc * 512, (dc + 1) * 512)
                        for ft in range(NFT):
                            S.op("pe", lambda e: e.matmul(po[:], ht[:, ft, ct * 128:(ct + 1) * 128], wdt[:, ft, dsl], start=(ft == 0), stop=(ft == NFT - 1)),
                                 reads=[Bwd, Bht], writes=[Bpo], inc=(ft == NFT - 1))
                        if fc == 0:
                            S.op("dve", lambda e: e.tensor_copy(yacc[:, ct, dsl], po[:]), reads=[Bpo], writes=[By[ct]])
                        else:
                            S.op("dve", lambda e: e.tensor_tensor(yacc[:, ct, dsl], yacc[:, ct, dsl], po[:], ALU.add), reads=[Bpo, By[ct]], writes=[By[ct]])
                if fc == NFC - 1:
                    for ct in range(CT):
                        if ct % 2 == 0:
                            S.op("act", lambda e: e.activation(Yst[0][:, ct, :], yacc[:, ct, :], AF.Copy, scale=gs[:, ex, ct:ct + 1]), reads=[By[ct], Bgs], writes=[Yst[1]])
                        else:
                            S.op("dve", lambda e: e.tensor_scalar(Yst[0][:, ct, :], yacc[:, ct, :], gs[:, ex, ct:ct + 1], None, ALU.mult), reads=[By[ct], Bgs], writes=[Yst[1]])
                    S.dma("sp", Ysd[:, ex * CT:(ex + 1) * CT, :], Yst[0][:], reads=[Yst[1]], writes=[BYs])

            NS = len(seq)
            load_x(0)
            load_gu(0)
            load_d(0)
            for i in range(NS + 1):
                if i + 1 < NS:
                    load_gu(i + 1)
                if i < NS:
                    if seq[i][1] == 0 and seq[i][0] + 1 < NE:
                        load_x(seq[i][0] + 1)
                    gateup(i)
                if i >= 1:
                    down(i - 1)
                if i + 1 < NS:
                    load_d(i + 1)
            S.barrier()

        with ExitStack() as st:
            NJ = NE * CT
            PTc = (K.sb(st, [128, NJ, 512], BF16), Buf())
            Ysc = [(K.sb(st, [128, NJ, 512], BF16), Buf()) for _ in range(2)]
            hacc = K.sb(st, [128, KT, 512], F32); Bh = [Buf() for _ in range(KT)]
            fnw = K.sb(st, [128, KT], F32); Bfn = Buf()
            S.dma("sp", fnw[:], W["fnw"], reads=[Bw], writes=[Bfn])
            tmp = norm_tmp(K, st)
            pn = (K.ps(st), Buf())
            pss = [(K.ps(st), Buf()) for _ in range(4)]
            n = 0
            q = 0
            for tc in range(T // 512):
                tsl = slice(tc * 512, (tc + 1) * 512)
                S.dma("sp", PTc[0][:], PTs[:, :, tsl], reads=[BPT], writes=[PTc[1]])
                S.dma("sp", hacc[:], hin_v[:, :, tsl], reads=[Bhin], writes=Bh)
                for dq in range(4):
                    ys, Bys = Ysc[q % 2]
                    q += 1
                    S.dma("sp", ys[:], Ysd[:, :, dq * 512:(dq + 1) * 512], reads=[BYs], writes=[Bys])
                    for dl in range(4):
                        dt = dq * 4 + dl
                        p, Bp = pss[n % 4]
                        n += 1
                        for j in range(NJ):
                            S.op("pe", lambda e: e.matmul(p[:], ys[:, j, dl * 128:(dl + 1) * 128], PTc[0][:, j, :], start=(j == 0), stop=(j == NJ - 1)),
                                 reads=[Bys, PTc[1]], writes=[Bp], inc=(j == NJ - 1))
                        S.op("dve", lambda e: e.tensor_tensor(hacc[:, dt, :], hacc[:, dt, :], p[:], ALU.add), reads=[Bp, Bh[dt]], writes=[Bh[dt]])
                Bo = Buf()
                norm_T(K, C, hacc, Bh, hacc, Bo, fnw, Bfn, 512, tmp, pn[0], pn[1])
                S.dma("sp", out_v[:, :, tsl], hacc[:], reads=[Bo] + Bh, writes=[Bout])
        S.barrier()


def out_proj(K, srcT, Bsrc, hin, Bhin, hout, Bhout, wo_d, Bw):
    S = K.S
    hin_v = hin.rearrange("(k p) t -> p k t", p=128)
    hout_v = hout.rearrange("(k p) t -> p k t", p=128)
    src_v = srcT.rearrange("(h p) t -> p h t", p=128)
    wo_v = wo_d.rearrange("(k p) f -> p k f", p=128)
    with ExitStack() as st:
        wo = K.sb(st, [128, KT, D], BF16); Bwo = Buf()
        for k0 in range(0, KT, 4):
            S.dma("pool", wo[:, k0:k0 + 4, :], wo_v[:, k0:k0 + 4, :], reads=[Bw], writes=[Bwo])
        och = [(K.sb(st, [128, KT, 512], BF16), Buf()) for _ in range(2)]
        hch = [(K.sb(st, [128, KT, 512], F32), [Buf() for _ in range(KT)]) for _ in range(2)]
        pcs = [(K.ps(st), Buf()) for _ in range(4)]
        n = 0
        for tc in range(T // 512):
            tsl = slice(tc * 512, (tc + 1) * 512)
            oc, Boc = och[tc % 2]
            hc, Bhc = hch[tc % 2]
            S.dma("sp", oc[:], src_v[:, :, tsl], reads=[Bsrc], writes=[Boc])
            S.dma("sp", hc[:], hin_v[:, :, tsl], reads=[Bhin], writes=Bhc)
            for dt in range(KT):
                p, Bp = pcs[n % 4]
                n += 1
                for k in range(KT):
                    S.op("pe", lambda e: e.matmul(p[:], wo[:, k, dt * 128:(dt + 1) * 128], oc[:, k, :], start=(k == 0), stop=(k == KT - 1)),
                         reads=[Bwo, Boc], writes=[Bp], inc=(k == KT - 1))
                S.op("dve", lambda e: e.tensor_tensor(hc[:, dt, :], hc[:, dt, :], p[:], ALU.add), reads=[Bp, Bhc[dt]], writes=[Bhc[dt]])
            S.dma("sp", hout_v[:, :, tsl], hc[:], reads=Bhc, writes=[Bhout])
    S.barrier()


NEG = -30000.0


def phase_gdn(K, C, hin, Bhin, hout, Bhout, W, Bw, stop=99):
    S = K.S
    hin_v = hin.rearrange("(k p) t -> p k t", p=128)
    win_v = W["win"].rearrange("(k p) f -> p k f", p=128)
    qkvT, zT, goT = W["qkvT"], W["zT"], W["goT"]
    Bqkv, Bz, Bgo = K.dbuf("qkvT"), K.dbuf("zT"), K.dbuf("goT")
    mask_incl = C["f"][:, 640:768]
    mask_strict = C["f"][:, 768:896]
    with ExitStack() as stg:
        sc_kbg = K.sb(stg, [128, 16, 16], F32); sc_kdec = K.sb(stg, [128, 16, 16], F32)
        nbeta = K.sb(stg, [128, 16, 16], F32); beta_tm = K.sb(stg, [128, 16, 16], F32)
        gcT = K.sb(stg, [16, T], F32)
        egl = K.sb(stg, [128, 16, 32], F32)
        stba = ExitStack()
        bT = K.sb(stba, [16, T], F32); aT = K.sb(stba, [16, T], F32); Bba = Buf()
        with ExitStack() as st:
            xn = K.sb(st, [128, KT, T], BF16); Bxn = Buf()
            lnw = K.sb(st, [128, KT], F32); Blnw = Buf()
            cw = K.sb(st, [128, 48, 4], F32)
            S.dma("sp", lnw[:], W["lnw"], reads=[Bw], writes=[Blnw])
            S.dma("sp", cw[:], W["cw"], reads=[Bw], writes=[Blnw])
            tmp = norm_tmp(K, st)
            pn = (K.ps(st), Buf())
            with ExitStack() as st2:
                hch = K.sb(st2, [128, KT, 512], F32); Bh = [Buf() for _ in range(KT)]
                for tc in range(4):
                    S.dma("sp", hch[:], hin_v[:, :, tc * 512:(tc + 1) * 512], reads=[Bhin], writes=Bh)
                    norm_T(K, C, hch, Bh, xn, Bxn, lnw, Blnw, 512, tmp, pn[0], pn[1], dcol=tc * 512)
                S.barrier()
            wb = [(K.sb(st, [128, KT, 512], BF16), Buf()) for _ in range(2)]
            wba = K.sb(st, [128, KT, 32], BF16); Bwba = Buf()
            S.dma("pool", wba[:], win_v[:, :, 8192:8224], reads=[Bw], writes=[Bwba])
            NSET = 2
            pcb = [(K.sb(st, [128, 3 + T], F32), Buf()) for _ in range(NSET)]
            cvb = [(K.sb(st, [128, T], F32), Buf()) for _ in range(NSET)]
            obb = [(K.sb(st, [128, T], BF16), Buf()) for _ in range(NSET)]
            pps = [(K.ps(st), Buf()) for _ in range(4)]
            pst = [(K.ps(st), Buf()) for _ in range(3)] + [pn]
            sq4 = K.sb(st, [128, T], F32); Bsq4 = Buf()
            rs4 = K.sb(st, [128, T], F32); Brs4 = Buf()
            onesr = K.sb(st, [128, 128], F32); Bonesr = Buf()
            S.op("dve", lambda e: e.tensor_copy(onesr[:].bitcast(F32R), C["ones_f"]), reads=[C["Bf"]], writes=[Bonesr])
            for pcx, Bpc in pcb:
                S.op("dve", lambda e: e.memset(pcx[:, 0:3], 0.0), writes=[Bpc])
            pi = [0]

            def projm(wt, Bwt, c0, m, tc, evac):
                p, Bp = pps[pi[0] % 4]
                pi[0] += 1
                for k in range(KT):
                    S.op("pe", lambda e: e.matmul(p[0:m, :], wt[:, k, c0:c0 + m], xn[:, k, tc * 512:(tc + 1) * 512], start=(k == 0), stop=(k == KT - 1)),
                         reads=[Bwt, Bxn], writes=[Bp], inc=(k == KT - 1))
                evac(p, Bp)

            for tc in range(4):
                tsl = slice(tc * 512, (tc + 1) * 512)
                projm(wba, Bwba, 0, 16, tc, lambda p, Bp: S.op("act", lambda e: e.copy(bT[:, tsl], p[0:16, :]), reads=[Bp], writes=[Bba]))
                projm(wba, Bwba, 16, 16, tc, lambda p, Bp: S.op("act", lambda e: e.copy(aT[:, tsl], p[0:16, :]), reads=[Bp], writes=[Bba]))
            S.dma("pool", wb[0][0][:], win_v[:, :, 0:512], reads=[Bw], writes=[wb[0][1]])

            def stageA(m):
                g, ml = m // 4, m % 4
                wt, Bwt = wb[g % 2]
                pcx, Bpc = pcb[m % NSET]
                cv, Bcv = cvb[m % NSET]
                if m >= 48:
                    for tc in range(4):
                        projm(wt, Bwt, ml * 128, 128, tc, lambda p, Bp: S.op("act", lambda e: e.copy(cv[:, tc * 512:(tc + 1) * 512], p[:]), reads=[Bp], writes=[Bcv]))
                    S.dma("sp", zT[(m - 48) * 128:(m - 47) * 128, :], cv[:], reads=[Bcv], writes=[Bz])
                    return
                for tc in range(4):
                    projm(wt, Bwt, ml * 128, 128, tc, lambda p, Bp: S.op("act", lambda e: e.copy(pcx[:, 3 + tc * 512:3 + (tc + 1) * 512], p[:]), reads=[Bp], writes=[Bpc]))
                S.op("dve", lambda e: e.tensor_scalar(cv[:], pcx[:, 0:T], cw[:, m, 0:1], None, ALU.mult), reads=[Bpc, Blnw], writes=[Bcv])
                for j in range(1, 4):
                    S.op("dve", lambda e: e.scalar_tensor_tensor(cv[:], pcx[:, j:j + T], cw[:, m, j:j + 1], cv[:], ALU.mult, ALU.add), reads=[Bpc, Bcv, Blnw], writes=[Bcv])
                S.op("act", lambda e: e.activation(cv[:], cv[:], AF.Silu), reads=[Bcv], writes=[Bcv])

            def stageB(m):
                if m >= 48:
                    return
                cv, Bcv = cvb[m % NSET]
                ob, Bob = obb[m % NSET]
                if m < 32:
                    sc = float(128 ** -0.5) if m < 16 else 1.0
                    for tc in range(4):
                        tsl = slice(tc * 512, (tc + 1) * 512)
                        S.op("act", lambda e: e.activation(sq4[:, tsl].bitcast(F32R), cv[:, tsl], AF.Square), reads=[Bcv], writes=[Bsq4])
                    for tc in range(4):
                        tsl = slice(tc * 512, (tc + 1) * 512)
                        pq, Bpq = pst[tc]
                        S.op("pe", lambda e: e.matmul(pq[:], onesr[:].bitcast(F32R), sq4[:, tsl].bitcast(F32R), start=True, stop=True), reads=[Bsq4, Bonesr], writes=[Bpq])
                    for tc in range(4):
                        tsl = slice(tc * 512, (tc + 1) * 512)
                        pq, Bpq = pst[tc]
                        S.op("act", lambda e: e.activation(rs4[:, tsl], pq[:], AF.Sqrt, bias=tmp["eps"][:, 0:1], scale=1.0), reads=[Bpq, tmp["Beps"]], writes=[Brs4])
                    S.op("dve", lambda e: e.reciprocal(rs4[:], rs4[:]), reads=[Brs4], writes=[Brs4])
                    S.op("dve", lambda e: e.scalar_tensor_tensor(ob[:], cv[:], sc, rs4[:], ALU.mult, ALU.mult), reads=[Bcv, Brs4], writes=[Bob])
                else:
                    S.op("dve", lambda e: e.tensor_copy(ob[:], cv[:]), reads=[Bcv], writes=[Bob])
                S.dma("sp", qkvT[m * 128:(m + 1) * 128, :], ob[:], reads=[Bob], writes=[Bqkv])

            for m in range(64):
                g = m // 4
                if m % 4 == 0 and g + 1 < 16:
                    S.dma("pool", wb[(g + 1) % 2][0][:], win_v[:, :, (g + 1) * 512:(g + 2) * 512], reads=[Bw], writes=[wb[(g + 1) % 2][1]])
                stageA(m)
                if m >= 1:
                    stageB(m - 1)
            stageB(63)
            S.barrier()
        if stop == 1:
            return

        Bg = Buf()
        selh = lambda h: C["f"][0:16, h:h + 1].to_broadcast([16, 128])
        nselh = lambda h: C["f"][0:16, 896 + h:897 + h].to_broadcast([16, 128])
        with ExitStack() as st:
            G = lambda eng, fn: S.op(eng, fn, reads=[Bg, Bba, Blnw2, C["Bf"]], writes=[Bg])
            Blnw2 = Buf()
            alog = K.sb(st, [16, 1], F32); dtb = K.sb(st, [16, 1], F32)
            S.dma("sp", alog[:], W["alog"], reads=[Bw], writes=[Blnw2])
            S.dma("sp", dtb[:], W["dtb"], reads=[Bw], writes=[Blnw2])
            betaT = K.sb(st, [16, T], F32); x = K.sb(st, [16, T], F32); y = K.sb(st, [16, T], F32)
            gT = K.sb(st, [16, T], F32); t1 = K.sb(st, [16, T], F32); glT = K.sb(st, [16, 32], F32); eglT = K.sb(st, [16, 32], F32)
            egcT = K.sb(st, [16, T], F32)
            nea = K.sb(st, [16, 1], F32)
            G("act", lambda e: e.activation(betaT[:], bT[:], AF.Sigmoid))
            G("act", lambda e: e.activation(nea[:], alog[:], AF.Exp))
            G("dve", lambda e: e.tensor_scalar(nea[:], nea[:], -1.0, None, ALU.mult))
            G("dve", lambda e: e.tensor_scalar(x[:], aT[:], dtb[:, 0:1], None, ALU.add))
            G("act", lambda e: e.activation(y[:], x[:], AF.Abs))
            G("act", lambda e: e.activation(y[:], y[:], AF.Exp, scale=-1.0))
            G("act", lambda e: e.activation(y[:], y[:], AF.Ln, bias=1.0))
            G("dve", lambda e: e.tensor_scalar(x[:], x[:], 0.0, None, ALU.max))
            G("dve", lambda e: e.tensor_tensor(x[:], x[:], y[:], ALU.add))
            G("dve", lambda e: e.tensor_scalar(gT[:], x[:], nea[:, 0:1], None, ALU.mult))
            G("dve", lambda e: e.tensor_copy(gcT[:], gT[:]))
            v3 = lambda t: t[:].rearrange("p (c i) -> p c i", i=64)
            sft = 1
            while sft < 64:
                G("dve", lambda e: e.tensor_copy(t1[:], gcT[:]))
                G("dve", lambda e: e.tensor_tensor(v3(gcT)[:, :, sft:64], v3(t1)[:, :, sft:64], v3(t1)[:, :, 0:64 - sft], ALU.add))
                sft *= 2
            G("dve", lambda e: e.tensor_copy(glT[:], v3(gcT)[:, :, 63]))
            G("act", lambda e: e.activation(eglT[:], glT[:], AF.Exp))
            G("act", lambda e: e.activation(egcT[:], gcT[:], AF.Exp))
            G("dve", lambda e: e.tensor_tensor(x[:], betaT[:], egcT[:], ALU.mult))
            G("dve", lambda e: e.tensor_tensor(v3(y), v3(gcT), glT[:].unsqueeze(2).to_broadcast([16, 32, 64]), ALU.subtract))
            G("act", lambda e: e.activation(y[:], y[:], AF.Exp, scale=-1.0))
            pt = (K.ps(st), Buf())
            for src, dst in ((x, sc_kbg), (y, sc_kdec), (betaT, beta_tm)):
                for tt in range(16):
                    S.op("pe", lambda e: e.transpose(pt[0][:, tt * 16:(tt + 1) * 16], src[:, tt * 128:(tt + 1) * 128], C["f"][0:16, 0:16]),
                         reads=[Bg, C["Bf"]], writes=[pt[1]], inc=(tt == 15))
                S.op("dve", lambda e: e.tensor_copy(dst[:], pt[0][:, 0:256].rearrange("p (a c) -> p a c", c=16)), reads=[pt[1]], writes=[Bg])
            G("dve", lambda e: e.tensor_scalar(nbeta[:], beta_tm[:], -1.0, None, ALU.mult))
            for h in range(NH):
                S.op("pe", lambda e: e.matmul(pt[0][:, 0:32], selh(h), eglT[:], start=True, stop=True), reads=[Bg, C["Bf"]], writes=[pt[1]])
                S.op("dve", lambda e: e.tensor_copy(egl[:, h, :], pt[0][:, 0:32]), reads=[pt[1]], writes=[Bg])
            S.barrier()
        stba.close()
        if stop == 2:
            return

        HG = 4
        strict01 = C["f"][:, 768:896]

        def run_chains(gens):
            gens = list(gens)
            while gens:
                for g_ in list(gens):
                    try:
                        next(g_)
                    except StopIteration:
                        gens.remove(g_)

        with ExitStack() as st:
            qkv_v = qkvT.rearrange("(m p) t -> m p t", p=128)
            z_v = zT.rearrange("(m p) t -> m p t", p=128)
            go_v = goT.rearrange("(m p) t -> m p t", p=128)
            gnw = K.sb(st, [128, 1], F32); Bgn = Buf()
            S.dma("sp", gnw[:], W["gnw"], reads=[Bw], writes=[Bgn])
            tmp = norm_tmp(K, st)
            PH = []
            for _ in range(HG):
                d = {}
                for nm in ("qd", "negw"):
                    d[nm] = (K.sb(st, [128, T], BF16), Buf())
                for nm in ("kdec", "vb", "TT", "AT"):
                    d[nm] = (K.sb(st, [128, 16, 128], BF16), Buf())
                d["oT"] = (K.sb(st, [128, T], F32), Buf())
                d["S"] = (K.sb(st, [128, 128], F32), Buf())
                d["Sbf"] = [(K.sb(st, [128, 128], BF16), Buf()) for _ in range(2)]
                d["vn"] = [(K.sb(st, [128, 128], BF16), Buf()) for _ in range(2)]
                for t_, B_ in d["vn"]:
                    S.op("dve", lambda e: e.memset(t_[:], 0.0), writes=[B_])
                PH.append(d)
            qT = K.sb(st, [128, T], BF16); BqT = Buf()
            kT = K.sb(st, [128, T], BF16); BkT = Buf()
            vT = K.sb(st, [128, T], BF16); BvT = Buf()
            kbg = K.sb(st, [128, 16, 128], BF16); Bkbg = Buf()
            zc = [(K.sb(st, [128, 512], F32), Buf()) for _ in range(2)]
            goc = [(K.sb(st, [128, 512], BF16), Buf()) for _ in range(2)]
            CH = []
            for _ in range(4):
                d = {"dm": (K.sb(st, [128, 128], F32), Buf()), "dms": (K.sb(st, [128, 128], F32), Buf()),
                     "M": [(K.sb(st, [128, 128], F32), Buf()) for _ in range(2)], "N": [(K.sb(st, [128, 128], F32), Buf()) for _ in range(2)],
                     "RT": (K.sb(st, [128, 128], F32), Buf()), "A": (K.sb(st, [128, 128], BF16), Buf())}
                CH.append(d)
            pKK = K.ps(st); LKK = Buf()
            pQK = K.ps(st); LQK = Buf()
            pDi = K.ps(st); LDi = Buf()
            pM = K.ps(st); LM = Buf()
            pN = K.ps(st); LN = Buf()
            pR = K.ps(st); LR = Buf()
            pTr = K.ps(st); LTr = Buf()
            pTb = K.ps(st, [128, 1024], BF16); LTb = Buf()
            v2 = lambda ap: ap.rearrange("p (a c) -> p a c", c=128)

            def pre_head(h, ph):
                qd, Bqd = ph["qd"]; negw, Bnw = ph["negw"]; kdec, Bkdec = ph["kdec"]; vb, Bvb = ph["vb"]; TT, BTT = ph["TT"]; AT, BAT = ph["AT"]
                S.dma("sp", qT[:], qkv_v[h], reads=[Bqkv], writes=[BqT])
                S.dma("sp", kT[:], qkv_v[16 + h], reads=[Bqkv], writes=[BkT])
                S.dma("sp", vT[:], qkv_v[32 + h], reads=[Bqkv], writes=[BvT])
                for tc in range(4):
                    tsl = slice(tc * 512, (tc + 1) * 512)
                    eg, Beg = tmp["sq"][tc % 2]
                    S.op("pe", lambda e: e.matmul(pTr[:], selh(h), gcT[:, tsl], start=True, stop=True), reads=[Bg, C["Bf"]], writes=[LTr])
                    S.op("act", lambda e: e.activation(eg[:], pTr[:], AF.Exp), reads=[], writes=[Beg, LTr])
                    S.op("dve", lambda e: e.tensor_tensor(qd[:, tsl], qT[:, tsl], eg[:], ALU.mult), reads=[BqT, Beg], writes=[Bqd])
                for g4 in range(4):
                    gs_ = slice(g4 * 4, (g4 + 1) * 4)
                    for j in range(4):
                        tl = slice((g4 * 4 + j) * 128, (g4 * 4 + j + 1) * 128)
                        S.op("pe", lambda e: e.transpose(pTb[:, j * 128:(j + 1) * 128], kT[:, tl], C["ident_b"]), reads=[BkT, C["Bb"]], writes=[LTb], inc=False)
                        S.op("pe", lambda e: e.transpose(pTb[:, 512 + j * 128:512 + (j + 1) * 128], vT[:, tl], C["ident_b"]), reads=[BvT, C["Bb"]], writes=[LTb], inc=(j == 3))
                    bc4 = lambda t_: t_[:, gs_, h:h + 1].to_broadcast([128, 4, 128])
                    S.op("dve", lambda e: e.tensor_tensor(kbg[:, gs_, :], v2(pTb[:, 0:512]), bc4(sc_kbg), ALU.mult), reads=[Bg], writes=[Bkbg, LTb])
                    S.op("dve", lambda e: e.tensor_tensor(kdec[:, gs_, :], v2(pTb[:, 0:512]), bc4(sc_kdec), ALU.mult), reads=[Bg], writes=[Bkdec, LTb])
                    S.op("dve", lambda e: e.tensor_tensor(vb[:, gs_, :], v2(pTb[:, 512:1024]), bc4(beta_tm), ALU.mult), reads=[Bg], writes=[Bvb, LTb])

                def chain(tt, c):
                    ch = CH[c]
                    dm, Bdm = ch["dm"]; dms, Bdms = ch["dms"]; RT, BRT = ch["RT"]; Ab, BAb = ch["A"]
                    o = c * 128
                    os_ = slice(o, o + 128)
                    tl = slice(tt * 128, (tt + 1) * 128)
                    R32 = lambda ap: ap.bitcast(F32R)
                    S.op("pe", lambda e: e.matmul(pKK[:, os_], kT[:, tl], kT[:, tl], start=True, stop=True), reads=[BkT], writes=[LKK])
                    S.op("pe", lambda e: e.matmul(pQK[:, os_], qT[:, tl], kT[:, tl], start=True, stop=True), reads=[BqT, BkT], writes=[LQK])
                    S.op("pe", lambda e: e.matmul(pDi[:, os_], gcT[:, tl], selh(h), start=True, stop=False), reads=[Bg, C["Bf"]], writes=[LDi], inc=False)
                    S.op("pe", lambda e: e.matmul(pDi[:, os_], nselh(h), gcT[:, tl], start=False, stop=False), reads=[Bg, C["Bf"]], writes=[LDi], inc=False)
                    S.op("pe", lambda e: e.matmul(pDi[:, os_], C["ident_f"], mask_incl, start=False, stop=True), reads=[C["Bf"]], writes=[LDi])
                    yield
                    S.op("act", lambda e: e.activation(dm[:], pDi[:, os_], AF.Exp), reads=[], writes=[Bdm, LDi])
                    yield
                    m0, Bm0 = ch["M"][0]
                    n0, Bn0 = ch["N"][0]
                    S.op("dve", lambda e: e.tensor_tensor(dms[:], dm[:], strict01, ALU.mult), reads=[Bdm, C["Bf"]], writes=[Bdms])
                    S.op("dve", lambda e: e.scalar_tensor_tensor(R32(m0[:]), pKK[:, os_], nbeta[:, tt, h:h + 1], dms[:], ALU.mult, ALU.mult),
                         reads=[Bg, Bdms], writes=[Bm0, LKK])
                    S.op("dve", lambda e: e.tensor_tensor(Ab[:], pQK[:, os_], dm[:], ALU.mult), reads=[Bdm], writes=[BAb, LQK])
                    yield
                    S.op("pe", lambda e: e.transpose(pTr[:, os_], m0[:], C["ident_f"]), reads=[Bm0, C["Bf"]], writes=[LTr])
                    S.op("pe", lambda e: e.transpose(pTb[:, os_], Ab[:], C["ident_b"]), reads=[BAb, C["Bb"]], writes=[LTb])
                    yield
                    S.op("act", lambda e: e.copy(R32(n0[:]), pTr[:, os_]), reads=[], writes=[Bn0, LTr])
                    S.op("act", lambda e: e.copy(AT[:, tt, :], pTb[:, os_]), reads=[], writes=[BAT, LTb])
                    yield
                    S.op("dve", lambda e: e.tensor_tensor(R32(RT[:]), n0[:], C["ident_f"], ALU.add), reads=[Bn0, C["Bf"]], writes=[BRT])
                    S.op("pe", lambda e: e.matmul(pM[:, os_], R32(n0[:]), R32(m0[:]), start=True, stop=True), reads=[Bn0, Bm0], writes=[LM])
                    S.op("pe", lambda e: e.matmul(pN[:, os_], R32(m0[:]), R32(n0[:]), start=True, stop=True), reads=[Bn0, Bm0], writes=[LN])
                    yield
                    for lvl in range(1, 6):
                        mn, Bmn = ch["M"][lvl % 2]
                        nn, Bnn = ch["N"][lvl % 2]
                        if lvl > 1:
                            S.op("dve", lambda e: e.tensor_tensor(R32(RT[:]), RT[:], pR[:, os_], ALU.add), reads=[BRT], writes=[BRT, LR])
                        S.op("act", lambda e: e.copy(R32(mn[:]), pM[:, os_]), reads=[], writes=[Bmn, LM])
                        if lvl < 5:
                            S.op("dve", lambda e: e.tensor_copy(R32(nn[:]), pN[:, os_]), reads=[], writes=[Bnn, LN])
                        yield
                        S.op("pe", lambda e: e.matmul(pR[:, os_], R32(mn[:]), R32(RT[:]), start=True, stop=True), reads=[Bmn, BRT], writes=[LR])
                        if lvl < 5:
                            S.op("pe", lambda e: e.matmul(pM[:, os_], R32(nn[:]), R32(mn[:]), start=True, stop=True), reads=[Bnn, Bmn], writes=[LM])
                        if lvl < 4:
                            S.op("pe", lambda e: e.matmul(pN[:, os_], R32(mn[:]), R32(nn[:]), start=True, stop=True), reads=[Bnn, Bmn], writes=[LN])
                        yield
                    S.op("dve", lambda e: e.tensor_tensor(R32(RT[:]), RT[:], pR[:, os_], ALU.add), reads=[BRT], writes=[BRT, LR])
                    yield
                    S.op("act", lambda e: e.copy(TT[:, tt, :], RT[:]), reads=[BRT], writes=[BTT])
                    yield
                    S.op("pe", lambda e: e.matmul(pR[:, os_], kbg[:, tt, :], TT[:, tt, :], start=True, stop=True), reads=[Bkbg, BTT], writes=[LR])
                    yield
                    S.op("dve", lambda e: e.tensor_scalar(negw[:, tl], pR[:, os_], -1.0, None, ALU.mult), reads=[], writes=[Bnw, LR])

                for rnd in range(4):
                    run_chains([chain(rnd * 4 + c, c) for c in range(4)])

            def scan_head(h, ph, i):
                qd, Bqd = ph["qd"]; negw, Bnw = ph["negw"]; kdec, Bkdec = ph["kdec"]; vb, Bvb = ph["vb"]; TT, BTT = ph["TT"]; AT, BAT = ph["AT"]
                oT, BoT = ph["oT"]; Sst, BS = ph["S"]
                pE, LE, pF, LF, pG_, LG = pKK, LKK, pQK, LQK, pDi, LDi
                S.op("dve", lambda e: e.memset(Sst[:], 0.0), writes=[BS])
                S.op("dve", lambda e: e.memset(ph["Sbf"][0][0][:], 0.0), writes=[ph["Sbf"][0][1]])
                pv = pE[:, i * 128:(i + 1) * 128]
                ps_ = pF[:, i * 128:(i + 1) * 128]
                po = pG_[:, i * 64:(i + 1) * 64]
                for c in range(32):
                    tt, half = c // 2, c % 2
                    gsl = slice(c * 64, (c + 1) * 64)
                    msz = 64 if half == 0 else 128
                    rows = slice(0, 64) if half == 0 else slice(64, 128)
                    sb_, Bsb = ph["Sbf"][c % 2]
                    sbn, Bsbn = ph["Sbf"][(c + 1) % 2]
                    vn, Bvn = ph["vn"][tt % 2]
                    S.op("pe", lambda e: e.matmul(pv[0:msz, :], TT[:, tt, 0:msz], vb[:, tt, :], start=True, stop=False), reads=[BTT, Bvb], writes=[LE], inc=False)
                    S.op("pe", lambda e: e.matmul(pv[0:msz, :], negw[:, tt * 128: tt * 128 + msz], sb_[:], start=False, stop=True), reads=[Bnw, Bsb], writes=[LE])
                    yield
                    S.op("act", lambda e: e.copy(vn[rows, :], pv[rows, :]), reads=[], writes=[Bvn, LE])
                    yield
                    S.op("pe", lambda e: e.matmul(ps_, kdec[rows, tt, :], vn[rows, :], start=True, stop=True), reads=[Bkdec, Bvn], writes=[LF])
                    S.op("pe", lambda e: e.matmul(po, sb_[:], qd[:, gsl], start=True, stop=False), reads=[Bsb, Bqd], writes=[LG], inc=False)
                    S.op("pe", lambda e: e.matmul(po, vn[:], AT[:, tt, half * 64:(half + 1) * 64], start=False, stop=True), reads=[Bvn, BAT], writes=[LG])
                    yield
                    S.op("dve", lambda e: e.scalar_tensor_tensor(sbn[:], Sst[:], egl[:, h, c:c + 1], ps_, ALU.mult, ALU.add), reads=[BS, Bg], writes=[Bsbn, LF])
                    S.op("dve", lambda e: e.scalar_tensor_tensor(Sst[:], Sst[:], egl[:, h, c:c + 1], ps_, ALU.mult, ALU.add), reads=[BS, Bg], writes=[BS, LF])
                    S.op("act", lambda e: e.copy(oT[:, gsl], po), reads=[], writes=[BoT, LG])
                    yield

            def finish_head(h, ph):
                oT, BoT = ph["oT"]
                for tc in range(4):
                    tsl = slice(tc * 512, (tc + 1) * 512)
                    sq, Bsq = tmp["sq"][tc % 2]
                    rs, Brs = tmp["rs"]
                    zt, Bzt = zc[tc % 2]
                    go, Bgo_ = goc[tc % 2]
                    S.dma("sp", zt[:], z_v[h][:, tsl], reads=[Bz], writes=[Bzt])
                    S.op("act", lambda e: e.activation(zt[:], zt[:], AF.Silu), reads=[Bzt], writes=[Bzt])
                    S.op("act", lambda e: e.activation(sq[:], oT[:, tsl], AF.Square), reads=[BoT], writes=[Bsq])
                    S.op("pe", lambda e: e.matmul(pTr[:], C["ones_f"], sq[:], start=True, stop=True), reads=[Bsq, C["Bf"]], writes=[LTr])
                    S.op("act", lambda e: e.activation(rs[:], pTr[:], AF.Sqrt, bias=tmp["eps"][:, 0:1], scale=1.0 / 128), reads=[tmp["Beps"]], writes=[Brs, LTr])
                    S.op("dve", lambda e: e.reciprocal(rs[:], rs[:]), reads=[Brs], writes=[Brs])
                    S.op("dve", lambda e: e.scalar_tensor_tensor(oT[:, tsl], oT[:, tsl], gnw[:, 0:1], rs[:], ALU.mult, ALU.mult), reads=[BoT, Bgn, Brs], writes=[BoT])
                    S.op("dve", lambda e: e.tensor_tensor(go[:], oT[:, tsl], zt[:], ALU.mult), reads=[BoT, Bzt], writes=[Bgo_])
                    S.dma("sp", go_v[h][:, tsl], go[:], reads=[Bgo_], writes=[Bgo])

            for hg in range(NH // HG):
                for i in range(HG):
                    pre_head(hg * HG + i, PH[i])
                run_chains([scan_head(hg * HG + i, PH[i], i) for i in range(HG)])
                for i in range(HG):
                    finish_head(hg * HG + i, PH[i])
            S.barrier()
    out_proj(K, goT, Bgo, hin, Bhin, hout, Bhout, W["wo"], Bw)


def _consts_np():
    c = np.zeros((128, 1024), np.float32)
    c[:, 0:128] = np.eye(128)
    c[:, 128:256] = 1.0
    p = np.arange(128)[:, None]
    q = np.arange(128)[None, :]
    c[:, 256:384] = np.where(q <= p, 0.0, -1e9)
    invf = (np.float32(10000.0) ** (-np.arange(0, 64, 2, dtype=np.float32) / np.float32(64))).astype(np.float32)
    c[0:64, 384] = np.concatenate([invf, invf])
    c[0:32, 385] = -1.0
    c[32:64, 385] = 1.0
    c[:, 512:640] = (p < q).astype(np.float32)
    same = (p // 64) == (q // 64)
    c[:, 640:768] = np.where(same & (q <= p), 0.0, NEG)
    c[:, 768:896] = (same & (q < p)).astype(np.float32)
    c[0:16, 896:912] = -np.eye(16)
    return c


def _pk(v):
    return np.ascontiguousarray(np.asarray(v, np.float32).reshape(-1, 128).T)


EI = "ExternalInput"
_IN_SPECS = [
    ("xT", [D, T], F32), ("posb", [64, T], I32), ("cst", [128, 1024], F32), ("iota", [128, CAP], F32),
    ("lnw_mla", [128, 16], F32), ("win", [D, 1152], F32), ("qnw", [128, 4], F32), ("wqb", [512, 4096], F32), ("kvnw", [128, 4], F32),
    ("wkvb", [512, 4096], F32), ("wo", [D, D], F32),
    ("lnw_ffn", [128, 16], F32), ("fwg", [D, DFF], F32), ("fwu", [D, DFF], F32), ("fwd", [DFF, D], F32),
    ("lnw_gdn", [128, 16], F32), ("gwin", [D, 8224], F32), ("gcw", [128, 48, 4], F32), ("alog", [16, 1], F32), ("dtb", [16, 1], F32),
    ("gnw", [128, 1], F32), ("gwo", [D, D], F32),
    ("lnw_moe", [128, 16], F32), ("rw", [128, 16, 8], F32), ("rb", [128, 8], F32), ("mwg", [NE, D, DFF], F32), ("mwu", [NE, D, DFF], F32),
    ("mwd", [NE, DFF, D], F32), ("fnw", [128, 16], F32),
]


def build_program():
    K = KB()
    A = {}
    for name, shape, dt in _IN_SPECS:
        A[name] = K.dram(name, shape, dt, kind=EI)
    outT = K.dram("outT", [D, T], F32, kind="ExternalOutput")
    h1 = K.dram("h1T", [D, T], F32)
    h2 = K.dram("h2T", [D, T], F32)
    h3 = K.dram("h3T", [D, T], F32)
    Bw = K.dbuf("cst")
    with K.st:
        C = load_consts(K, K.st, A["cst"])
        Wm = {"win": A["win"], "wqb": A["wqb"], "wkvb": A["wkvb"], "wo": A["wo"], "lnw": A["lnw_mla"], "qnw": A["qnw"], "kvnw": A["kvnw"],
              "posb": A["posb"], "oT": K.dram("oT", [D, T], BF16)}
        phase_mla(K, C, A["xT"], K.dbuf("xT"), h1, K.dbuf("h1T"), Wm, Bw)
        phase_ffn(K, C, h1, K.dbuf("h1T"), h2, K.dbuf("h2T"), A["lnw_ffn"], A["fwg"], A["fwu"], A["fwd"], Bw)
        Wg = {"win": A["gwin"], "cw": A["gcw"], "alog": A["alog"], "dtb": A["dtb"], "gnw": A["gnw"], "wo": A["gwo"], "lnw": A["lnw_gdn"],
              "qkvT": K.dram("qkvT", [6144, T], BF16), "zT": K.dram("zT", [D, T], F32), "goT": K.dram("goT", [D, T], BF16)}
        phase_gdn(K, C, h2, K.dbuf("h2T"), h3, K.dbuf("h3T"), Wg, Bw)
        We = {"rw": A["rw"], "rb": A["rb"], "lnw": A["lnw_moe"], "fnw": A["fnw"], "iota": A["iota"], "wg": A["mwg"], "wu": A["mwu"], "wd": A["mwd"],
              "XeT": K.dram("XeT", [NE, D, CAP], BF16), "PTs": K.dram("PTs", [NE * CAP, T], BF16), "Ys": K.dram("Ys", [NE * CAP, D], BF16)}
        phase_moe(K, C, h3, K.dbuf("h3T"), outT, K.dbuf("outT"), We, Bw)
        K.S._wait("sp", [K.dbuf("outT").w])
        K.S.barrier()
    return K


def _host_inputs(inp):
    g = lambda k: np.asarray(inp[k])
    w_in = g("mla_w_in")[0]
    win = np.concatenate([w_in, w_in[:, 1056:1088], w_in[:, 1024:1056]], axis=1)
    wqb = g("mla_w_qb")[0].reshape(512, 16, 192)
    wqb_aug = np.concatenate([wqb, wqb[:, :, 160:192], wqb[:, :, 128:160]], axis=2).reshape(512, 16 * 256)
    shared = {
        "cst": _consts_np(),
        "iota": np.ascontiguousarray(np.broadcast_to(np.arange(CAP, dtype=np.float32)[None, :], (128, CAP))),
        "lnw_mla": _pk(g("ln_mix_mla")[0]), "win": np.ascontiguousarray(win), "qnw": _pk(g("mla_q_norm")[0]), "wqb": np.ascontiguousarray(wqb_aug),
        "kvnw": _pk(g("mla_kv_norm")[0]), "wkvb": np.ascontiguousarray(g("mla_w_kvb")[0]), "wo": np.ascontiguousarray(g("mla_w_o")[0]),
        "lnw_ffn": _pk(g("ln_ffn_dense")[0]), "fwg": np.ascontiguousarray(g("ffn_w_gate")[0]), "fwu": np.ascontiguousarray(g("ffn_w_up")[0]),
        "fwd": np.ascontiguousarray(g("ffn_w_down")[0]),
        "lnw_gdn": _pk(g("ln_mix_gdn")[0]), "gwin": np.ascontiguousarray(g("gdn_w_in")[0]),
        "gcw": np.ascontiguousarray(g("gdn_conv_w")[0].T.reshape(48, 128, 4).transpose(1, 0, 2)),
        "alog": np.ascontiguousarray(g("gdn_a_log")[0].reshape(16, 1)), "dtb": np.ascontiguousarray(g("gdn_dt_bias")[0].reshape(16, 1)),
        "gnw": np.ascontiguousarray(g("gdn_norm")[0].reshape(128, 1)), "gwo": np.ascontiguousarray(g("gdn_w_o")[0]),
        "lnw_moe": _pk(g("ln_ffn_moe")[0]),
        "rw": np.ascontiguousarray(g("moe_router")[0].reshape(16, 128, 8).transpose(1, 0, 2)),
        "rb": np.ascontiguousarray(np.broadcast_to(g("moe_router_bias")[0][None, :], (128, 8))),
        "mwg": np.ascontiguousarray(g("moe_w_gate")[0]), "mwu": np.ascontiguousarray(g("moe_w_up")[0]), "mwd": np.ascontiguousarray(g("moe_w_down")[0]),
        "fnw": _pk(g("final_norm")),
    }
    shared = {k: (v if v.dtype != np.float64 else v.astype(np.float32)) for k, v in shared.items()}
    x = g("x")
    pos = g("positions")
    maps = []
    for b in range(x.shape[0]):
        m = dict(shared)
        m["xT"] = np.ascontiguousarray(x[b].T)
        m["posb"] = np.ascontiguousarray(np.broadcast_to(pos[b][None, :], (64, T))).astype(np.int32)
        maps.append(m)
    return maps


def kernel(**inputs):
    maps = _host_inputs(inputs)
    K = build_program()
    res = run_bass_kernel_spmd(K.nc, maps, core_ids=list(range(len(maps))))
    out = np.stack([np.ascontiguousarray(r["outT"].T) for r in res.results], axis=0)
    return out.astype(np.float32)
```

```python
import numpy as np
from contextlib import ExitStack
import concourse.bass as bass
import concourse.mybir as mybir
from concourse.bass_utils import run_bass_kernel_spmd

F32 = mybir.dt.float32
BF16 = mybir.dt.bfloat16
F32R = mybir.dt.float32r
AF = mybir.ActivationFunctionType
ALU = mybir.AluOpType
AX = mybir.AxisListType

D = 2048
T = 2048
KT = 16
DFF = 7168
EPS = 1e-6


class Buf:
    __slots__ = ("w", "r")

    def __init__(self):
        self.w = None
        self.r = {}


class Sched:
    NDS = 8

    def __init__(self, nc, stack):
        self.nc = nc
        self.engs = {"pe": nc.tensor, "act": nc.scalar, "dve": nc.vector,
                     "pool": nc.gpsimd, "sp": nc.sync}
        self.sem = {k: stack.enter_context(nc.semaphore("s_" + k)) for k in self.engs}
        self.cnt = {k: 0 for k in self.engs}
        self.known = {k: {} for k in self.engs}
        self.dsem, self.dval, self.dnext = {}, {}, {}
        for q in ("sp", "pool"):
            self.dsem[q] = [stack.enter_context(nc.semaphore("d_%s%d" % (q, i))) for i in range(self.NDS)]
            self.dval[q] = [0] * self.NDS
            self.dnext[q] = 0
        self.n_ins = 0
        self.n_wait = 0

    def _semh(self, key):
        if key[0] == "E":
            return self.sem[key[1]]
        return self.dsem[key[1]][key[2]]

    def _wait(self, eng, toks, defer=False):
        need = {}
        for (key, val) in toks:
            if val > need.get(key, 0):
                need[key] = val
        kn = self.known[eng]
        lst = [(key, val) for key, val in need.items() if kn.get(key, 0) < val]
        pend = None
        if defer and lst:
            pend = lst.pop()
        for key, val in lst:
            self.engs[eng].wait_ge(self._semh(key), val)
            self.n_wait += 1
            kn[key] = val
        if pend is not None:
            kn[pend[0]] = pend[1]
        return pend

    def _deps(self, eng, reads, writes):
        toks = []
        own = ("E", eng)
        for b in reads:
            if b.w is not None:
                if b.w[0] == own and eng == "pe":
                    continue
                toks.append(b.w)
        for b in writes:
            if b.w is not None and b.w[0] != own:
                toks.append(b.w)
            for key, val in b.r.items():
                if key != own:
                    toks.append((key, val))
        return toks

    def _mark(self, tok, reads, writes):
        key, val = tok
        for b in reads:
            b.r[key] = val
        for b in writes:
            b.w = tok
            b.r = {}

    def op(self, eng, fn, reads=(), writes=(), inc=True):
        pend = self._wait(eng, self._deps(eng, reads, writes), defer=True)
        ins = fn(self.engs[eng])
        if pend is not None:
            ins._wait_ge(self._semh(pend[0]), pend[1])
        self.n_ins += 1
        if not inc:
            self._mark((("E", eng), self.cnt[eng] + 1), reads, writes)
            return ins
        self.cnt[eng] += 1
        ins.then_inc(self.sem[eng], 1)
        self._mark((("E", eng), self.cnt[eng]), reads, writes)
        return ins

    def dma(self, q, out, in_, reads=(), writes=(), **kw):
        self._wait(q, self._deps(q, reads, writes))
        i = self.dnext[q]
        self.dnext[q] = (i + 1) % self.NDS
        key = ("D", q, i)
        if self.dval[q][i] > 0:
            self._wait(q, [(key, self.dval[q][i])])
        self.dval[q][i] += 16
        self.engs[q].dma_start(out=out, in_=in_, **kw).then_inc(self.dsem[q][i], 16)
        self.n_ins += 1
        tok = (key, self.dval[q][i])
        self._mark(tok, reads, writes)
        return tok

    def barrier(self):
        toks = [(("E", k), v) for k, v in self.cnt.items() if v > 0]
        for q in self.dsem:
            for i in range(self.NDS):
                if self.dval[q][i] > 0:
                    toks.append((("D", q, i), self.dval[q][i]))
        for e in self.engs:
            self._wait(e, [t for t in toks if t[0] != ("E", e)])


class KB:
    def __init__(self):
        self.nc = bass.Bass("TRN2", target_bir_lowering=False)
        self.st = ExitStack()
        self.S = Sched(self.nc, self.st)
        self.uid = 0
        self.dram_bufs = {}

    def name(self, p="t"):
        self.uid += 1
        return "%s%d" % (p, self.uid)

    def sb(self, stack, shape, dt):
        return stack.enter_context(self.nc.sbuf_tensor(self.name("sb"), list(shape), dt))

    def ps(self, stack, shape=(128, 512), dt=F32):
        return stack.enter_context(self.nc.psum_tensor(self.name("ps"), list(shape), dt))

    def dram(self, name, shape, dt, kind="Internal"):
        t = self.nc.dram_tensor(name, list(shape), dt, kind=kind).ap()
        self.dram_bufs[name] = Buf()
        return t

    def dbuf(self, name):
        return self.dram_bufs[name]


def load_consts(K, stack, cst_dram):
    S = K.S
    c = {}
    cf = K.sb(stack, [128, 1024], F32)
    B = Buf()
    S.dma("sp", cf[:], cst_dram, reads=[K.dbuf("cst")], writes=[B])
    cb = K.sb(stack, [128, 1024], BF16)
    Bb = Buf()
    S.op("dve", lambda e: e.tensor_copy(cb[:], cf[:]), reads=[B], writes=[Bb])
    c["f"], c["b"], c["Bf"], c["Bb"] = cf, cb, B, Bb
    c["ident_f"] = cf[:, 0:128]
    c["ones_f"] = cf[:, 128:256]
    c["ident_b"] = cb[:, 0:128]
    c["ones_b"] = cb[:, 128:256]
    c["U_b"] = cb[:, 512:640]
    return c


def norm_T(K, C, src, Bsrc, dst, Bdst, lnw, Blnw, ntok, tmp, ps_bank, Bps, dst32=None, nk=KT, dim=D, dcol=0):
    S = K.S
    for c0 in range(0, ntok, 512):
        n = min(512, ntok - c0)
        for k in range(nk):
            sq, Bsq = tmp["sq"][k % 2]
            S.op("act", lambda e: e.activation(sq[:, :n], src[:, k, c0:c0 + n], AF.Square), reads=[Bsrc[k]], writes=[Bsq])
            S.op("pe", lambda e: e.matmul(ps_bank[:, :n], C["ones_f"], sq[:, :n], start=(k == 0), stop=(k == nk - 1)),
                 reads=[Bsq, C["Bf"]], writes=[Bps], inc=True)
        rs, Brs = tmp["rs"]
        S.op("act", lambda e: e.activation(rs[:, :n], ps_bank[:, :n], AF.Sqrt, bias=tmp["eps"][:, 0:1], scale=1.0 / dim), reads=[Bps, tmp["Beps"]], writes=[Brs])
        S.op("dve", lambda e: e.reciprocal(rs[:, :n], rs[:, :n]), reads=[Brs], writes=[Brs])
        for k in range(nk):
            if dst32 is None:
                S.op("dve", lambda e: e.scalar_tensor_tensor(dst[:, k, dcol + c0:dcol + c0 + n], src[:, k, c0:c0 + n], lnw[:, k:k + 1], rs[:, :n], ALU.mult, ALU.mult),
                     reads=[Bsrc[k], Blnw, Brs], writes=[Bdst])
            else:
                d32, Bd32 = dst32
                S.op("dve", lambda e: e.scalar_tensor_tensor(d32[:, k, c0:c0 + n], src[:, k, c0:c0 + n], lnw[:, k:k + 1], rs[:, :n], ALU.mult, ALU.mult),
                     reads=[Bsrc[k], Blnw, Brs], writes=[Bd32])
                S.op("pool", lambda e: e.tensor_copy(dst[:, k, dcol + c0:dcol + c0 + n], d32[:, k, c0:c0 + n]), reads=[Bd32], writes=[Bdst])


def norm_tmp(K, stack):
    S = K.S
    tmp = {"sq": [], "rs": None}
    for i in range(2):
        tmp["sq"].append((K.sb(stack, [128, 512], F32), Buf()))
    tmp["rs"] = (K.sb(stack, [128, 512], F32), Buf())
    eps = K.sb(stack, [128, 1], F32)
    tmp["eps"] = eps
    tmp["Beps"] = Buf()
    S.op("dve", lambda e: e.memset(eps[:], EPS), writes=[tmp["Beps"]])
    return tmp


def phase_ffn(K, C, hin, Bhin, hout, Bhout, lnw_d, wg, wu, wd, Bw):
    S = K.S
    TH = 1024
    FC = 256
    NFC = DFF // FC
    NFT = FC // 128
    hin_v = hin.rearrange("(k p) t -> p k t", p=128)
    hout_v = hout.rearrange("(k p) t -> p k t", p=128)
    wg_v = wg.rearrange("(k p) f -> p k f", p=128)
    wu_v = wu.rearrange("(k p) f -> p k f", p=128)
    wd_v = wd.rearrange("(j p) d -> p j d", p=128)
    with ExitStack() as st:
        xn = K.sb(st, [128, KT, TH], BF16); Bxn = Buf()
        yacc = K.sb(st, [128, KT, TH], F32); By = [Buf() for _ in range(KT)]
        lnw = K.sb(st, [128, KT], F32); Blnw = Buf()
        S.dma("sp", lnw[:], lnw_d, reads=[Bw], writes=[Blnw])
        tmp = norm_tmp(K, st)
        wgs = [(K.sb(st, [128, KT, FC], BF16), Buf()) for _ in range(2)]
        wus = [(K.sb(st, [128, KT, FC], BF16), Buf()) for _ in range(2)]
        wds = [(K.sb(st, [128, NFT, D], BF16), Buf()) for _ in range(2)]
        hts = [(K.sb(st, [128, NFT, TH], BF16), Buf()) for _ in range(2)]
        sgs = [(K.sb(st, [128, 512], F32), Buf()) for _ in range(2)]
        pgs = [(K.ps(st), Buf()) for _ in range(2)]
        pus = [(K.ps(st), Buf()) for _ in range(2)]
        pos = [(K.ps(st), Buf()) for _ in range(2)]
        pn = (K.ps(st), Buf())

        def load_gu(fc):
            s = fc % 2
            S.dma("pool", wgs[s][0][:], wg_v[:, :, fc * FC:(fc + 1) * FC], reads=[Bw], writes=[wgs[s][1]])
            S.dma("pool", wus[s][0][:], wu_v[:, :, fc * FC:(fc + 1) * FC], reads=[Bw], writes=[wus[s][1]])

        def load_d(fc):
            s = fc % 2
            S.dma("pool", wds[s][0][:], wd_v[:, fc * NFT:(fc + 1) * NFT, :], reads=[Bw], writes=[wds[s][1]])

        cnt = [0]

        def gateup(fc):
            s = fc % 2
            wgt, Bwg = wgs[s]
            wut, Bwu = wus[s]
            ht, Bht = hts[s]
            for ft in range(NFT):
                for tc in range(TH // 512):
                    i = cnt[0] % 2
                    cnt[0] += 1
                    pg, Bpg = pgs[i]
                    pu, Bpu = pus[i]
                    sg, Bsg = sgs[i]
                    tsl = slice(tc * 512, (tc + 1) * 512)
                    for k in range(KT):
                        S.op("pe", lambda e: e.matmul(pg[:], wgt[:, k, ft * 128:(ft + 1) * 128], xn[:, k, tsl], start=(k == 0), stop=(k == KT - 1)),
                             reads=[Bwg, Bxn], writes=[Bpg], inc=(k == KT - 1))
                    for k in range(KT):
                        S.op("pe", lambda e: e.matmul(pu[:], wut[:, k, ft * 128:(ft + 1) * 128], xn[:, k, tsl], start=(k == 0), stop=(k == KT - 1)),
                             reads=[Bwu, Bxn], writes=[Bpu], inc=(k == KT - 1))
                    S.op("act", lambda e: e.activation(sg[:], pg[:], AF.Silu), reads=[Bpg], writes=[Bsg])
                    S.op("dve", lambda e: e.tensor_tensor(ht[:, ft, tsl], sg[:], pu[:], ALU.mult), reads=[Bsg, Bpu], writes=[Bht])

        ocnt = [0]

        def down(fc):
            s = fc % 2
            wdt, Bwd = wds[s]
            ht, Bht = hts[s]
            for dt in range(KT):
                for tc in range(TH // 512):
                    i = ocnt[0] % 2
                    ocnt[0] += 1
                    po, Bpo = pos[i]
                    tsl = slice(tc * 512, (tc + 1) * 512)
                    for ft in range(NFT):
                        S.op("pe", lambda e: e.matmul(po[:], wdt[:, ft, dt * 128:(dt + 1) * 128], ht[:, ft, tsl], start=(ft == 0), stop=(ft == NFT - 1)),
                             reads=[Bwd, Bht], writes=[Bpo], inc=(ft == NFT - 1))
                    S.op("dve", lambda e: e.tensor_tensor(yacc[:, dt, tsl], yacc[:, dt, tsl], po[:], ALU.add), reads=[Bpo, By[dt]], writes=[By[dt]])

        for half in range(T // TH):
            hs = slice(half * TH, (half + 1) * TH)
            S.dma("sp", yacc[:], hin_v[:, :, hs], reads=[Bhin], writes=By)
            load_gu(0)
            load_d(0)
            norm_T(K, C, yacc, By, xn, Bxn, lnw, Blnw, TH, tmp, pn[0], pn[1])
            for fc in range(NFC + 1):
                if fc + 1 < NFC:
                    load_gu(fc + 1)
                if fc < NFC:
                    gateup(fc)
                if fc >= 1:
                    down(fc - 1)
                if fc + 1 < NFC:
                    load_d(fc + 1)
            S.dma("sp", hout_v[:, :, hs], yacc[:], reads=By, writes=[Bhout])
    S.barrier()


I32 = mybir.dt.int32
NH = 16
TWO_PI = float(2 * np.pi)
CW1 = 6.28125
CW2 = float(2 * np.pi - 6.28125)


def rope_tables(K, C, st0, posb, Bpos):
    S = K.S
    cos2 = K.sb(st0, [64, T], F32)
    sin2 = K.sb(st0, [64, T], F32)
    Brope = Buf()
    invf = C["f"][0:64, 384:385]
    sign = C["f"][0:64, 385:386]
    with ExitStack() as st:
        pi_ = K.sb(st, [64, T], I32); Bpi = Buf()
        ang = K.sb(st, [64, T], F32); Bang = Buf()
        a = K.sb(st, [64, T], F32); Ba = Buf()
        ki = K.sb(st, [64, T], I32); Bki = Buf()
        kf = K.sb(st, [64, T], F32); Bkf = Buf()
        m = K.sb(st, [64, T], F32); Bm = Buf()
        negpi = None
        S.dma("sp", pi_[:], posb, reads=[Bpos], writes=[Bpi])
        S.op("dve", lambda e: e.tensor_copy(a[:], pi_[:]), reads=[Bpi], writes=[Ba])
        S.op("dve", lambda e: e.tensor_scalar(ang[:], a[:], invf, None, ALU.mult), reads=[Ba, C["Bf"]], writes=[Bang])
        for dst, shift in ((sin2, 0.0), (cos2, float(np.pi / 2))):
            S.op("dve", lambda e: e.tensor_scalar(a[:], ang[:], shift, None, ALU.add), reads=[Bang], writes=[Ba])
            S.op("dve", lambda e: e.tensor_scalar(ki[:], a[:], 1.0 / TWO_PI, None, ALU.mult), reads=[Ba], writes=[Bki])
            S.op("dve", lambda e: e.tensor_copy(kf[:], ki[:]), reads=[Bki], writes=[Bkf])
            S.op("dve", lambda e: e.scalar_tensor_tensor(a[:], kf[:], -CW1, a[:], ALU.mult, ALU.add), reads=[Bkf, Ba], writes=[Ba])
            S.op("dve", lambda e: e.scalar_tensor_tensor(a[:], kf[:], -CW2, a[:], ALU.mult, ALU.add), reads=[Bkf, Ba], writes=[Ba])
            S.op("dve", lambda e: e.tensor_scalar(m[:], a[:], float(np.pi), -TWO_PI, ALU.is_gt, ALU.mult), reads=[Ba], writes=[Bm])
            S.op("dve", lambda e: e.tensor_tensor(a[:], a[:], m[:], ALU.add), reads=[Ba, Bm], writes=[Ba])
            S.op("dve", lambda e: e.tensor_scalar(m[:], a[:], -float(np.pi), TWO_PI, ALU.is_lt, ALU.mult), reads=[Ba], writes=[Bm])
            S.op("dve", lambda e: e.tensor_tensor(a[:], a[:], m[:], ALU.add), reads=[Ba, Bm], writes=[Ba])
            S.op("act", lambda e: e.activation(dst[:], a[:], AF.Sin), reads=[Ba], writes=[Brope])
        S.op("dve", lambda e: e.tensor_scalar(sin2[:], sin2[:], sign, None, ALU.mult), reads=[Brope, C["Bf"]], writes=[Brope])
        S.barrier()
    return cos2, sin2, Brope


def phase_mla(K, C, hin, Bhin, hout, Bhout, W, Bw):
    S = K.S
    scale = float(192 ** -0.5)
    hin_v = hin.rearrange("(k p) t -> p k t", p=128)
    hout_v = hout.rearrange("(k p) t -> p k t", p=128)
    oT = W["oT"]
    BoT = K.dbuf("oT")
    oT_v = oT.rearrange("(h p) t -> p h t", p=128)
    win_v = W["win"].rearrange("(k p) f -> p k f", p=128)
    wqb_v = W["wqb"].rearrange("(k p) f -> p k f", p=128)
    wkvb_v = W["wkvb"].rearrange("(k p) f -> p k f", p=128)
    wo_v = W["wo"].rearrange("(k p) f -> p k f", p=128)
    maskb = C["f"][:, 256:384]
    with ExitStack() as st0:
        cqn = K.sb(st0, [128, 4, T], BF16); Bcqn = Buf()
        ckvn = K.sb(st0, [128, 4, T], BF16); Bckvn = Buf()
        kr = K.sb(st0, [64, T], BF16); Bkr = Buf()
        cos2, sin2, Brope = rope_tables(K, C, st0, W["posb"], Bw)

        with ExitStack() as st:
            win = K.sb(st, [128, KT, 1152], BF16); Bwin = Buf()
            for k0 in range(0, KT, 4):
                S.dma("pool", win[:, k0:k0 + 4, :], win_v[:, k0:k0 + 4, :], reads=[Bw], writes=[Bwin])
            lnw = K.sb(st, [128, KT], F32); Blnw = Buf()
            qnw = K.sb(st, [128, 4], F32); kvnw = K.sb(st, [128, 4], F32)
            S.dma("sp", lnw[:], W["lnw"], reads=[Bw], writes=[Blnw])
            S.dma("sp", qnw[:], W["qnw"], reads=[Bw], writes=[Blnw])
            S.dma("sp", kvnw[:], W["kvnw"], reads=[Bw], writes=[Blnw])
            tmp = norm_tmp(K, st)
            hch = K.sb(st, [128, KT, 512], F32); Bh = [Buf() for _ in range(KT)]
            xn = K.sb(st, [128, KT, 512], BF16); Bxn = Buf()
            cq32 = K.sb(st, [128, 4, 512], F32); Bcq = [Buf() for _ in range(4)]
            ckv32 = K.sb(st, [128, 4, 512], F32); Bckv = [Buf() for _ in range(4)]
            t1 = K.sb(st, [64, 512], F32); Bt1 = Buf()
            t2 = K.sb(st, [64, 512], F32); Bt2 = Buf()
            pn = (K.ps(st), Buf())
            pp = [(K.ps(st), Buf()) for _ in range(3)]
            pc = [0]

            def proj(col0, m, evac):
                p, Bp = pp[pc[0] % 3]
                pc[0] += 1
                for k in range(KT):
                    S.op("pe", lambda e: e.matmul(p[0:m, :], win[:, k, col0:col0 + m], xn[:, k, :], start=(k == 0), stop=(k == KT - 1)),
                         reads=[Bwin, Bxn], writes=[Bp], inc=(k == KT - 1))
                evac(p, Bp)

            for tc in range(T // 512):
                tsl = slice(tc * 512, (tc + 1) * 512)
                S.dma("sp", hch[:], hin_v[:, :, tsl], reads=[Bhin], writes=Bh)
                norm_T(K, C, hch, Bh, xn, Bxn, lnw, Blnw, 512, tmp, pn[0], pn[1])
                for m in range(4):
                    proj(m * 128, 128, lambda p, Bp: S.op("act", lambda e: e.copy(cq32[:, m, :], p[:]), reads=[Bp], writes=[Bcq[m]]))
                for m in range(4):
                    proj(512 + m * 128, 128, lambda p, Bp: S.op("act", lambda e: e.copy(ckv32[:, m, :], p[:]), reads=[Bp], writes=[Bckv[m]]))
                proj(1024, 64, lambda p, Bp: S.op("dve", lambda e: e.tensor_tensor(t1[:], p[0:64, :], cos2[:, tsl], ALU.mult), reads=[Bp, Brope], writes=[Bt1]))
                proj(1088, 64, lambda p, Bp: S.op("dve", lambda e: e.tensor_tensor(t2[:], p[0:64, :], sin2[:, tsl], ALU.mult), reads=[Bp, Brope], writes=[Bt2]))
                S.op("dve", lambda e: e.tensor_tensor(kr[:, tsl], t1[:], t2[:], ALU.add), reads=[Bt1, Bt2], writes=[Bkr])
                norm_T(K, C, cq32, Bcq, cqn, Bcqn, qnw, Blnw, 512, tmp, pn[0], pn[1], nk=4, dim=512, dcol=tc * 512)
                norm_T(K, C, ckv32, Bckv, ckvn, Bckvn, kvnw, Blnw, 512, tmp, pn[0], pn[1], nk=4, dim=512, dcol=tc * 512)
            S.barrier()

        with ExitStack() as st:
            wqb = K.sb(st, [128, 4, 4096], BF16); Bwqb = Buf()
            wkvb = K.sb(st, [128, 4, 4096], BF16); Bwkvb = Buf()
            for k in range(4):
                S.dma("pool", wqb[:, k, :], wqb_v[:, k, :], reads=[Bw], writes=[Bwqb])
                S.dma("pool", wkvb[:, k, :], wkvb_v[:, k, :], reads=[Bw], writes=[Bwkvb])
            qn = [(K.sb(st, [128, T], BF16), Buf()) for _ in range(2)]
            qr = [(K.sb(st, [64, T], BF16), Buf()) for _ in range(2)]
            kn = [(K.sb(st, [128, T], BF16), Buf()) for _ in range(2)]
            Vt = [(K.sb(st, [128, 16, 128], BF16), Buf()) for _ in range(2)]
            oTh = [(K.sb(st, [128, T], BF16), Buf()) for _ in range(2)]
            Pb = [(K.sb(st, [128, T], BF16), Buf()) for _ in range(2)]
            PTb = [(K.sb(st, [128, 16, 128], BF16), Buf()) for _ in range(2)]
            otm = [(K.sb(st, [128, 128], BF16), Buf()) for _ in range(2)]
            sm = [(K.sb(st, [128, 4], F32), Buf()) for _ in range(4)]
            t1 = K.sb(st, [64, 512], F32); Bt1 = Buf()
            t2 = K.sb(st, [64, 512], F32); Bt2 = Buf()
            Sps = K.ps(st, [128, 2560], F32); Bs = [Buf() for _ in range(5)]
            PTp = K.ps(st, [128, 1024], BF16); LPT = Buf()
            pob = K.ps(st); Lpo = Buf()
            ppj = [(K.ps(st), Buf()) for _ in range(1)]
            bc = [0]

            def run_chains(gens):
                gens = list(gens)
                while gens:
                    for g_ in list(gens):
                        try:
                            next(g_)
                        except StopIteration:
                            gens.remove(g_)

            def both(g1, g2):
                gens = [g1, g2]
                while gens:
                    for g_ in list(gens):
                        try:
                            next(g_)
                        except StopIteration:
                            gens.remove(g_)
                    yield

            def projB(wt, Bwt, col0, m, src, Bsrc_, tsl, evac):
                p, Bp = ppj[0]
                for k in range(4):
                    S.op("pe", lambda e: e.matmul(p[0:m, :], wt[:, k, col0:col0 + m], src[:, k, tsl], start=(k == 0), stop=(k == 3)),
                         reads=[Bwt, Bsrc_], writes=[Bp], inc=(k == 3))
                evac(p, Bp)

            def proj_gen(h):
                s = h % 2
                qnt, Bqn = qn[s]
                qrt, Bqr = qr[s]
                knt, Bkn = kn[s]
                vt, Bv = Vt[s]
                for tc in range(4):
                    tsl = slice(tc * 512, (tc + 1) * 512)
                    projB(wqb, Bwqb, h * 256, 128, cqn, Bcqn, tsl,
                          lambda p, Bp: S.op("act", lambda e: e.copy(qnt[:, tsl], p[:]), reads=[Bp], writes=[Bqn]))
                    yield
                    projB(wqb, Bwqb, h * 256 + 128, 64, cqn, Bcqn, tsl,
                          lambda p, Bp: S.op("dve", lambda e: e.tensor_tensor(t1[:], p[0:64, :], cos2[:, tsl], ALU.mult), reads=[Bp, Brope], writes=[Bt1]))
                    yield
                    projB(wqb, Bwqb, h * 256 + 192, 64, cqn, Bcqn, tsl,
                          lambda p, Bp: S.op("dve", lambda e: e.tensor_tensor(t2[:], p[0:64, :], sin2[:, tsl], ALU.mult), reads=[Bp, Brope], writes=[Bt2]))
                    S.op("dve", lambda e: e.tensor_tensor(qrt[:, tsl], t1[:], t2[:], ALU.add), reads=[Bt1, Bt2], writes=[Bqr])
                    yield
                    projB(wkvb, Bwkvb, h * 256, 128, ckvn, Bckvn, tsl,
                          lambda p, Bp: S.op("act", lambda e: e.copy(knt[:, tsl], p[:]), reads=[Bp], writes=[Bkn]))
                    yield
                for g in range(4):
                    p, Bp = ppj[0]
                    for j in range(4):
                        tt = g * 4 + j
                        for k in range(4):
                            S.op("pe", lambda e: e.matmul(p[:, j * 128:(j + 1) * 128], ckvn[:, k, tt * 128:(tt + 1) * 128],
                                                          wkvb[:, k, h * 256 + 128: h * 256 + 256], start=(k == 0), stop=(k == 3)),
                                 reads=[Bckvn, Bwkvb], writes=[Bp], inc=(k == 3 and j == 3))
                    S.op("act", lambda e: e.copy(vt[:, g * 4:(g + 1) * 4, :], p[:].rearrange("p (a c) -> p a c", a=4)), reads=[Bp], writes=[Bv])
                    yield

            def tile_chain(h, i, lane):
                s = h % 2
                qnt, Bqn = qn[s]
                qrt, Bqr = qr[s]
                knt, Bkn = kn[s]
                vt, Bv = Vt[s]
                oth, Both = oTh[s]
                s1 = (i + 1) * 128
                nb = (s1 + 511) // 512
                b0 = 0 if lane == 0 else 5 - nb
                sb = b0 * 512
                qsl = slice(i * 128, (i + 1) * 128)
                smt, Bsm = sm[lane * 2 + (i % 2)]
                P, BP = Pb[lane]
                PT, BPT = PTb[lane]
                ot, Bot = otm[lane]
                for kc in range(nb):
                    n = min(512, s1 - kc * 512)
                    S.op("pe", lambda e: e.matmul(Sps[:, sb + kc * 512: sb + kc * 512 + n], qnt[:, qsl], knt[:, kc * 512: kc * 512 + n], start=True, stop=False),
                         reads=[Bqn, Bkn], writes=[Bs[b0 + kc]], inc=False)
                    S.op("pe", lambda e: e.matmul(Sps[:, sb + kc * 512: sb + kc * 512 + n], qrt[:, qsl], kr[:, kc * 512: kc * 512 + n], start=False, stop=True),
                         reads=[Bqr, Bkr], writes=[Bs[b0 + kc]], inc=True)
                yield
                bd = b0 + i // 4
                dsl = slice(sb + i * 128, sb + (i + 1) * 128)
                S.op("dve", lambda e: e.tensor_tensor(Sps[:, dsl], Sps[:, dsl], maskb, ALU.add), reads=[Bs[bd], C["Bf"]], writes=[Bs[bd]])
                S.op("dve", lambda e: e.memset(smt[:], 0.0), writes=[Bsm])
                S.op("dve", lambda e: e.tensor_reduce(smt[:, 0:1], Sps[:, sb:sb + s1], AX.X, ALU.max), reads=Bs[b0:b0 + nb], writes=[Bsm])
                S.op("dve", lambda e: e.tensor_scalar(smt[:, 1:2], smt[:, 0:1], -scale, None, ALU.mult), reads=[Bsm], writes=[Bsm])
                yield
                S.op("act", lambda e: e.activation(P[:, 0:s1], Sps[:, sb:sb + s1], AF.Exp, bias=smt[:, 1:2], scale=scale, accum_out=smt[:, 2:3]),
                     reads=Bs[b0:b0 + nb] + [Bsm], writes=[BP, Bsm])
                S.op("dve", lambda e: e.reciprocal(smt[:, 3:4], smt[:, 2:3]), reads=[Bsm], writes=[Bsm])
                yield
                po_ = lane * 512
                for g0 in range(0, i + 1, 4):
                    g1 = min(i + 1, g0 + 4)
                    for kt in range(g0, g1):
                        S.op("pe", lambda e: e.transpose(PTp[:, po_ + (kt - g0) * 128: po_ + (kt - g0 + 1) * 128], P[:, kt * 128:(kt + 1) * 128], C["ident_b"]),
                             reads=[BP, C["Bb"]], writes=[LPT], inc=(kt == g1 - 1))
                    yield
                    src = PTp[:, po_: po_ + (g1 - g0) * 128].rearrange("p (a c) -> p a c", c=128)
                    if (g0 // 4 + lane) % 2 == 0:
                        S.op("act", lambda e: e.copy(PT[:, g0:g1, :], src), reads=[], writes=[BPT, LPT])
                    else:
                        S.op("dve", lambda e: e.tensor_copy(PT[:, g0:g1, :], src), reads=[], writes=[BPT, LPT])
                    yield
                oo = lane * 128
                for kt in range(i + 1):
                    S.op("pe", lambda e: e.matmul(pob[:, oo:oo + 128], PT[:, kt, :], vt[:, kt, :], start=(kt == 0), stop=(kt == i)),
                         reads=[BPT, Bv], writes=[Lpo], inc=(kt == i))
                yield
                S.op("dve", lambda e: e.tensor_scalar(ot[:], pob[:, oo:oo + 128], smt[:, 3:4], None, ALU.mult), reads=[Bsm], writes=[Bot, Lpo])
                yield
                oTv = pob[:, 256 + lane * 64: 320 + lane * 64].bitcast(BF16)
                S.op("pe", lambda e: e.transpose(oTv, ot[:], C["ident_b"]), reads=[Bot, C["Bb"]], writes=[Lpo])
                yield
                S.op("act", lambda e: e.copy(oth[:, qsl], oTv), reads=[], writes=[Both, Lpo])
                yield

            def attn_gen(h):
                for i in range(8):
                    yield from both(tile_chain(h, i, 0), tile_chain(h, 15 - i, 1))
                S.dma("sp", oT_v[:, h, :], oTh[h % 2][0][:], reads=[oTh[h % 2][1]], writes=[BoT])

            run_chains([proj_gen(0)])
            for h in range(NH):
                gens = [attn_gen(h)]
                if h + 1 < NH:
                    gens.append(proj_gen(h + 1))
                run_chains(gens)
            S.barrier()

    with ExitStack() as st:
        wo = K.sb(st, [128, KT, D], BF16); Bwo = Buf()
        for k0 in range(0, KT, 4):
            S.dma("pool", wo[:, k0:k0 + 4, :], wo_v[:, k0:k0 + 4, :], reads=[Bw], writes=[Bwo])
        och = [(K.sb(st, [128, KT, 512], BF16), Buf()) for _ in range(2)]
        hch = [(K.sb(st, [128, KT, 512], F32), [Buf() for _ in range(KT)]) for _ in range(2)]
        pcs = [(K.ps(st), Buf()) for _ in range(4)]
        n = 0
        for tc in range(T // 512):
            tsl = slice(tc * 512, (tc + 1) * 512)
            oc, Boc = och[tc % 2]
            hc, Bhc = hch[tc % 2]
            S.dma("sp", oc[:], oT_v[:, :, tsl], reads=[BoT], writes=[Boc])
            S.dma("sp", hc[:], hin_v[:, :, tsl], reads=[Bhin], writes=Bhc)
            for dt in range(KT):
                p, Bp = pcs[n % 4]
                n += 1
                for k in range(KT):
                    S.op("pe", lambda e: e.matmul(p[:], wo[:, k, dt * 128:(dt + 1) * 128], oc[:, k, :], start=(k == 0), stop=(k == KT - 1)),
                         reads=[Bwo, Boc], writes=[Bp], inc=(k == KT - 1))
                S.op("dve", lambda e: e.tensor_tensor(hc[:, dt, :], hc[:, dt, :], p[:], ALU.add), reads=[Bp, Bhc[dt]], writes=[Bhc[dt]])
            S.dma("sp", hout_v[:, :, tsl], hc[:], reads=Bhc, writes=[Bhout])
    S.barrier()


NE = 8
CAP = 768
CT = CAP // 128
CCH = CAP // 2


def phase_moe(K, C, hin, Bhin, outT, Bout, W, Bw):
    S = K.S
    hin_v = hin.rearrange("(k p) t -> p k t", p=128)
    out_v = outT.rearrange("(k p) t -> p k t", p=128)
    XeT = W["XeT"].rearrange("e (k p) c -> e p k c", p=128)
    PTs = W["PTs"].rearrange("(j p) t -> p j t", p=128)
    Ysd = W["Ys"].rearrange("(j p) d -> p j d", p=128)
    BXe, BPT, BYs = K.dbuf("XeT"), K.dbuf("PTs"), K.dbuf("Ys")
    with ExitStack() as stg:
        gs = K.sb(stg, [128, NE, CT], F32); Bgs = Buf()
        with ExitStack() as st1:
            xn_tm = K.sb(st1, [128, 16, D], BF16); Bxtm = Buf()
            logits = K.sb(st1, [128, 16, NE], F32); Blog = Buf()
            rw = K.sb(st1, [128, KT, NE], F32); Brw = Buf()
            rb = K.sb(st1, [128, NE], F32)
            lnw = K.sb(st1, [128, KT], F32); Blnw = Buf()
            S.dma("sp", rw[:], W["rw"], reads=[Bw], writes=[Brw])
            S.dma("sp", rb[:], W["rb"], reads=[Bw], writes=[Brw])
            S.dma("sp", lnw[:], W["lnw"], reads=[Bw], writes=[Blnw])
            with ExitStack() as st:
                tmp = norm_tmp(K, st)
                hch = K.sb(st, [128, KT, 512], F32); Bh = [Buf() for _ in range(KT)]
                xn = K.sb(st, [128, KT, 512], BF16); Bxn = Buf()
                xn32 = K.sb(st, [128, KT, 512], F32); Bxn32 = Buf()
                pn = (K.ps(st), Buf())
                pl = (K.ps(st), Buf())
                ptp = [(K.ps(st, [128, 1024], BF16), Buf()) for _ in range(2)]
                n = 0
                for tc in range(T // 512):
                    tsl = slice(tc * 512, (tc + 1) * 512)
                    S.dma("sp", hch[:], hin_v[:, :, tsl], reads=[Bhin], writes=Bh)
                    norm_T(K, C, hch, Bh, xn, Bxn, lnw, Blnw, 512, tmp, pn[0], pn[1], dst32=(xn32, Bxn32))
                    for j in range(4):
                        tt = tc * 4 + j
                        for k in range(KT):
                            S.op("pe", lambda e: e.matmul(pl[0][:, j * 8:(j + 1) * 8], xn32[:, k, j * 128:(j + 1) * 128], rw[:, k, :], start=(k == 0), stop=(k == KT - 1)),
                                 reads=[Bxn32, Brw], writes=[pl[1]], inc=(k == KT - 1))
                        for g in range(2):
                            p, Bp = ptp[n % 2]
                            n += 1
                            for k in range(8):
                                kk = g * 8 + k
                                S.op("pe", lambda e: e.transpose(p[:, k * 128:(k + 1) * 128], xn[:, kk, j * 128:(j + 1) * 128], C["ident_b"]),
                                     reads=[Bxn, C["Bb"]], writes=[Bp], inc=(k == 7))
                            if g == 0:
                                S.op("act", lambda e: e.copy(xn_tm[:, tt, g * 1024:(g + 1) * 1024], p[:]), reads=[Bp], writes=[Bxtm])
                            else:
                                S.op("dve", lambda e: e.tensor_copy(xn_tm[:, tt, g * 1024:(g + 1) * 1024], p[:]), reads=[Bp], writes=[Bxtm])
                    S.op("dve", lambda e: e.tensor_tensor(logits[:, tc * 4:(tc + 1) * 4, :], pl[0][:, 0:32].rearrange("p (a c) -> p a c", c=8),
                                                          rb[:].unsqueeze(1).to_broadcast([128, 4, NE]), ALU.add), reads=[pl[1], Brw], writes=[Blog])
                S.barrier()
            sh = [128, 16, NE]
            m1 = K.sb(st1, [128, 16], F32); m2 = K.sb(st1, [128, 16], F32)
            g1 = K.sb(st1, [128, 16], F32); g2 = K.sb(st1, [128, 16], F32)
            mk1 = K.sb(st1, sh, F32); mk2 = K.sb(st1, sh, F32); l2 = K.sb(st1, sh, F32)
            sel = K.sb(st1, sh, F32); selb = K.sb(st1, sh, BF16); gate = K.sb(st1, sh, F32)
            ghl = K.sb(st1, [128, 16, NE, 2], BF16); gt = K.sb(st1, sh, F32)
            pos = K.sb(st1, sh, F32)
            Br = Buf()
            bc = lambda a: a[:].unsqueeze(2).to_broadcast(sh)
            R = lambda fn: S.op("dve", fn, reads=[Br, Blog], writes=[Br])
            R(lambda e: e.tensor_reduce(m1[:], logits[:], AX.X, ALU.max))
            R(lambda e: e.tensor_tensor(mk1[:], logits[:], bc(m1), ALU.is_equal))
            R(lambda e: e.scalar_tensor_tensor(l2[:], mk1[:], -1e30, logits[:], ALU.mult, ALU.add))
            R(lambda e: e.tensor_reduce(m2[:], l2[:], AX.X, ALU.max))
            R(lambda e: e.tensor_tensor(mk2[:], l2[:], bc(m2), ALU.is_equal))
            R(lambda e: e.tensor_tensor(sel[:], mk1[:], mk2[:], ALU.add))
            R(lambda e: e.tensor_copy(selb[:], sel[:]))
            R(lambda e: e.tensor_tensor(g2[:], m2[:], m1[:], ALU.subtract))
            S.op("act", lambda e: e.activation(g2[:], g2[:], AF.Exp), reads=[Br], writes=[Br])
            R(lambda e: e.tensor_scalar(g1[:], g2[:], 1.0, None, ALU.add))
            R(lambda e: e.reciprocal(g1[:], g1[:]))
            R(lambda e: e.tensor_tensor(g2[:], g2[:], g1[:], ALU.mult))
            R(lambda e: e.tensor_tensor(gate[:], mk1[:], bc(g1), ALU.mult))
            R(lambda e: e.tensor_tensor(gt[:], mk2[:], bc(g2), ALU.mult))
            R(lambda e: e.tensor_tensor(gate[:], gate[:], gt[:], ALU.add))
            R(lambda e: e.tensor_copy(ghl[:, :, :, 0], gate[:]))
            R(lambda e: e.tensor_copy(gt[:], ghl[:, :, :, 0]))
            R(lambda e: e.tensor_tensor(gt[:], gate[:], gt[:], ALU.subtract))
            R(lambda e: e.tensor_copy(ghl[:, :, :, 1], gt[:]))
            with ExitStack() as st:
                pp = (K.ps(st), Buf())
                for tt in range(16):
                    for i in range(tt + 1):
                        lhs = C["ones_b"] if i < tt else C["U_b"]
                        S.op("pe", lambda e: e.matmul(pp[0][:, tt * 8:(tt + 1) * 8], lhs, selb[:, i, :], start=(i == 0), stop=(i == tt)),
                             reads=[Br, C["Bb"]], writes=[pp[1]], inc=(i == tt))
                S.op("dve", lambda e: e.tensor_copy(pos[:], pp[0][:, 0:128].rearrange("p (a c) -> p a c", c=8)), reads=[pp[1]], writes=[Br])
                S.barrier()
            with ExitStack() as st:
                iota = K.sb(st, [128, CAP], F32); Bio = Buf()
                S.dma("sp", iota[:], W["iota"], reads=[Bw], writes=[Bio])
                Pe = [(K.sb(st, [128, 16, CAP], BF16), Buf()) for _ in range(1)]
                PTst = (K.sb(st, [128, CT, T], BF16), Buf())
                Xst = (K.sb(st, [128, KT, CAP], BF16), Buf())
                ptp = [(K.ps(st, [128, 1024], BF16), Buf()) for _ in range(2)]
                pgs = (K.ps(st), Buf())
                gtmp = K.sb(st, [128, 2 * CT], F32); Bgtmp = Buf()
                pga = [(K.ps(st), Buf()) for _ in range(4)]
                n = 0
                m = 0
                for ex in range(NE):
                    P, BP = Pe[0]
                    for tt in range(16):
                        eng = "dve"
                        S.op(eng, lambda e: e.tensor_scalar(P[:, tt, :], iota[:], pos[:, tt, ex:ex + 1], sel[:, tt, ex:ex + 1], ALU.is_equal, ALU.mult),
                             reads=[Bio, Br], writes=[BP])
                    for ct in range(CT):
                        for g in range(2):
                            p, Bp = ptp[n % 2]
                            n += 1
                            for k in range(8):
                                tt = g * 8 + k
                                S.op("pe", lambda e: e.transpose(p[:, k * 128:(k + 1) * 128], P[:, tt, ct * 128:(ct + 1) * 128], C["ident_b"]),
                                     reads=[BP, C["Bb"]], writes=[Bp], inc=(k == 7))
                            if g == 0:
                                S.op("act", lambda e: e.copy(PTst[0][:, ct, g * 1024:(g + 1) * 1024], p[:]), reads=[Bp], writes=[PTst[1]])
                            else:
                                S.op("dve", lambda e: e.tensor_copy(PTst[0][:, ct, g * 1024:(g + 1) * 1024], p[:]), reads=[Bp], writes=[PTst[1]])
                    S.dma("sp", PTs[:, ex * CT:(ex + 1) * CT, :], PTst[0][:], reads=[PTst[1]], writes=[BPT])
                    for ct in range(CT):
                        for tt in range(16):
                            S.op("pe", lambda e: e.matmul(pgs[0][:, ct * 2:(ct + 1) * 2], P[:, tt, ct * 128:(ct + 1) * 128], ghl[:, tt, ex, :], start=(tt == 0), stop=(tt == 15)),
                                 reads=[BP, Br], writes=[pgs[1]], inc=(tt == 15))
                    S.op("act", lambda e: e.copy(gtmp[:], pgs[0][:, 0:2 * CT]), reads=[pgs[1]], writes=[Bgtmp])
                    pv = gtmp[:].rearrange("p (a c) -> p a c", c=2)
                    S.op("dve", lambda e: e.tensor_tensor(gs[:, ex, :], pv[:, :, 0], pv[:, :, 1], ALU.add), reads=[Bgtmp], writes=[Bgs])
                    for dt in range(KT):
                        for cc in range(2):
                            p, Bp = pga[m % 4]
                            m += 1
                            for tt in range(16):
                                S.op("pe", lambda e: e.matmul(p[:, 0:CCH], xn_tm[:, tt, dt * 128:(dt + 1) * 128], P[:, tt, cc * CCH:(cc + 1) * CCH], start=(tt == 0), stop=(tt == 15)),
                                     reads=[Bxtm, BP], writes=[Bp], inc=(tt == 15))
                            if m % 2 == 0:
                                S.op("act", lambda e: e.copy(Xst[0][:, dt, cc * CCH:(cc + 1) * CCH], p[:, 0:CCH]), reads=[Bp], writes=[Xst[1]])
                            else:
                                S.op("dve", lambda e: e.tensor_copy(Xst[0][:, dt, cc * CCH:(cc + 1) * CCH], p[:, 0:CCH]), reads=[Bp], writes=[Xst[1]])
                    S.dma("sp", XeT[ex], Xst[0][:], reads=[Xst[1]], writes=[BXe])
                S.barrier()

        FC = 256
        NFC = DFF // FC
        NFT = FC // 128
        with ExitStack() as st:
            Xe = [(K.sb(st, [128, KT, CAP], BF16), Buf()) for _ in range(2)]
            yacc = K.sb(st, [128, CT, D], F32); By = [Buf() for _ in range(CT)]
            Yst = (K.sb(st, [128, CT, D], BF16), Buf())
            wgs = [(K.sb(st, [128, KT, FC], BF16), Buf()) for _ in range(2)]
            wus = [(K.sb(st, [128, KT, FC], BF16), Buf()) for _ in range(2)]
            wds = [(K.sb(st, [128, NFT, D], BF16), Buf()) for _ in range(2)]
            hts = [(K.sb(st, [128, NFT, CAP], BF16), Buf()) for _ in range(2)]
            sgs = [(K.sb(st, [128, CCH], F32), Buf()) for _ in range(2)]
            pgs = [(K.ps(st), Buf()) for _ in range(2)]
            pus = [(K.ps(st), Buf()) for _ in range(2)]
            pos_ = [(K.ps(st), Buf()) for _ in range(4)]
            seq = [(ex, fc) for ex in range(NE) for fc in range(NFC)]

            def wview(w, ex):
                return w[ex].rearrange("(k p) f -> p k f", p=128)

            def load_gu(i):
                ex, fc = seq[i]
                s = i % 2
                S.dma("pool", wgs[s][0][:], wview(W["wg"], ex)[:, :, fc * FC:(fc + 1) * FC], reads=[Bw], writes=[wgs[s][1]])
                S.dma("pool", wus[s][0][:], wview(W["wu"], ex)[:, :, fc * FC:(fc + 1) * FC], reads=[Bw], writes=[wus[s][1]])

            def load_d(i):
                ex, fc = seq[i]
                s = i % 2
                S.dma("pool", wds[s][0][:], W["wd"][ex].rearrange("(j p) d -> p j d", p=128)[:, fc * NFT:(fc + 1) * NFT, :], reads=[Bw], writes=[wds[s][1]])

            def load_x(ex):
                S.dma("sp", Xe[ex % 2][0][:], XeT[ex], reads=[BXe], writes=[Xe[ex % 2][1]])

            cnt = [0]
            ocnt = [0]

            def gateup(i):
                ex, fc = seq[i]
                s = i % 2
                x, Bx = Xe[ex % 2]
                wgt, Bwg = wgs[s]
                wut, Bwu = wus[s]
                ht, Bht = hts[s]
                for ft in range(NFT):
                    for cc in range(2):
                        j = cnt[0] % 2
                        cnt[0] += 1
                        pg, Bpg = pgs[j]
                        pu, Bpu = pus[j]
                        sg, Bsg = sgs[j]
                        csl = slice(cc * CCH, (cc + 1) * CCH)
                        for k in range(KT):
                            S.op("pe", lambda e: e.matmul(pg[:, 0:CCH], wgt[:, k, ft * 128:(ft + 1) * 128], x[:, k, csl], start=(k == 0), stop=(k == KT - 1)),
                                 reads=[Bwg, Bx], writes=[Bpg], inc=(k == KT - 1))
                        for k in range(KT):
                            S.op("pe", lambda e: e.matmul(pu[:, 0:CCH], wut[:, k, ft * 128:(ft + 1) * 128], x[:, k, csl], start=(k == 0), stop=(k == KT - 1)),
                                 reads=[Bwu, Bx], writes=[Bpu], inc=(k == KT - 1))
                        S.op("act", lambda e: e.activation(sg[:], pg[:, 0:CCH], AF.Silu), reads=[Bpg], writes=[Bsg])
                        S.op("dve", lambda e: e.tensor_tensor(ht[:, ft, csl], sg[:], pu[:, 0:CCH], ALU.mult), reads=[Bsg, Bpu], writes=[Bht])

            def down(i):
                ex, fc = seq[i]
                s = i % 2
                wdt, Bwd = wds[s]
                ht, Bht = hts[s]
                for ct in range(CT):
                    for dc in range(4):
                        j = ocnt[0] % 4
                        ocnt[0] += 1
                        po, Bpo = pos_[j]
                        dsl = slice(dc * 512, (dc + 1) * 512)
                        for ft in range(NFT):
                            S.op("pe", lambda e: e.matmul(po[:], ht[:, ft, ct * 128:(ct + 1) * 128], wdt[:, ft, dsl], start=(ft == 0), stop=(ft == NFT - 1)),
                                 reads=[Bwd, Bht], writes=[Bpo], inc=(ft == NFT - 1))
                        if fc == 0:
                            S.op("dve", lambda e: e.tensor_copy(yacc[:, ct, dsl], po[:]), reads=[Bpo], writes=[By[ct]])
                        else:
                            S.op("dve", lambda e: e.tensor_tensor(yacc[:, ct, dsl], yacc[:, ct, dsl], po[:], ALU.add), reads=[Bpo, By[ct]], writes=[By[ct]])
                if fc == NFC - 1:
                    for ct in range(CT):
                        if ct % 2 == 0:
                            S.op("act", lambda e: e.activation(Yst[0][:, ct, :], yacc[:, ct, :], AF.Copy, scale=gs[:, ex, ct:ct + 1]), reads=[By[ct], Bgs], writes=[Yst[1]])
                        else:
                            S.op("dve", lambda e: e.tensor_scalar(Yst[0][:, ct, :], yacc[:, ct, :], gs[:, ex, ct:ct + 1], None, ALU.mult), reads=[By[ct], Bgs], writes=[Yst[1]])
                    S.dma("sp", Ysd[:, ex * CT:(ex + 1) * CT, :], Yst[0][:], reads=[Yst[1]], writes=[BYs])

            NS = len(seq)
            load_x(0)
            load_gu(0)
            load_d(0)
            for i in range(NS + 1):
                if i + 1 < NS:
                    load_gu(i + 1)
                if i < NS:
                    if seq[i][1] == 0 and seq[i][0] + 1 < NE:
                        load_x(seq[i][0] + 1)
                    gateup(i)
                if i >= 1:
                    down(i - 1)
                if i + 1 < NS:
                    load_d(i + 1)
            S.barrier()

        with ExitStack() as st:
            NJ = NE * CT
            PTc = (K.sb(st, [128, NJ, 512], BF16), Buf())
            Ysc = [(K.sb(st, [128, NJ, 512], BF16), Buf()) for _ in range(2)]
            hacc = K.sb(st, [128, KT, 512], F32); Bh = [Buf() for _ in range(KT)]
            fnw = K.sb(st, [128, KT], F32); Bfn = Buf()
            S.dma("sp", fnw[:], W["fnw"], reads=[Bw], writes=[Bfn])
            tmp = norm_tmp(K, st)
            pn = (K.ps(st), Buf())
            pss = [(K.ps(st), Buf()) for _ in range(4)]
            n = 0
            q = 0
            for tc in range(T // 512):
                tsl = slice(tc * 512, (tc + 1) * 512)
                S.dma("sp", PTc[0][:], PTs[:, :, tsl], reads=[BPT], writes=[PTc[1]])
                S.dma("sp", hacc[:], hin_v[:, :, tsl], reads=[Bhin], writes=Bh)
                for dq in range(4):
                    ys, Bys = Ysc[q % 2]
                    q += 1
                    S.dma("sp", ys[:], Ysd[:, :, dq * 512:(dq + 1) * 512], reads=[BYs], writes=[Bys])
                    for dl in range(4):
                        dt = dq * 4 + dl
                        p, Bp = pss[n % 4]
                        n += 1
                        for j in range(NJ):
                            S.op("pe", lambda e: e.matmul(p[:], ys[:, j, dl * 128:(dl + 1) * 128], PTc[0][:, j, :], start=(j == 0), stop=(j == NJ - 1)),
                                 reads=[Bys, PTc[1]], writes=[Bp], inc=(j == NJ - 1))
                        S.op("dve", lambda e: e.tensor_tensor(hacc[:, dt, :], hacc[:, dt, :], p[:], ALU.add), reads=[Bp, Bh[dt]], writes=[Bh[dt]])
                Bo = Buf()
                norm_T(K, C, hacc, Bh, hacc, Bo, fnw, Bfn, 512, tmp, pn[0], pn[1])
                S.dma("sp", out_v[:, :, tsl], hacc[:], reads=[Bo] + Bh, writes=[Bout])
        S.barrier()


def out_proj(K, srcT, Bsrc, hin, Bhin, hout, Bhout, wo_d, Bw):
    S = K.S
    hin_v = hin.rearrange("(k p) t -> p k t", p=128)
    hout_v = hout.rearrange("(k p) t -> p k t", p=128)
    src_v = srcT.rearrange("(h p) t -> p h t", p=128)
    wo_v = wo_d.rearrange("(k p) f -> p k f", p=128)
    with ExitStack() as st:
        wo = K.sb(st, [128, KT, D], BF16); Bwo = Buf()
        for k0 in range(0, KT, 4):
            S.dma("pool", wo[:, k0:k0 + 4, :], wo_v[:, k0:k0 + 4, :], reads=[Bw], writes=[Bwo])
        och = [(K.sb(st, [128, KT, 512], BF16), Buf()) for _ in range(2)]
        hch = [(K.sb(st, [128, KT, 512], F32), [Buf() for _ in range(KT)]) for _ in range(2)]
        pcs = [(K.ps(st), Buf()) for _ in range(4)]
        n = 0
        for tc in range(T // 512):
            tsl = slice(tc * 512, (tc + 1) * 512)
            oc, Boc = och[tc % 2]
            hc, Bhc = hch[tc % 2]
            S.dma("sp", oc[:], src_v[:, :, tsl], reads=[Bsrc], writes=[Boc])
            S.dma("sp", hc[:], hin_v[:, :, tsl], reads=[Bhin], writes=Bhc)
            for dt in range(KT):
                p, Bp = pcs[n % 4]
                n += 1
                for k in range(KT):
                    S.op("pe", lambda e: e.matmul(p[:], wo[:, k, dt * 128:(dt + 1) * 128], oc[:, k, :], start=(k == 0), stop=(k == KT - 1)),
                         reads=[Bwo, Boc], writes=[Bp], inc=(k == KT - 1))
                S.op("dve", lambda e: e.tensor_tensor(hc[:, dt, :], hc[:, dt, :], p[:], ALU.add), reads=[Bp, Bhc[dt]], writes=[Bhc[dt]])
            S.dma("sp", hout_v[:, :, tsl], hc[:], reads=Bhc, writes=[Bhout])
    S.barrier()


NEG = -30000.0


def phase_gdn(K, C, hin, Bhin, hout, Bhout, W, Bw, stop=99):
    S = K.S
    hin_v = hin.rearrange("(k p) t -> p k t", p=128)
    win_v = W["win"].rearrange("(k p) f -> p k f", p=128)
    qkvT, zT, goT = W["qkvT"], W["zT"], W["goT"]
    Bqkv, Bz, Bgo = K.dbuf("qkvT"), K.dbuf("zT"), K.dbuf("goT")
    mask_incl = C["f"][:, 640:768]
    mask_strict = C["f"][:, 768:896]
    with ExitStack() as stg:
        sc_kbg = K.sb(stg, [128, 16, 16], F32); sc_kdec = K.sb(stg, [128, 16, 16], F32)
        nbeta = K.sb(stg, [128, 16, 16], F32); beta_tm = K.sb(stg, [128, 16, 16], F32)
        gc_tm = K.sb(stg, [128, 16, 16], F32)
        gcT = K.sb(stg, [16, T], F32)
        egl = K.sb(stg, [128, 16, 32], F32)
        stba = ExitStack()
        bT = K.sb(stba, [16, T], F32); aT = K.sb(stba, [16, T], F32); Bba = Buf()
        with ExitStack() as st:
            xn = K.sb(st, [128, KT, T], BF16); Bxn = Buf()
            lnw = K.sb(st, [128, KT], F32); Blnw = Buf()
            cw = K.sb(st, [128, 48, 4], F32)
            S.dma("sp", lnw[:], W["lnw"], reads=[Bw], writes=[Blnw])
            S.dma("sp", cw[:], W["cw"], reads=[Bw], writes=[Blnw])
            tmp = norm_tmp(K, st)
            pn = (K.ps(st), Buf())
            with ExitStack() as st2:
                hch = K.sb(st2, [128, KT, 512], F32); Bh = [Buf() for _ in range(KT)]
                for tc in range(4):
                    S.dma("sp", hch[:], hin_v[:, :, tc * 512:(tc + 1) * 512], reads=[Bhin], writes=Bh)
                    norm_T(K, C, hch, Bh, xn, Bxn, lnw, Blnw, 512, tmp, pn[0], pn[1], dcol=tc * 512)
                S.barrier()
            wb = [(K.sb(st, [128, KT, 512], BF16), Buf()) for _ in range(2)]
            wba = K.sb(st, [128, KT, 32], BF16); Bwba = Buf()
            S.dma("pool", wba[:], win_v[:, :, 8192:8224], reads=[Bw], writes=[Bwba])
            NSET = 2
            pcb = [(K.sb(st, [128, 3 + T], F32), Buf()) for _ in range(NSET)]
            cvb = [(K.sb(st, [128, T], F32), Buf()) for _ in range(NSET)]
            obb = [(K.sb(st, [128, T], BF16), Buf()) for _ in range(NSET)]
            pps = [(K.ps(st), Buf()) for _ in range(4)]
            pst = [(K.ps(st), Buf()) for _ in range(3)] + [pn]
            sq4 = K.sb(st, [128, T], F32); Bsq4 = Buf()
            rs4 = K.sb(st, [128, T], F32); Brs4 = Buf()
            onesr = K.sb(st, [128, 128], F32); Bonesr = Buf()
            S.op("dve", lambda e: e.tensor_copy(onesr[:].bitcast(F32R), C["ones_f"]), reads=[C["Bf"]], writes=[Bonesr])
            for pcx, Bpc in pcb:
                S.op("dve", lambda e: e.memset(pcx[:, 0:3], 0.0), writes=[Bpc])
            pi = [0]

            def projm(wt, Bwt, c0, m, tc, evac):
                p, Bp = pps[pi[0] % 4]
                pi[0] += 1
                for k in range(KT):
                    S.op("pe", lambda e: e.matmul(p[0:m, :], wt[:, k, c0:c0 + m], xn[:, k, tc * 512:(tc + 1) * 512], start=(k == 0), stop=(k == KT - 1)),
                         reads=[Bwt, Bxn], writes=[Bp], inc=(k == KT - 1))
                evac(p, Bp)

            for tc in range(4):
                tsl = slice(tc * 512, (tc + 1) * 512)
                projm(wba, Bwba, 0, 16, tc, lambda p, Bp: S.op("act", lambda e: e.copy(bT[:, tsl], p[0:16, :]), reads=[Bp], writes=[Bba]))
                projm(wba, Bwba, 16, 16, tc, lambda p, Bp: S.op("act", lambda e: e.copy(aT[:, tsl], p[0:16, :]), reads=[Bp], writes=[Bba]))
            S.dma("pool", wb[0][0][:], win_v[:, :, 0:512], reads=[Bw], writes=[wb[0][1]])

            def stageA(m):
                g, ml = m // 4, m % 4
                wt, Bwt = wb[g % 2]
                pcx, Bpc = pcb[m % NSET]
                cv, Bcv = cvb[m % NSET]
                if m >= 48:
                    for tc in range(4):
                        projm(wt, Bwt, ml * 128, 128, tc, lambda p, Bp: S.op("act", lambda e: e.copy(cv[:, tc * 512:(tc + 1) * 512], p[:]), reads=[Bp], writes=[Bcv]))
                    S.dma("sp", zT[(m - 48) * 128:(m - 47) * 128, :], cv[:], reads=[Bcv], writes=[Bz])
                    return
                for tc in range(4):
                    projm(wt, Bwt, ml * 128, 128, tc, lambda p, Bp: S.op("act", lambda e: e.copy(pcx[:, 3 + tc * 512:3 + (tc + 1) * 512], p[:]), reads=[Bp], writes=[Bpc]))
                S.op("dve", lambda e: e.tensor_scalar(cv[:], pcx[:, 0:T], cw[:, m, 0:1], None, ALU.mult), reads=[Bpc, Blnw], writes=[Bcv])
                for j in range(1, 4):
                    S.op("dve", lambda e: e.scalar_tensor_tensor(cv[:], pcx[:, j:j + T], cw[:, m, j:j + 1], cv[:], ALU.mult, ALU.add), reads=[Bpc, Bcv, Blnw], writes=[Bcv])
                S.op("act", lambda e: e.activation(cv[:], cv[:], AF.Silu), reads=[Bcv], writes=[Bcv])

            def stageB(m):
                if m >= 48:
                    return
                cv, Bcv = cvb[m % NSET]
                ob, Bob = obb[m % NSET]
                if m < 32:
                    sc = float(128 ** -0.5) if m < 16 else 1.0
                    for tc in range(4):
                        tsl = slice(tc * 512, (tc + 1) * 512)
                        S.op("act", lambda e: e.activation(sq4[:, tsl].bitcast(F32R), cv[:, tsl], AF.Square), reads=[Bcv], writes=[Bsq4])
                    for tc in range(4):
                        tsl = slice(tc * 512, (tc + 1) * 512)
                        pq, Bpq = pst[tc]
                        S.op("pe", lambda e: e.matmul(pq[:], onesr[:].bitcast(F32R), sq4[:, tsl].bitcast(F32R), start=True, stop=True), reads=[Bsq4, Bonesr], writes=[Bpq])
                    for tc in range(4):
                        tsl = slice(tc * 512, (tc + 1) * 512)
                        pq, Bpq = pst[tc]
                        S.op("act", lambda e: e.activation(rs4[:, tsl], pq[:], AF.Sqrt, bias=tmp["eps"][:, 0:1], scale=1.0), reads=[Bpq, tmp["Beps"]], writes=[Brs4])
                    S.op("dve", lambda e: e.reciprocal(rs4[:], rs4[:]), reads=[Brs4], writes=[Brs4])
                    S.op("dve", lambda e: e.scalar_tensor_tensor(ob[:], cv[:], sc, rs4[:], ALU.mult, ALU.mult), reads=[Bcv, Brs4], writes=[Bob])
                else:
                    S.op("dve", lambda e: e.tensor_copy(ob[:], cv[:]), reads=[Bcv], writes=[Bob])
                S.dma("sp", qkvT[m * 128:(m + 1) * 128, :], ob[:], reads=[Bob], writes=[Bqkv])

            for m in range(64):
                g = m // 4
                if m % 4 == 0 and g + 1 < 16:
                    S.dma("pool", wb[(g + 1) % 2][0][:], win_v[:, :, (g + 1) * 512:(g + 2) * 512], reads=[Bw], writes=[wb[(g + 1) % 2][1]])
                stageA(m)
                if m >= 1:
                    stageB(m - 1)
            stageB(63)
            S.barrier()
        if stop == 1:
            return

        Bg = Buf()
        selh = lambda h: C["f"][0:16, h:h + 1].to_broadcast([16, 128])
        nselh = lambda h: C["f"][0:16, 896 + h:897 + h].to_broadcast([16, 128])
        with ExitStack() as st:
            G = lambda eng, fn: S.op(eng, fn, reads=[Bg, Bba, Blnw2, C["Bf"]], writes=[Bg])
            Blnw2 = Buf()
            alog = K.sb(st, [16, 1], F32); dtb = K.sb(st, [16, 1], F32)
            S.dma("sp", alog[:], W["alog"], reads=[Bw], writes=[Blnw2])
            S.dma("sp", dtb[:], W["dtb"], reads=[Bw], writes=[Blnw2])
            betaT = K.sb(st, [16, T], F32); x = K.sb(st, [16, T], F32); y = K.sb(st, [16, T], F32)
            gT = K.sb(st, [16, T], F32); t1 = K.sb(st, [16, T], F32); glT = K.sb(st, [16, 32], F32); eglT = K.sb(st, [16, 32], F32)
            egcT = K.sb(st, [16, T], F32)
            nea = K.sb(st, [16, 1], F32)
            G("act", lambda e: e.activation(betaT[:], bT[:], AF.Sigmoid))
            G("act", lambda e: e.activation(nea[:], alog[:], AF.Exp))
            G("dve", lambda e: e.tensor_scalar(nea[:], nea[:], -1.0, None, ALU.mult))
            G("dve", lambda e: e.tensor_scalar(x[:], aT[:], dtb[:, 0:1], None, ALU.add))
            G("act", lambda e: e.activation(y[:], x[:], AF.Abs))
            G("act", lambda e: e.activation(y[:], y[:], AF.Exp, scale=-1.0))
            G("act", lambda e: e.activation(y[:], y[:], AF.Ln, bias=1.0))
            G("dve", lambda e: e.tensor_scalar(x[:], x[:], 0.0, None, ALU.max))
            G("dve", lambda e: e.tensor_tensor(x[:], x[:], y[:], ALU.add))
            G("dve", lambda e: e.tensor_scalar(gT[:], x[:], nea[:, 0:1], None, ALU.mult))
            G("dve", lambda e: e.tensor_copy(gcT[:], gT[:]))
            v3 = lambda t: t[:].rearrange("p (c i) -> p c i", i=64)
            sft = 1
            while sft < 64:
                G("dve", lambda e: e.tensor_copy(t1[:], gcT[:]))
                G("dve", lambda e: e.tensor_tensor(v3(gcT)[:, :, sft:64], v3(t1)[:, :, sft:64], v3(t1)[:, :, 0:64 - sft], ALU.add))
                sft *= 2
            G("dve", lambda e: e.tensor_copy(glT[:], v3(gcT)[:, :, 63]))
            G("act", lambda e: e.activation(eglT[:], glT[:], AF.Exp))
            G("act", lambda e: e.activation(egcT[:], gcT[:], AF.Exp))
            G("dve", lambda e: e.tensor_tensor(x[:], betaT[:], egcT[:], ALU.mult))
            G("dve", lambda e: e.tensor_tensor(v3(y), v3(gcT), glT[:].unsqueeze(2).to_broadcast([16, 32, 64]), ALU.subtract))
            G("act", lambda e: e.activation(y[:], y[:], AF.Exp, scale=-1.0))
            pt = (K.ps(st), Buf())
            for src, dst in ((x, sc_kbg), (y, sc_kdec), (betaT, beta_tm), (gcT, gc_tm)):
                for tt in range(16):
                    S.op("pe", lambda e: e.transpose(pt[0][:, tt * 16:(tt + 1) * 16], src[:, tt * 128:(tt + 1) * 128], C["f"][0:16, 0:16]),
                         reads=[Bg, C["Bf"]], writes=[pt[1]], inc=(tt == 15))
                S.op("dve", lambda e: e.tensor_copy(dst[:], pt[0][:, 0:256].rearrange("p (a c) -> p a c", c=16)), reads=[pt[1]], writes=[Bg])
            G("dve", lambda e: e.tensor_scalar(nbeta[:], beta_tm[:], -1.0, None, ALU.mult))
            for h in range(NH):
                S.op("pe", lambda e: e.matmul(pt[0][:, 0:32], selh(h), eglT[:], start=True, stop=True), reads=[Bg, C["Bf"]], writes=[pt[1]])
                S.op("dve", lambda e: e.tensor_copy(egl[:, h, :], pt[0][:, 0:32]), reads=[pt[1]], writes=[Bg])
            S.barrier()
        stba.close()
        if stop == 2:
            return

        HG = 4
        strict01 = C["f"][:, 768:896]

        def run_chains(gens, stagger=0):
            gens = list(gens)
            for k_, g_ in enumerate(list(gens)):
                for _ in range(k_ * stagger):
                    try:
                        next(g_)
                    except StopIteration:
                        gens.remove(g_)
                        break
            while gens:
                for g_ in list(gens):
                    try:
                        next(g_)
                    except StopIteration:
                        gens.remove(g_)

        with ExitStack() as st:
            qkv_v = qkvT.rearrange("(m p) t -> m p t", p=128)
            z_v = zT.rearrange("(m p) t -> m p t", p=128)
            go_v = goT.rearrange("(m p) t -> m p t", p=128)
            gnw = K.sb(st, [128, 1], F32); Bgn = Buf()
            S.dma("sp", gnw[:], W["gnw"], reads=[Bw], writes=[Bgn])
            tmp = norm_tmp(K, st)
            PH = []
            for _ in range(HG):
                d = {}
                for nm in ("qd", "negw"):
                    d[nm] = (K.sb(st, [128, T], BF16), Buf())
                for nm in ("kdec", "vb", "TT", "AT"):
                    d[nm] = (K.sb(st, [128, 16, 128], BF16), Buf())
                d["oT"] = (K.sb(st, [128, T], F32), Buf())
                d["S"] = (K.sb(st, [128, 128], F32), Buf())
                d["Sbf"] = [(K.sb(st, [128, 128], BF16), Buf()) for _ in range(2)]
                d["vn"] = [(K.sb(st, [128, 128], BF16), Buf()) for _ in range(2)]
                for t_, B_ in d["vn"]:
                    S.op("dve", lambda e: e.memset(t_[:], 0.0), writes=[B_])
                PH.append(d)
            qT = K.sb(st, [128, T], BF16); BqT = Buf()
            kT = K.sb(st, [128, T], BF16); BkT = Buf()
            vT = K.sb(st, [128, T], BF16); BvT = Buf()
            kbg = K.sb(st, [128, 16, 128], BF16); Bkbg = Buf()
            gcrow = K.sb(st, [128, 512], F32); Bgcrow = Buf()
            zc = [(K.sb(st, [128, 512], F32), Buf()) for _ in range(2)]
            goc = [(K.sb(st, [128, 512], BF16), Buf()) for _ in range(2)]
            CH = []
            for _ in range(4):
                d = {"dm": (K.sb(st, [128, 128], F32), Buf()), "dms": (K.sb(st, [128, 128], F32), Buf()),
                     "M": [(K.sb(st, [128, 128], F32), Buf()) for _ in range(2)], "N": [(K.sb(st, [128, 128], F32), Buf()) for _ in range(2)],
                     "RT": (K.sb(st, [128, 128], F32), Buf()), "A": (K.sb(st, [128, 128], BF16), Buf())}
                CH.append(d)
            pKK = K.ps(st); LKK = Buf()
            pQK = K.ps(st); LQK = Buf()
            pDi = K.ps(st); LDi = Buf()
            pM = K.ps(st); LM = Buf()
            pN = K.ps(st); LN = Buf()
            pR = K.ps(st); LR = Buf()
            pTr = K.ps(st); LTr = Buf()
            pTb = K.ps(st, [128, 1024], BF16); LTb = Buf()
            v2 = lambda ap: ap.rearrange("p (a c) -> p a c", c=128)

            def pre_head(h, ph):
                qd, Bqd = ph["qd"]; negw, Bnw = ph["negw"]; kdec, Bkdec = ph["kdec"]; vb, Bvb = ph["vb"]; TT, BTT = ph["TT"]; AT, BAT = ph["AT"]
                S.dma("sp", qT[:], qkv_v[h], reads=[Bqkv], writes=[BqT])
                S.dma("sp", kT[:], qkv_v[16 + h], reads=[Bqkv], writes=[BkT])
                S.dma("sp", vT[:], qkv_v[32 + h], reads=[Bqkv], writes=[BvT])
                for tc in range(4):
                    tsl = slice(tc * 512, (tc + 1) * 512)
                    eg, Beg = tmp["sq"][tc % 2]
                    S.op("pe", lambda e: e.matmul(pTr[:], selh(h), gcT[:, tsl], start=True, stop=True), reads=[Bg, C["Bf"]], writes=[LTr])
                    S.op("act", lambda e: e.activation(eg[:], pTr[:], AF.Exp), reads=[], writes=[Beg, LTr])
                    S.op("dve", lambda e: e.tensor_tensor(qd[:, tsl], qT[:, tsl], eg[:], ALU.mult), reads=[BqT, Beg], writes=[Bqd])
                for g4 in range(4):
                    gs_ = slice(g4 * 4, (g4 + 1) * 4)
                    for j in range(4):
                        tl = slice((g4 * 4 + j) * 128, (g4 * 4 + j + 1) * 128)
                        S.op("pe", lambda e: e.transpose(pTb[:, j * 128:(j + 1) * 128], kT[:, tl], C["ident_b"]), reads=[BkT, C["Bb"]], writes=[LTb], inc=False)
                        S.op("pe", lambda e: e.transpose(pTb[:, 512 + j * 128:512 + (j + 1) * 128], vT[:, tl], C["ident_b"]), reads=[BvT, C["Bb"]], writes=[LTb], inc=(j == 3))
                    bc4 = lambda t_: t_[:, gs_, h:h + 1].to_broadcast([128, 4, 128])
                    S.op("dve", lambda e: e.tensor_tensor(kbg[:, gs_, :], v2(pTb[:, 0:512]), bc4(sc_kbg), ALU.mult), reads=[Bg], writes=[Bkbg, LTb])
                    S.op("dve", lambda e: e.tensor_tensor(kdec[:, gs_, :], v2(pTb[:, 0:512]), bc4(sc_kdec), ALU.mult), reads=[Bg], writes=[Bkdec, LTb])
                    S.op("dve", lambda e: e.tensor_tensor(vb[:, gs_, :], v2(pTb[:, 512:1024]), bc4(beta_tm), ALU.mult), reads=[Bg], writes=[Bvb, LTb])

                def chain(tt, c):
                    ch = CH[c]
                    dm, Bdm = ch["dm"]; dms, Bdms = ch["dms"]; RT, BRT = ch["RT"]; Ab, BAb = ch["A"]
                    o = c * 128
                    os_ = slice(o, o + 128)
                    tl = slice(tt * 128, (tt + 1) * 128)
                    R32 = lambda ap: ap.bitcast(F32R)
                    S.op("pe", lambda e: e.matmul(pKK[:, os_], kT[:, tl], kT[:, tl], start=True, stop=True), reads=[BkT], writes=[LKK])
                    S.op("pe", lambda e: e.matmul(pQK[:, os_], qT[:, tl], kT[:, tl], start=True, stop=True), reads=[BqT, BkT], writes=[LQK])
                    S.op("dve", lambda e: e.scalar_tensor_tensor(dms[:], gcrow[:, os_], gc_tm[:, tt, h:h + 1], mask_incl, ALU.subtract, ALU.subtract),
                         reads=[Bgcrow, Bg, C["Bf"]], writes=[Bdms])
                    yield
                    S.op("act", lambda e: e.activation(dm[:], dms[:], AF.Exp, scale=-1.0), reads=[Bdms], writes=[Bdm])
                    yield
                    m0, Bm0 = ch["M"][0]
                    n0, Bn0 = ch["N"][0]
                    S.op("dve", lambda e: e.tensor_tensor(dms[:], dm[:], strict01, ALU.mult), reads=[Bdm, C["Bf"]], writes=[Bdms])
                    S.op("dve", lambda e: e.scalar_tensor_tensor(R32(m0[:]), pKK[:, os_], nbeta[:, tt, h:h + 1], dms[:], ALU.mult, ALU.mult),
                         reads=[Bg, Bdms], writes=[Bm0, LKK])
                    S.op("dve", lambda e: e.tensor_tensor(Ab[:], pQK[:, os_], dm[:], ALU.mult), reads=[Bdm], writes=[BAb, LQK])
                    yield
                    S.op("pe", lambda e: e.transpose(pTr[:, os_], m0[:], C["ident_f"]), reads=[Bm0, C["Bf"]], writes=[LTr])
                    S.op("pe", lambda e: e.transpose(pTb[:, os_], Ab[:], C["ident_b"]), reads=[BAb, C["Bb"]], writes=[LTb])
                    yield
                    S.op("act", lambda e: e.copy(R32(n0[:]), pTr[:, os_]), reads=[], writes=[Bn0, LTr])
                    S.op("act", lambda e: e.copy(AT[:, tt, :], pTb[:, os_]), reads=[], writes=[BAT, LTb])
                    yield
                    S.op("dve", lambda e: e.tensor_tensor(R32(RT[:]), n0[:], C["ident_f"], ALU.add), reads=[Bn0, C["Bf"]], writes=[BRT])
                    S.op("pe", lambda e: e.matmul(pM[:, os_], R32(n0[:]), R32(m0[:]), start=True, stop=True), reads=[Bn0, Bm0], writes=[LM])
                    S.op("pe", lambda e: e.matmul(pN[:, os_], R32(m0[:]), R32(n0[:]), start=True, stop=True), reads=[Bn0, Bm0], writes=[LN])
                    yield
                    for lvl in range(1, 6):
                        mn, Bmn = ch["M"][lvl % 2]
                        nn, Bnn = ch["N"][lvl % 2]
                        if lvl > 1:
                            S.op("dve", lambda e: e.tensor_tensor(R32(RT[:]), RT[:], pR[:, os_], ALU.add), reads=[BRT], writes=[BRT, LR])
                        S.op("act", lambda e: e.copy(R32(mn[:]), pM[:, os_]), reads=[], writes=[Bmn, LM])
                        if lvl < 5:
                            if c % 2 == 0:
                                S.op("dve", lambda e: e.tensor_copy(R32(nn[:]), pN[:, os_]), reads=[], writes=[Bnn, LN])
                            else:
                                S.op("act", lambda e: e.copy(R32(nn[:]), pN[:, os_]), reads=[], writes=[Bnn, LN])
                        yield
                        S.op("pe", lambda e: e.matmul(pR[:, os_], R32(mn[:]), R32(RT[:]), start=True, stop=True), reads=[Bmn, BRT], writes=[LR])
                        if lvl < 5:
                            S.op("pe", lambda e: e.matmul(pM[:, os_], R32(nn[:]), R32(mn[:]), start=True, stop=True), reads=[Bnn, Bmn], writes=[LM])
                        if lvl < 4:
                            S.op("pe", lambda e: e.matmul(pN[:, os_], R32(mn[:]), R32(nn[:]), start=True, stop=True), reads=[Bnn, Bmn], writes=[LN])
                        yield
                    S.op("dve", lambda e: e.tensor_tensor(R32(RT[:]), RT[:], pR[:, os_], ALU.add), reads=[BRT], writes=[BRT, LR])
                    yield
                    S.op("act", lambda e: e.copy(TT[:, tt, :], RT[:]), reads=[BRT], writes=[BTT])
                    yield
                    S.op("pe", lambda e: e.matmul(pR[:, os_], kbg[:, tt, :], TT[:, tt, :], start=True, stop=True), reads=[Bkbg, BTT], writes=[LR])
                    yield
                    S.op("dve", lambda e: e.tensor_scalar(negw[:, tl], pR[:, os_], -1.0, None, ALU.mult), reads=[], writes=[Bnw, LR])

                for rnd in range(4):
                    S.op("pe", lambda e: e.matmul(pDi[:], selh(h), gcT[:, rnd * 512:(rnd + 1) * 512], start=True, stop=True), reads=[Bg, C["Bf"]], writes=[LDi])
                    S.op("act", lambda e: e.copy(gcrow[:], pDi[:]), reads=[], writes=[Bgcrow, LDi])
                    run_chains([chain(rnd * 4 + c, c) for c in range(4)], stagger=1)

            def scan_head(h, ph, i):
                qd, Bqd = ph["qd"]; negw, Bnw = ph["negw"]; kdec, Bkdec = ph["kdec"]; vb, Bvb = ph["vb"]; TT, BTT = ph["TT"]; AT, BAT = ph["AT"]
                oT, BoT = ph["oT"]; Sst, BS = ph["S"]
                pE, LE, pF, LF, pG_, LG = pKK, LKK, pQK, LQK, pDi, LDi
                S.op("dve", lambda e: e.memset(Sst[:], 0.0), writes=[BS])
                S.op("dve", lambda e: e.memset(ph["Sbf"][0][0][:], 0.0), writes=[ph["Sbf"][0][1]])
                pv = pE[:, i * 128:(i + 1) * 128]
                ps_ = pF[:, i * 128:(i + 1) * 128]
                po = pG_[:, i * 64:(i + 1) * 64]
                for c in range(32):
                    tt, half = c // 2, c % 2
                    gsl = slice(c * 64, (c + 1) * 64)
                    msz = 64 if half == 0 else 128
                    rows = slice(0, 64) if half == 0 else slice(64, 128)
                    sb_, Bsb = ph["Sbf"][c % 2]
                    sbn, Bsbn = ph["Sbf"][(c + 1) % 2]
                    vn, Bvn = ph["vn"][tt % 2]
                    S.op("pe", lambda e: e.matmul(pv[0:msz, :], TT[:, tt, 0:msz], vb[:, tt, :], start=True, stop=False), reads=[BTT, Bvb], writes=[LE], inc=False)
                    S.op("pe", lambda e: e.matmul(pv[0:msz, :], negw[:, tt * 128: tt * 128 + msz], sb_[:], start=False, stop=True), reads=[Bnw, Bsb], writes=[LE])
                    yield
                    S.op("act", lambda e: e.copy(vn[rows, :], pv[rows, :]), reads=[], writes=[Bvn, LE])
                    yield
                    S.op("pe", lambda e: e.matmul(ps_, kdec[rows, tt, :], vn[rows, :], start=True, stop=True), reads=[Bkdec, Bvn], writes=[LF])
                    S.op("pe", lambda e: e.matmul(po, sb_[:], qd[:, gsl], start=True, stop=False), reads=[Bsb, Bqd], writes=[LG], inc=False)
                    S.op("pe", lambda e: e.matmul(po, vn[:], AT[:, tt, half * 64:(half + 1) * 64], start=False, stop=True), reads=[Bvn, BAT], writes=[LG])
                    yield
                    S.op("dve", lambda e: e.scalar_tensor_tensor(sbn[:], Sst[:], egl[:, h, c:c + 1], ps_, ALU.mult, ALU.add), reads=[BS, Bg], writes=[Bsbn, LF])
                    S.op("dve", lambda e: e.scalar_tensor_tensor(Sst[:], Sst[:], egl[:, h, c:c + 1], ps_, ALU.mult, ALU.add), reads=[BS, Bg], writes=[BS, LF])
                    S.op("act", lambda e: e.copy(oT[:, gsl], po), reads=[], writes=[BoT, LG])
                    yield

            def finish_head(h, ph):
                oT, BoT = ph["oT"]
                for tc in range(4):
                    tsl = slice(tc * 512, (tc + 1) * 512)
                    sq, Bsq = tmp["sq"][tc % 2]
                    rs, Brs = tmp["rs"]
                    zt, Bzt = zc[tc % 2]
                    go, Bgo_ = goc[tc % 2]
                    S.dma("sp", zt[:], z_v[h][:, tsl], reads=[Bz], writes=[Bzt])
                    S.op("act", lambda e: e.activation(zt[:], zt[:], AF.Silu), reads=[Bzt], writes=[Bzt])
                    S.op("act", lambda e: e.activation(sq[:], oT[:, tsl], AF.Square), reads=[BoT], writes=[Bsq])
                    S.op("pe", lambda e: e.matmul(pTr[:], C["ones_f"], sq[:], start=True, stop=True), reads=[Bsq, C["Bf"]], writes=[LTr])
                    S.op("act", lambda e: e.activation(rs[:], pTr[:], AF.Sqrt, bias=tmp["eps"][:, 0:1], scale=1.0 / 128), reads=[tmp["Beps"]], writes=[Brs, LTr])
                    S.op("dve", lambda e: e.reciprocal(rs[:], rs[:]), reads=[Brs], writes=[Brs])
                    S.op("dve", lambda e: e.scalar_tensor_tensor(oT[:, tsl], oT[:, tsl], gnw[:, 0:1], rs[:], ALU.mult, ALU.mult), reads=[BoT, Bgn, Brs], writes=[BoT])
                    S.op("dve", lambda e: e.tensor_tensor(go[:], oT[:, tsl], zt[:], ALU.mult), reads=[BoT, Bzt], writes=[Bgo_])
                    S.dma("sp", go_v[h][:, tsl], go[:], reads=[Bgo_], writes=[Bgo])

            for hg in range(NH // HG):
                for i in range(HG):
                    pre_head(hg * HG + i, PH[i])
                run_chains([scan_head(hg * HG + i, PH[i], i) for i in range(HG)], stagger=1)
                for i in range(HG):
                    finish_head(hg * HG + i, PH[i])
            S.barrier()
    out_proj(K, goT, Bgo, hin, Bhin, hout, Bhout, W["wo"], Bw)


def _consts_np():
    c = np.zeros((128, 1024), np.float32)
    c[:, 0:128] = np.eye(128)
    c[:, 128:256] = 1.0
    p = np.arange(128)[:, None]
    q = np.arange(128)[None, :]
    c[:, 256:384] = np.where(q <= p, 0.0, -1e9)
    invf = (np.float32(10000.0) ** (-np.arange(0, 64, 2, dtype=np.float32) / np.float32(64))).astype(np.float32)
    c[0:64, 384] = np.concatenate([invf, invf])
    c[0:32, 385] = -1.0
    c[32:64, 385] = 1.0
    c[:, 512:640] = (p < q).astype(np.float32)
    same = (p // 64) == (q // 64)
    c[:, 640:768] = np.where(same & (q <= p), 0.0, NEG)
    c[:, 768:896] = (same & (q < p)).astype(np.float32)
    c[0:16, 896:912] = -np.eye(16)
    return c


def _pk(v):
    return np.ascontiguousarray(np.asarray(v, np.float32).reshape(-1, 128).T)


EI = "ExternalInput"
_IN_SPECS = [
    ("xT", [D, T], F32), ("posb", [64, T], I32), ("cst", [128, 1024], F32), ("iota", [128, CAP], F32),
    ("lnw_mla", [128, 16], F32), ("win", [D, 1152], F32), ("qnw", [128, 4], F32), ("wqb", [512, 4096], F32), ("kvnw", [128, 4], F32),
    ("wkvb", [512, 4096], F32), ("wo", [D, D], F32),
    ("lnw_ffn", [128, 16], F32), ("fwg", [D, DFF], F32), ("fwu", [D, DFF], F32), ("fwd", [DFF, D], F32),
    ("lnw_gdn", [128, 16], F32), ("gwin", [D, 8224], F32), ("gcw", [128, 48, 4], F32), ("alog", [16, 1], F32), ("dtb", [16, 1], F32),
    ("gnw", [128, 1], F32), ("gwo", [D, D], F32),
    ("lnw_moe", [128, 16], F32), ("rw", [128, 16, 8], F32), ("rb", [128, 8], F32), ("mwg", [NE, D, DFF], F32), ("mwu", [NE, D, DFF], F32),
    ("mwd", [NE, DFF, D], F32), ("fnw", [128, 16], F32),
]


def build_program():
    K = KB()
    A = {}
    for name, shape, dt in _IN_SPECS:
        A[name] = K.dram(name, shape, dt, kind=EI)
    outT = K.dram("outT", [D, T], F32, kind="ExternalOutput")
    h1 = K.dram("h1T", [D, T], F32)
    h2 = K.dram("h2T", [D, T], F32)
    h3 = K.dram("h3T", [D, T], F32)
    Bw = K.dbuf("cst")
    with K.st:
        C = load_consts(K, K.st, A["cst"])
        Wm = {"win": A["win"], "wqb": A["wqb"], "wkvb": A["wkvb"], "wo": A["wo"], "lnw": A["lnw_mla"], "qnw": A["qnw"], "kvnw": A["kvnw"],
              "posb": A["posb"], "oT": K.dram("oT", [D, T], BF16)}
        phase_mla(K, C, A["xT"], K.dbuf("xT"), h1, K.dbuf("h1T"), Wm, Bw)
        phase_ffn(K, C, h1, K.dbuf("h1T"), h2, K.dbuf("h2T"), A["lnw_ffn"], A["fwg"], A["fwu"], A["fwd"], Bw)
        Wg = {"win": A["gwin"], "cw": A["gcw"], "alog": A["alog"], "dtb": A["dtb"], "gnw": A["gnw"], "wo": A["gwo"], "lnw": A["lnw_gdn"],
              "qkvT": K.dram("qkvT", [6144, T], BF16), "zT": K.dram("zT", [D, T], F32), "goT": K.dram("goT", [D, T], BF16)}
        phase_gdn(K, C, h2, K.dbuf("h2T"), h3, K.dbuf("h3T"), Wg, Bw)
        We = {"rw": A["rw"], "rb": A["rb"], "lnw": A["lnw_moe"], "fnw": A["fnw"], "iota": A["iota"], "wg": A["mwg"], "wu": A["mwu"], "wd": A["mwd"],
              "XeT": K.dram("XeT", [NE, D, CAP], BF16), "PTs": K.dram("PTs", [NE * CAP, T], BF16), "Ys": K.dram("Ys", [NE * CAP, D], BF16)}
        phase_moe(K, C, h3, K.dbuf("h3T"), outT, K.dbuf("outT"), We, Bw)
        K.S._wait("sp", [K.dbuf("outT").w])
        K.S.barrier()
    return K


def _host_inputs(inp):
    g = lambda k: np.asarray(inp[k])
    w_in = g("mla_w_in")[0]
    win = np.concatenate([w_in, w_in[:, 1056:1088], w_in[:, 1024:1056]], axis=1)
    wqb = g("mla_w_qb")[0].reshape(512, 16, 192)
    wqb_aug = np.concatenate([wqb, wqb[:, :, 160:192], wqb[:, :, 128:160]], axis=2).reshape(512, 16 * 256)
    shared = {
        "cst": _consts_np(),
        "iota": np.ascontiguousarray(np.broadcast_to(np.arange(CAP, dtype=np.float32)[None, :], (128, CAP))),
        "lnw_mla": _pk(g("ln_mix_mla")[0]), "win": np.ascontiguousarray(win), "qnw": _pk(g("mla_q_norm")[0]), "wqb": np.ascontiguousarray(wqb_aug),
        "kvnw": _pk(g("mla_kv_norm")[0]), "wkvb": np.ascontiguousarray(g("mla_w_kvb")[0]), "wo": np.ascontiguousarray(g("mla_w_o")[0]),
        "lnw_ffn": _pk(g("ln_ffn_dense")[0]), "fwg": np.ascontiguousarray(g("ffn_w_gate")[0]), "fwu": np.ascontiguousarray(g("ffn_w_up")[0]),
        "fwd": np.ascontiguousarray(g("ffn_w_down")[0]),
        "lnw_gdn": _pk(g("ln_mix_gdn")[0]), "gwin": np.ascontiguousarray(g("gdn_w_in")[0]),
        "gcw": np.ascontiguousarray(g("gdn_conv_w")[0].T.reshape(48, 128, 4).transpose(1, 0, 2)),
        "alog": np.ascontiguousarray(g("gdn_a_log")[0].reshape(16, 1)), "dtb": np.ascontiguousarray(g("gdn_dt_bias")[0].reshape(16, 1)),
        "gnw": np.ascontiguousarray(g("gdn_norm")[0].reshape(128, 1)), "gwo": np.ascontiguousarray(g("gdn_w_o")[0]),
        "lnw_moe": _pk(g("ln_ffn_moe")[0]),
        "rw": np.ascontiguousarray(g("moe_router")[0].reshape(16, 128, 8).transpose(1, 0, 2)),
        "rb": np.ascontiguousarray(np.broadcast_to(g("moe_router_bias")[0][None, :], (128, 8))),
        "mwg": np.ascontiguousarray(g("moe_w_gate")[0]), "mwu": np.ascontiguousarray(g("moe_w_up")[0]), "mwd": np.ascontiguousarray(g("moe_w_down")[0]),
        "fnw": _pk(g("final_norm")),
    }
    shared = {k: (v if v.dtype != np.float64 else v.astype(np.float32)) for k, v in shared.items()}
    x = g("x")
    pos = g("positions")
    maps = []
    for b in range(x.shape[0]):
        m = dict(shared)
        m["xT"] = np.ascontiguousarray(x[b].T)
        m["posb"] = np.ascontiguousarray(np.broadcast_to(pos[b][None, :], (64, T))).astype(np.int32)
        maps.append(m)
    return maps


def kernel(**inputs):
    maps = _host_inputs(inputs)
    K = build_program()
    res = run_bass_kernel_spmd(K.nc, maps, core_ids=list(range(len(maps))))
    out = np.stack([np.ascontiguousarray(r["outT"].T) for r in res.results], axis=0)
    return out.astype(np.float32)
```

```python
import numpy as np
from contextlib import ExitStack
import concourse.bass as bass
import concourse.mybir as mybir
from concourse.bass_utils import run_bass_kernel_spmd

F32 = mybir.dt.float32
BF16 = mybir.dt.bfloat16
F32R = mybir.dt.float32r
AF = mybir.ActivationFunctionType
ALU = mybir.AluOpType
AX = mybir.AxisListType

D = 2048
T = 2048
KT = 16
DFF = 7168
EPS = 1e-6


class Buf:
    __slots__ = ("w", "r")

    def __init__(self):
        self.w = None
        self.r = {}


class Sched:
    NDS = 8

    def __init__(self, nc, stack):
        self.nc = nc
        self.engs = {"pe": nc.tensor, "act": nc.scalar, "dve": nc.vector,
                     "pool": nc.gpsimd, "sp": nc.sync}
        self.sem = {k: stack.enter_context(nc.semaphore("s_" + k)) for k in self.engs}
        self.cnt = {k: 0 for k in self.engs}
        self.known = {k: {} for k in self.engs}
        self.dsem, self.dval, self.dnext = {}, {}, {}
        for q in ("sp", "pool"):
            self.dsem[q] = [stack.enter_context(nc.semaphore("d_%s%d" % (q, i))) for i in range(self.NDS)]
            self.dval[q] = [0] * self.NDS
            self.dnext[q] = 0
        self.n_ins = 0
        self.n_wait = 0

    def _semh(self, key):
        if key[0] == "E":
            return self.sem[key[1]]
        return self.dsem[key[1]][key[2]]

    def _wait(self, eng, toks, defer=False):
        need = {}
        for (key, val) in toks:
            if val > need.get(key, 0):
                need[key] = val
        kn = self.known[eng]
        lst = [(key, val) for key, val in need.items() if kn.get(key, 0) < val]
        pend = None
        if defer and lst:
            pend = lst.pop()
        for key, val in lst:
            self.engs[eng].wait_ge(self._semh(key), val)
            self.n_wait += 1
            kn[key] = val
        if pend is not None:
            kn[pend[0]] = pend[1]
        return pend

    def _deps(self, eng, reads, writes):
        toks = []
        own = ("E", eng)
        for b in reads:
            if b.w is not None:
                if b.w[0] == own and eng == "pe":
                    continue
                toks.append(b.w)
        for b in writes:
            if b.w is not None and b.w[0] != own:
                toks.append(b.w)
            for key, val in b.r.items():
                if key != own:
                    toks.append((key, val))
        return toks

    def _mark(self, tok, reads, writes):
        key, val = tok
        for b in reads:
            b.r[key] = val
        for b in writes:
            b.w = tok
            b.r = {}

    def op(self, eng, fn, reads=(), writes=(), inc=True):
        pend = self._wait(eng, self._deps(eng, reads, writes), defer=True)
        ins = fn(self.engs[eng])
        if pend is not None:
            ins._wait_ge(self._semh(pend[0]), pend[1])
        self.n_ins += 1
        if not inc:
            self._mark((("E", eng), self.cnt[eng] + 1), reads, writes)
            return ins
        self.cnt[eng] += 1
        ins.then_inc(self.sem[eng], 1)
        self._mark((("E", eng), self.cnt[eng]), reads, writes)
        return ins

    def dma(self, q, out, in_, reads=(), writes=(), **kw):
        self._wait(q, self._deps(q, reads, writes))
        i = self.dnext[q]
        self.dnext[q] = (i + 1) % self.NDS
        key = ("D", q, i)
        if self.dval[q][i] > 0:
            self._wait(q, [(key, self.dval[q][i])])
        self.dval[q][i] += 16
        self.engs[q].dma_start(out=out, in_=in_, **kw).then_inc(self.dsem[q][i], 16)
        self.n_ins += 1
        tok = (key, self.dval[q][i])
        self._mark(tok, reads, writes)
        return tok

    def barrier(self):
        toks = [(("E", k), v) for k, v in self.cnt.items() if v > 0]
        for q in self.dsem:
            for i in range(self.NDS):
                if self.dval[q][i] > 0:
                    toks.append((("D", q, i), self.dval[q][i]))
        for e in self.engs:
            self._wait(e, [t for t in toks if t[0] != ("E", e)])


class KB:
    def __init__(self):
        self.nc = bass.Bass("TRN2", target_bir_lowering=False)
        self.st = ExitStack()
        self.S = Sched(self.nc, self.st)
        self.uid = 0
        self.dram_bufs = {}

    def name(self, p="t"):
        self.uid += 1
        return "%s%d" % (p, self.uid)

    def sb(self, stack, shape, dt):
        return stack.enter_context(self.nc.sbuf_tensor(self.name("sb"), list(shape), dt))

    def ps(self, stack, shape=(128, 512), dt=F32):
        return stack.enter_context(self.nc.psum_tensor(self.name("ps"), list(shape), dt))

    def dram(self, name, shape, dt, kind="Internal"):
        t = self.nc.dram_tensor(name, list(shape), dt, kind=kind).ap()
        self.dram_bufs[name] = Buf()
        return t

    def dbuf(self, name):
        return self.dram_bufs[name]


def load_consts(K, stack, cst_dram):
    S = K.S
    c = {}
    cf = K.sb(stack, [128, 1024], F32)
    B = Buf()
    S.dma("sp", cf[:], cst_dram, reads=[K.dbuf("cst")], writes=[B])
    cb = K.sb(stack, [128, 1024], BF16)
    Bb = Buf()
    S.op("dve", lambda e: e.tensor_copy(cb[:], cf[:]), reads=[B], writes=[Bb])
    c["f"], c["b"], c["Bf"], c["Bb"] = cf, cb, B, Bb
    c["ident_f"] = cf[:, 0:128]
    c["ones_f"] = cf[:, 128:256]
    c["ident_b"] = cb[:, 0:128]
    c["ones_b"] = cb[:, 128:256]
    c["U_b"] = cb[:, 512:640]
    return c


def norm_T(K, C, src, Bsrc, dst, Bdst, lnw, Blnw, ntok, tmp, ps_bank, Bps, dst32=None, nk=KT, dim=D, dcol=0):
    S = K.S
    for c0 in range(0, ntok, 512):
        n = min(512, ntok - c0)
        for k in range(nk):
            sq, Bsq = tmp["sq"][k % 2]
            S.op("act", lambda e: e.activation(sq[:, :n], src[:, k, c0:c0 + n], AF.Square), reads=[Bsrc[k]], writes=[Bsq])
            S.op("pe", lambda e: e.matmul(ps_bank[:, :n], C["ones_f"], sq[:, :n], start=(k == 0), stop=(k == nk - 1)),
                 reads=[Bsq, C["Bf"]], writes=[Bps], inc=True)
        rs, Brs = tmp["rs"]
        S.op("act", lambda e: e.activation(rs[:, :n], ps_bank[:, :n], AF.Ln, bias=tmp["eps"][:, 0:1], scale=1.0 / dim), reads=[Bps, tmp["Beps"]], writes=[Brs])
        S.op("act", lambda e: e.activation(rs[:, :n], rs[:, :n], AF.Exp, scale=-0.5), reads=[Brs], writes=[Brs])
        for k in range(nk):
            if dst32 is None:
                S.op("dve", lambda e: e.scalar_tensor_tensor(dst[:, k, dcol + c0:dcol + c0 + n], src[:, k, c0:c0 + n], lnw[:, k:k + 1], rs[:, :n], ALU.mult, ALU.mult),
                     reads=[Bsrc[k], Blnw, Brs], writes=[Bdst])
            else:
                d32, Bd32 = dst32
                S.op("dve", lambda e: e.scalar_tensor_tensor(d32[:, k, c0:c0 + n], src[:, k, c0:c0 + n], lnw[:, k:k + 1], rs[:, :n], ALU.mult, ALU.mult),
                     reads=[Bsrc[k], Blnw, Brs], writes=[Bd32])
                S.op("pool", lambda e: e.tensor_copy(dst[:, k, dcol + c0:dcol + c0 + n], d32[:, k, c0:c0 + n]), reads=[Bd32], writes=[Bdst])


def norm_tmp(K, stack):
    S = K.S
    tmp = {"sq": [], "rs": None}
    for i in range(2):
        tmp["sq"].append((K.sb(stack, [128, 512], F32), Buf()))
    tmp["rs"] = (K.sb(stack, [128, 512], F32), Buf())
    eps = K.sb(stack, [128, 1], F32)
    tmp["eps"] = eps
    tmp["Beps"] = Buf()
    S.op("dve", lambda e: e.memset(eps[:], EPS), writes=[tmp["Beps"]])
    return tmp


def phase_ffn(K, C, hin, Bhin, hout, Bhout, lnw_d, wg, wu, wd, Bw):
    S = K.S
    TH = 1024
    FC = 256
    NFC = DFF // FC
    NFT = FC // 128
    hin_v = hin.rearrange("(k p) t -> p k t", p=128)
    hout_v = hout.rearrange("(k p) t -> p k t", p=128)
    wg_v = wg.rearrange("(k p) f -> p k f", p=128)
    wu_v = wu.rearrange("(k p) f -> p k f", p=128)
    wd_v = wd.rearrange("(j p) d -> p j d", p=128)
    with ExitStack() as st:
        xn = K.sb(st, [128, KT, TH], BF16); Bxn = Buf()
        yacc = K.sb(st, [128, KT, TH], F32); By = [Buf() for _ in range(KT)]
        lnw = K.sb(st, [128, KT], F32); Blnw = Buf()
        S.dma("sp", lnw[:], lnw_d, reads=[Bw], writes=[Blnw])
        tmp = norm_tmp(K, st)
        wgs = [(K.sb(st, [128, KT, FC], BF16), Buf()) for _ in range(2)]
        wus = [(K.sb(st, [128, KT, FC], BF16), Buf()) for _ in range(2)]
        wds = [(K.sb(st, [128, NFT, D], BF16), Buf()) for _ in range(2)]
        hts = [(K.sb(st, [128, NFT, TH], BF16), Buf()) for _ in range(2)]
        sgs = [(K.sb(st, [128, 512], F32), Buf()) for _ in range(2)]
        pgs = [(K.ps(st), Buf()) for _ in range(2)]
        pus = [(K.ps(st), Buf()) for _ in range(2)]
        pos = [(K.ps(st), Buf()) for _ in range(2)]
        pn = (K.ps(st), Buf())

        def load_gu(fc):
            s = fc % 2
            S.dma("pool", wgs[s][0][:], wg_v[:, :, fc * FC:(fc + 1) * FC], reads=[Bw], writes=[wgs[s][1]])
            S.dma("pool", wus[s][0][:], wu_v[:, :, fc * FC:(fc + 1) * FC], reads=[Bw], writes=[wus[s][1]])

        def load_d(fc):
            s = fc % 2
            S.dma("pool", wds[s][0][:], wd_v[:, fc * NFT:(fc + 1) * NFT, :], reads=[Bw], writes=[wds[s][1]])

        cnt = [0]

        def gateup(fc):
            s = fc % 2
            wgt, Bwg = wgs[s]
            wut, Bwu = wus[s]
            ht, Bht = hts[s]
            for ft in range(NFT):
                for tc in range(TH // 512):
                    i = cnt[0] % 2
                    cnt[0] += 1
                    pg, Bpg = pgs[i]
                    pu, Bpu = pus[i]
                    sg, Bsg = sgs[i]
                    tsl = slice(tc * 512, (tc + 1) * 512)
                    for k in range(KT):
                        S.op("pe", lambda e: e.matmul(pg[:], wgt[:, k, ft * 128:(ft + 1) * 128], xn[:, k, tsl], start=(k == 0), stop=(k == KT - 1)),
                             reads=[Bwg, Bxn], writes=[Bpg], inc=(k == KT - 1))
                    for k in range(KT):
                        S.op("pe", lambda e: e.matmul(pu[:], wut[:, k, ft * 128:(ft + 1) * 128], xn[:, k, tsl], start=(k == 0), stop=(k == KT - 1)),
                             reads=[Bwu, Bxn], writes=[Bpu], inc=(k == KT - 1))
                    S.op("act", lambda e: e.activation(sg[:], pg[:], AF.Silu), reads=[Bpg], writes=[Bsg])
                    S.op("dve", lambda e: e.tensor_tensor(ht[:, ft, tsl], sg[:], pu[:], ALU.mult), reads=[Bsg, Bpu], writes=[Bht])

        ocnt = [0]

        def down(fc):
            s = fc % 2
            wdt, Bwd = wds[s]
            ht, Bht = hts[s]
            for dt in range(KT):
                for tc in range(TH // 512):
                    i = ocnt[0] % 2
                    ocnt[0] += 1
                    po, Bpo = pos[i]
                    tsl = slice(tc * 512, (tc + 1) * 512)
                    for ft in range(NFT):
                        S.op("pe", lambda e: e.matmul(po[:], wdt[:, ft, dt * 128:(dt + 1) * 128], ht[:, ft, tsl], start=(ft == 0), stop=(ft == NFT - 1)),
                             reads=[Bwd, Bht], writes=[Bpo], inc=(ft == NFT - 1))
                    S.op("dve", lambda e: e.tensor_tensor(yacc[:, dt, tsl], yacc[:, dt, tsl], po[:], ALU.add), reads=[Bpo, By[dt]], writes=[By[dt]])

        for half in range(T // TH):
            hs = slice(half * TH, (half + 1) * TH)
            S.dma("sp", yacc[:], hin_v[:, :, hs], reads=[Bhin], writes=By)
            load_gu(0)
            load_d(0)
            norm_T(K, C, yacc, By, xn, Bxn, lnw, Blnw, TH, tmp, pn[0], pn[1])
            for fc in range(NFC + 1):
                if fc + 1 < NFC:
                    load_gu(fc + 1)
                if fc < NFC:
                    gateup(fc)
                if fc >= 1:
                    down(fc - 1)
                if fc + 1 < NFC:
                    load_d(fc + 1)
            S.dma("sp", hout_v[:, :, hs], yacc[:], reads=By, writes=[Bhout])
    S.barrier()


I32 = mybir.dt.int32
NH = 16
TWO_PI = float(2 * np.pi)
CW1 = 6.28125
CW2 = float(2 * np.pi - 6.28125)


def rope_tables(K, C, st0, posb, Bpos):
    S = K.S
    cos2 = K.sb(st0, [64, T], F32)
    sin2 = K.sb(st0, [64, T], F32)
    Brope = Buf()
    invf = C["f"][0:64, 384:385]
    sign = C["f"][0:64, 385:386]
    with ExitStack() as st:
        pi_ = K.sb(st, [64, T], I32); Bpi = Buf()
        ang = K.sb(st, [64, T], F32); Bang = Buf()
        a = K.sb(st, [64, T], F32); Ba = Buf()
        ki = K.sb(st, [64, T], I32); Bki = Buf()
        kf = K.sb(st, [64, T], F32); Bkf = Buf()
        m = K.sb(st, [64, T], F32); Bm = Buf()
        negpi = None
        S.dma("sp", pi_[:], posb, reads=[Bpos], writes=[Bpi])
        S.op("dve", lambda e: e.tensor_copy(a[:], pi_[:]), reads=[Bpi], writes=[Ba])
        S.op("dve", lambda e: e.tensor_scalar(ang[:], a[:], invf, None, ALU.mult), reads=[Ba, C["Bf"]], writes=[Bang])
        for dst, shift in ((sin2, 0.0), (cos2, float(np.pi / 2))):
            S.op("dve", lambda e: e.tensor_scalar(a[:], ang[:], shift, None, ALU.add), reads=[Bang], writes=[Ba])
            S.op("dve", lambda e: e.tensor_scalar(ki[:], a[:], 1.0 / TWO_PI, None, ALU.mult), reads=[Ba], writes=[Bki])
            S.op("dve", lambda e: e.tensor_copy(kf[:], ki[:]), reads=[Bki], writes=[Bkf])
            S.op("dve", lambda e: e.scalar_tensor_tensor(a[:], kf[:], -CW1, a[:], ALU.mult, ALU.add), reads=[Bkf, Ba], writes=[Ba])
            S.op("dve", lambda e: e.scalar_tensor_tensor(a[:], kf[:], -CW2, a[:], ALU.mult, ALU.add), reads=[Bkf, Ba], writes=[Ba])
            S.op("dve", lambda e: e.tensor_scalar(m[:], a[:], float(np.pi), -TWO_PI, ALU.is_gt, ALU.mult), reads=[Ba], writes=[Bm])
            S.op("dve", lambda e: e.tensor_tensor(a[:], a[:], m[:], ALU.add), reads=[Ba, Bm], writes=[Ba])
            S.op("dve", lambda e: e.tensor_scalar(m[:], a[:], -float(np.pi), TWO_PI, ALU.is_lt, ALU.mult), reads=[Ba], writes=[Bm])
            S.op("dve", lambda e: e.tensor_tensor(a[:], a[:], m[:], ALU.add), reads=[Ba, Bm], writes=[Ba])
            S.op("act", lambda e: e.activation(dst[:], a[:], AF.Sin), reads=[Ba], writes=[Brope])
        S.op("dve", lambda e: e.tensor_scalar(sin2[:], sin2[:], sign, None, ALU.mult), reads=[Brope, C["Bf"]], writes=[Brope])
        S.barrier()
    return cos2, sin2, Brope


def phase_mla(K, C, hin, Bhin, hout, Bhout, W, Bw):
    S = K.S
    scale = float(192 ** -0.5)
    hin_v = hin.rearrange("(k p) t -> p k t", p=128)
    hout_v = hout.rearrange("(k p) t -> p k t", p=128)
    oT = W["oT"]
    BoT = K.dbuf("oT")
    oT_v = oT.rearrange("(h p) t -> p h t", p=128)
    win_v = W["win"].rearrange("(k p) f -> p k f", p=128)
    wqb_v = W["wqb"].rearrange("(k p) f -> p k f", p=128)
    wkvb_v = W["wkvb"].rearrange("(k p) f -> p k f", p=128)
    wo_v = W["wo"].rearrange("(k p) f -> p k f", p=128)
    maskb = C["f"][:, 256:384]
    with ExitStack() as st0:
        cqn = K.sb(st0, [128, 4, T], BF16); Bcqn = Buf()
        ckvn = K.sb(st0, [128, 4, T], BF16); Bckvn = Buf()
        kr = K.sb(st0, [64, T], BF16); Bkr = Buf()
        cos2, sin2, Brope = rope_tables(K, C, st0, W["posb"], Bw)

        with ExitStack() as st:
            win = K.sb(st, [128, KT, 1152], BF16); Bwin = Buf()
            for k0 in range(0, KT, 4):
                S.dma("pool", win[:, k0:k0 + 4, :], win_v[:, k0:k0 + 4, :], reads=[Bw], writes=[Bwin])
            lnw = K.sb(st, [128, KT], F32); Blnw = Buf()
            qnw = K.sb(st, [128, 4], F32); kvnw = K.sb(st, [128, 4], F32)
            S.dma("sp", lnw[:], W["lnw"], reads=[Bw], writes=[Blnw])
            S.dma("sp", qnw[:], W["qnw"], reads=[Bw], writes=[Blnw])
            S.dma("sp", kvnw[:], W["kvnw"], reads=[Bw], writes=[Blnw])
            tmp = norm_tmp(K, st)
            hch = K.sb(st, [128, KT, 512], F32); Bh = [Buf() for _ in range(KT)]
            xn = K.sb(st, [128, KT, 512], BF16); Bxn = Buf()
            cq32 = K.sb(st, [128, 4, 512], F32); Bcq = [Buf() for _ in range(4)]
            ckv32 = K.sb(st, [128, 4, 512], F32); Bckv = [Buf() for _ in range(4)]
            t1 = K.sb(st, [64, 512], F32); Bt1 = Buf()
            t2 = K.sb(st, [64, 512], F32); Bt2 = Buf()
            pn = (K.ps(st), Buf())
            pp = [(K.ps(st), Buf()) for _ in range(3)]
            pc = [0]

            def proj(col0, m, evac):
                p, Bp = pp[pc[0] % 3]
                pc[0] += 1
                for k in range(KT):
                    S.op("pe", lambda e: e.matmul(p[0:m, :], win[:, k, col0:col0 + m], xn[:, k, :], start=(k == 0), stop=(k == KT - 1)),
                         reads=[Bwin, Bxn], writes=[Bp], inc=(k == KT - 1))
                evac(p, Bp)

            for tc in range(T // 512):
                tsl = slice(tc * 512, (tc + 1) * 512)
                S.dma("sp", hch[:], hin_v[:, :, tsl], reads=[Bhin], writes=Bh)
                norm_T(K, C, hch, Bh, xn, Bxn, lnw, Blnw, 512, tmp, pn[0], pn[1])
                for m in range(4):
                    proj(m * 128, 128, lambda p, Bp: S.op("act", lambda e: e.copy(cq32[:, m, :], p[:]), reads=[Bp], writes=[Bcq[m]]))
                for m in range(4):
                    proj(512 + m * 128, 128, lambda p, Bp: S.op("act", lambda e: e.copy(ckv32[:, m, :], p[:]), reads=[Bp], writes=[Bckv[m]]))
                proj(1024, 64, lambda p, Bp: S.op("dve", lambda e: e.tensor_tensor(t1[:], p[0:64, :], cos2[:, tsl], ALU.mult), reads=[Bp, Brope], writes=[Bt1]))
                proj(1088, 64, lambda p, Bp: S.op("dve", lambda e: e.tensor_tensor(t2[:], p[0:64, :], sin2[:, tsl], ALU.mult), reads=[Bp, Brope], writes=[Bt2]))
                S.op("dve", lambda e: e.tensor_tensor(kr[:, tsl], t1[:], t2[:], ALU.add), reads=[Bt1, Bt2], writes=[Bkr])
                norm_T(K, C, cq32, Bcq, cqn, Bcqn, qnw, Blnw, 512, tmp, pn[0], pn[1], nk=4, dim=512, dcol=tc * 512)
                norm_T(K, C, ckv32, Bckv, ckvn, Bckvn, kvnw, Blnw, 512, tmp, pn[0], pn[1], nk=4, dim=512, dcol=tc * 512)
            S.barrier()

        with ExitStack() as st:
            wqb = K.sb(st, [128, 4, 4096], BF16); Bwqb = Buf()
            wkvb = K.sb(st, [128, 4, 4096], BF16); Bwkvb = Buf()
            for k in range(4):
                S.dma("pool", wqb[:, k, :], wqb_v[:, k, :], reads=[Bw], writes=[Bwqb])
                S.dma("pool", wkvb[:, k, :], wkvb_v[:, k, :], reads=[Bw], writes=[Bwkvb])
            qn = [(K.sb(st, [128, T], BF16), Buf()) for _ in range(2)]
            qr = [(K.sb(st, [64, T], BF16), Buf()) for _ in range(2)]
            kn = [(K.sb(st, [128, T], BF16), Buf()) for _ in range(2)]
            Vt = [(K.sb(st, [128, 16, 128], BF16), Buf()) for _ in range(2)]
            oTh = [(K.sb(st, [128, T], BF16), Buf()) for _ in range(2)]
            Pb = [(K.sb(st, [128, T], BF16), Buf()) for _ in range(2)]
            PTb = [(K.sb(st, [128, 16, 128], BF16), Buf()) for _ in range(2)]
            otm = [(K.sb(st, [128, 128], BF16), Buf()) for _ in range(2)]
            sm = [(K.sb(st, [128, 4], F32), Buf()) for _ in range(4)]
            t1 = K.sb(st, [64, 512], F32); Bt1 = Buf()
            t2 = K.sb(st, [64, 512], F32); Bt2 = Buf()
            Sps = K.ps(st, [128, 2560], F32); Bs = [Buf() for _ in range(5)]
            PTp = K.ps(st, [128, 1024], BF16); LPT = Buf()
            pob = K.ps(st); Lpo = Buf()
            ppj = [(K.ps(st), Buf()) for _ in range(1)]
            bc = [0]

            def run_chains(gens):
                gens = list(gens)
                while gens:
                    for g_ in list(gens):
                        try:
                            next(g_)
                        except StopIteration:
                            gens.remove(g_)

            def both(g1, g2):
                gens = [g1, g2]
                while gens:
                    for g_ in list(gens):
                        try:
                            next(g_)
                        except StopIteration:
                            gens.remove(g_)
                    yield

            def projB(wt, Bwt, col0, m, src, Bsrc_, tsl, evac):
                p, Bp = ppj[0]
                for k in range(4):
                    S.op("pe", lambda e: e.matmul(p[0:m, :], wt[:, k, col0:col0 + m], src[:, k, tsl], start=(k == 0), stop=(k == 3)),
                         reads=[Bwt, Bsrc_], writes=[Bp], inc=(k == 3))
                evac(p, Bp)

            def proj_gen(h):
                s = h % 2
                qnt, Bqn = qn[s]
                qrt, Bqr = qr[s]
                knt, Bkn = kn[s]
                vt, Bv = Vt[s]
                for tc in range(4):
                    tsl = slice(tc * 512, (tc + 1) * 512)
                    projB(wqb, Bwqb, h * 256, 128, cqn, Bcqn, tsl,
                          lambda p, Bp: S.op("act", lambda e: e.copy(qnt[:, tsl], p[:]), reads=[Bp], writes=[Bqn]))
                    yield
                    projB(wqb, Bwqb, h * 256 + 128, 64, cqn, Bcqn, tsl,
                          lambda p, Bp: S.op("dve", lambda e: e.tensor_tensor(t1[:], p[0:64, :], cos2[:, tsl], ALU.mult), reads=[Bp, Brope], writes=[Bt1]))
                    yield
                    projB(wqb, Bwqb, h * 256 + 192, 64, cqn, Bcqn, tsl,
                          lambda p, Bp: S.op("dve", lambda e: e.tensor_tensor(t2[:], p[0:64, :], sin2[:, tsl], ALU.mult), reads=[Bp, Brope], writes=[Bt2]))
                    S.op("dve", lambda e: e.tensor_tensor(qrt[:, tsl], t1[:], t2[:], ALU.add), reads=[Bt1, Bt2], writes=[Bqr])
                    yield
                    projB(wkvb, Bwkvb, h * 256, 128, ckvn, Bckvn, tsl,
                          lambda p, Bp: S.op("act", lambda e: e.copy(knt[:, tsl], p[:]), reads=[Bp], writes=[Bkn]))
                    yield
                for g in range(4):
                    p, Bp = ppj[0]
                    for j in range(4):
                        tt = g * 4 + j
                        for k in range(4):
                            S.op("pe", lambda e: e.matmul(p[:, j * 128:(j + 1) * 128], ckvn[:, k, tt * 128:(tt + 1) * 128],
                                                          wkvb[:, k, h * 256 + 128: h * 256 + 256], start=(k == 0), stop=(k == 3)),
                                 reads=[Bckvn, Bwkvb], writes=[Bp], inc=(k == 3 and j == 3))
                    S.op("act", lambda e: e.copy(vt[:, g * 4:(g + 1) * 4, :], p[:].rearrange("p (a c) -> p a c", a=4)), reads=[Bp], writes=[Bv])
                    yield

            def tile_chain(h, i, lane):
                s = h % 2
                qnt, Bqn = qn[s]
                qrt, Bqr = qr[s]
                knt, Bkn = kn[s]
                vt, Bv = Vt[s]
                oth, Both = oTh[s]
                s1 = (i + 1) * 128
                nb = (s1 + 511) // 512
                b0 = 0 if lane == 0 else 5 - nb
                sb = b0 * 512
                qsl = slice(i * 128, (i + 1) * 128)
                smt, Bsm = sm[lane * 2 + (i % 2)]
                P, BP = Pb[lane]
                PT, BPT = PTb[lane]
                ot, Bot = otm[lane]
                for kc in range(nb):
                    n = min(512, s1 - kc * 512)
                    S.op("pe", lambda e: e.matmul(Sps[:, sb + kc * 512: sb + kc * 512 + n], qnt[:, qsl], knt[:, kc * 512: kc * 512 + n], start=True, stop=False),
                         reads=[Bqn, Bkn], writes=[Bs[b0 + kc]], inc=False)
                    S.op("pe", lambda e: e.matmul(Sps[:, sb + kc * 512: sb + kc * 512 + n], qrt[:, qsl], kr[:, kc * 512: kc * 512 + n], start=False, stop=True),
                         reads=[Bqr, Bkr], writes=[Bs[b0 + kc]], inc=True)
                yield
                bd = b0 + i // 4
                dsl = slice(sb + i * 128, sb + (i + 1) * 128)
                S.op("dve", lambda e: e.tensor_tensor(Sps[:, dsl], Sps[:, dsl], maskb, ALU.add), reads=[Bs[bd], C["Bf"]], writes=[Bs[bd]])
                S.op("dve", lambda e: e.memset(smt[:], 0.0), writes=[Bsm])
                S.op("dve", lambda e: e.tensor_reduce(smt[:, 0:1], Sps[:, sb:sb + s1], AX.X, ALU.max), reads=Bs[b0:b0 + nb], writes=[Bsm])
                S.op("dve", lambda e: e.tensor_scalar(smt[:, 1:2], smt[:, 0:1], -scale, None, ALU.mult), reads=[Bsm], writes=[Bsm])
                yield
                S.op("act", lambda e: e.activation(P[:, 0:s1], Sps[:, sb:sb + s1], AF.Exp, bias=smt[:, 1:2], scale=scale, accum_out=smt[:, 2:3]),
                     reads=Bs[b0:b0 + nb] + [Bsm], writes=[BP, Bsm])
                S.op("dve", lambda e: e.reciprocal(smt[:, 3:4], smt[:, 2:3]), reads=[Bsm], writes=[Bsm])
                yield
                po_ = lane * 512
                for g0 in range(0, i + 1, 4):
                    g1 = min(i + 1, g0 + 4)
                    for kt in range(g0, g1):
                        S.op("pe", lambda e: e.transpose(PTp[:, po_ + (kt - g0) * 128: po_ + (kt - g0 + 1) * 128], P[:, kt * 128:(kt + 1) * 128], C["ident_b"]),
                             reads=[BP, C["Bb"]], writes=[LPT], inc=(kt == g1 - 1))
                    yield
                    src = PTp[:, po_: po_ + (g1 - g0) * 128].rearrange("p (a c) -> p a c", c=128)
                    if (g0 // 4 + lane) % 2 == 0:
                        S.op("act", lambda e: e.copy(PT[:, g0:g1, :], src), reads=[], writes=[BPT, LPT])
                    else:
                        S.op("dve", lambda e: e.tensor_copy(PT[:, g0:g1, :], src), reads=[], writes=[BPT, LPT])
                    yield
                oo = lane * 128
                for kt in range(i + 1):
                    S.op("pe", lambda e: e.matmul(pob[:, oo:oo + 128], PT[:, kt, :], vt[:, kt, :], start=(kt == 0), stop=(kt == i)),
                         reads=[BPT, Bv], writes=[Lpo], inc=(kt == i))
                yield
                S.op("dve", lambda e: e.tensor_scalar(ot[:], pob[:, oo:oo + 128], smt[:, 3:4], None, ALU.mult), reads=[Bsm], writes=[Bot, Lpo])
                yield
                oTv = pob[:, 256 + lane * 64: 320 + lane * 64].bitcast(BF16)
                S.op("pe", lambda e: e.transpose(oTv, ot[:], C["ident_b"]), reads=[Bot, C["Bb"]], writes=[Lpo])
                yield
                S.op("act", lambda e: e.copy(oth[:, qsl], oTv), reads=[], writes=[Both, Lpo])
                yield

            def attn_gen(h):
                for i in range(8):
                    yield from both(tile_chain(h, i, 0), tile_chain(h, 15 - i, 1))
                S.dma("sp", oT_v[:, h, :], oTh[h % 2][0][:], reads=[oTh[h % 2][1]], writes=[BoT])

            run_chains([proj_gen(0)])
            for h in range(NH):
                gens = [attn_gen(h)]
                if h + 1 < NH:
                    gens.append(proj_gen(h + 1))
                run_chains(gens)
            S.barrier()

    with ExitStack() as st:
        wo = K.sb(st, [128, KT, D], BF16); Bwo = Buf()
        for k0 in range(0, KT, 4):
            S.dma("pool", wo[:, k0:k0 + 4, :], wo_v[:, k0:k0 + 4, :], reads=[Bw], writes=[Bwo])
        och = [(K.sb(st, [128, KT, 512], BF16), Buf()) for _ in range(2)]
        hch = [(K.sb(st, [128, KT, 512], F32), [Buf() for _ in range(KT)]) for _ in range(2)]
        pcs = [(K.ps(st), Buf()) for _ in range(4)]
        n = 0
        for tc in range(T // 512):
            tsl = slice(tc * 512, (tc + 1) * 512)
            oc, Boc = och[tc % 2]
            hc, Bhc = hch[tc % 2]
            S.dma("sp", oc[:], oT_v[:, :, tsl], reads=[BoT], writes=[Boc])
            S.dma("sp", hc[:], hin_v[:, :, tsl], reads=[Bhin], writes=Bhc)
            for dt in range(KT):
                p, Bp = pcs[n % 4]
                n += 1
                for k in range(KT):
                    S.op("pe", lambda e: e.matmul(p[:], wo[:, k, dt * 128:(dt + 1) * 128], oc[:, k, :], start=(k == 0), stop=(k == KT - 1)),
                         reads=[Bwo, Boc], writes=[Bp], inc=(k == KT - 1))
                S.op("dve", lambda e: e.tensor_tensor(hc[:, dt, :], hc[:, dt, :], p[:], ALU.add), reads=[Bp, Bhc[dt]], writes=[Bhc[dt]])
            S.dma("sp", hout_v[:, :, tsl], hc[:], reads=Bhc, writes=[Bhout])
    S.barrier()


NE = 8
CAP = 768
CT = CAP // 128
CCH = CAP // 2


def phase_moe(K, C, hin, Bhin, outT, Bout, W, Bw):
    S = K.S
    hin_v = hin.rearrange("(k p) t -> p k t", p=128)
    out_v = outT.rearrange("(k p) t -> p k t", p=128)
    XeT = W["XeT"].rearrange("e (k p) c -> e p k c", p=128)
    PTs = W["PTs"].rearrange("(j p) t -> p j t", p=128)
    Ysd = W["Ys"].rearrange("(j p) d -> p j d", p=128)
    BXe, BPT, BYs = K.dbuf("XeT"), K.dbuf("PTs"), K.dbuf("Ys")
    with ExitStack() as stg:
        gs = K.sb(stg, [128, NE, CT], F32); Bgs = Buf()
        with ExitStack() as st1:
            xn_tm = K.sb(st1, [128, 16, D], BF16); Bxtm = Buf()
            logits = K.sb(st1, [128, 16, NE], F32); Blog = Buf()
            rw = K.sb(st1, [128, KT, NE], F32); Brw = Buf()
            rb = K.sb(st1, [128, NE], F32)
            lnw = K.sb(st1, [128, KT], F32); Blnw = Buf()
            S.dma("sp", rw[:], W["rw"], reads=[Bw], writes=[Brw])
            S.dma("sp", rb[:], W["rb"], reads=[Bw], writes=[Brw])
            S.dma("sp", lnw[:], W["lnw"], reads=[Bw], writes=[Blnw])
            with ExitStack() as st:
                tmp = norm_tmp(K, st)
                hch = K.sb(st, [128, KT, 512], F32); Bh = [Buf() for _ in range(KT)]
                xn = K.sb(st, [128, KT, 512], BF16); Bxn = Buf()
                xn32 = K.sb(st, [128, KT, 512], F32); Bxn32 = Buf()
                pn = (K.ps(st), Buf())
                pl = (K.ps(st), Buf())
                ptp = [(K.ps(st, [128, 1024], BF16), Buf()) for _ in range(2)]
                n = 0
                for tc in range(T // 512):
                    tsl = slice(tc * 512, (tc + 1) * 512)
                    S.dma("sp", hch[:], hin_v[:, :, tsl], reads=[Bhin], writes=Bh)
                    norm_T(K, C, hch, Bh, xn, Bxn, lnw, Blnw, 512, tmp, pn[0], pn[1], dst32=(xn32, Bxn32))
                    for j in range(4):
                        tt = tc * 4 + j
                        for k in range(KT):
                            S.op("pe", lambda e: e.matmul(pl[0][:, j * 8:(j + 1) * 8], xn32[:, k, j * 128:(j + 1) * 128], rw[:, k, :], start=(k == 0), stop=(k == KT - 1)),
                                 reads=[Bxn32, Brw], writes=[pl[1]], inc=(k == KT - 1))
                        for g in range(2):
                            p, Bp = ptp[n % 2]
                            n += 1
                            for k in range(8):
                                kk = g * 8 + k
                                S.op("pe", lambda e: e.transpose(p[:, k * 128:(k + 1) * 128], xn[:, kk, j * 128:(j + 1) * 128], C["ident_b"]),
                                     reads=[Bxn, C["Bb"]], writes=[Bp], inc=(k == 7))
                            if g == 0:
                                S.op("act", lambda e: e.copy(xn_tm[:, tt, g * 1024:(g + 1) * 1024], p[:]), reads=[Bp], writes=[Bxtm])
                            else:
                                S.op("dve", lambda e: e.tensor_copy(xn_tm[:, tt, g * 1024:(g + 1) * 1024], p[:]), reads=[Bp], writes=[Bxtm])
                    S.op("dve", lambda e: e.tensor_tensor(logits[:, tc * 4:(tc + 1) * 4, :], pl[0][:, 0:32].rearrange("p (a c) -> p a c", c=8),
                                                          rb[:].unsqueeze(1).to_broadcast([128, 4, NE]), ALU.add), reads=[pl[1], Brw], writes=[Blog])
                S.barrier()
            sh = [128, 16, NE]
            m1 = K.sb(st1, [128, 16], F32); m2 = K.sb(st1, [128, 16], F32)
            g1 = K.sb(st1, [128, 16], F32); g2 = K.sb(st1, [128, 16], F32)
            mk1 = K.sb(st1, sh, F32); mk2 = K.sb(st1, sh, F32); l2 = K.sb(st1, sh, F32)
            sel = K.sb(st1, sh, F32); selb = K.sb(st1, sh, BF16); gate = K.sb(st1, sh, F32)
            ghl = K.sb(st1, [128, 16, NE, 2], BF16); gt = K.sb(st1, sh, F32)
            pos = K.sb(st1, sh, F32)
            Br = Buf()
            bc = lambda a: a[:].unsqueeze(2).to_broadcast(sh)
            R = lambda fn: S.op("dve", fn, reads=[Br, Blog], writes=[Br])
            R(lambda e: e.tensor_reduce(m1[:], logits[:], AX.X, ALU.max))
            R(lambda e: e.tensor_tensor(mk1[:], logits[:], bc(m1), ALU.is_equal))
            R(lambda e: e.scalar_tensor_tensor(l2[:], mk1[:], -1e30, logits[:], ALU.mult, ALU.add))
            R(lambda e: e.tensor_reduce(m2[:], l2[:], AX.X, ALU.max))
            R(lambda e: e.tensor_tensor(mk2[:], l2[:], bc(m2), ALU.is_equal))
            R(lambda e: e.tensor_tensor(sel[:], mk1[:], mk2[:], ALU.add))
            R(lambda e: e.tensor_copy(selb[:], sel[:]))
            R(lambda e: e.tensor_tensor(g2[:], m2[:], m1[:], ALU.subtract))
            S.op("act", lambda e: e.activation(g2[:], g2[:], AF.Exp), reads=[Br], writes=[Br])
            R(lambda e: e.tensor_scalar(g1[:], g2[:], 1.0, None, ALU.add))
            R(lambda e: e.reciprocal(g1[:], g1[:]))
            R(lambda e: e.tensor_tensor(g2[:], g2[:], g1[:], ALU.mult))
            R(lambda e: e.tensor_tensor(gate[:], mk1[:], bc(g1), ALU.mult))
            R(lambda e: e.tensor_tensor(gt[:], mk2[:], bc(g2), ALU.mult))
            R(lambda e: e.tensor_tensor(gate[:], gate[:], gt[:], ALU.add))
            R(lambda e: e.tensor_copy(ghl[:, :, :, 0], gate[:]))
            R(lambda e: e.tensor_copy(gt[:], ghl[:, :, :, 0]))
            R(lambda e: e.tensor_tensor(gt[:], gate[:], gt[:], ALU.subtract))
            R(lambda e: e.tensor_copy(ghl[:, :, :, 1], gt[:]))
            with ExitStack() as st:
                pp = (K.ps(st), Buf())
                for tt in range(16):
                    for i in range(tt + 1):
                        lhs = C["ones_b"] if i < tt else C["U_b"]
                        S.op("pe", lambda e: e.matmul(pp[0][:, tt * 8:(tt + 1) * 8], lhs, selb[:, i, :], start=(i == 0), stop=(i == tt)),
                             reads=[Br, C["Bb"]], writes=[pp[1]], inc=(i == tt))
                S.op("dve", lambda e: e.tensor_copy(pos[:], pp[0][:, 0:128].rearrange("p (a c) -> p a c", c=8)), reads=[pp[1]], writes=[Br])
                S.barrier()
            with ExitStack() as st:
                iota = K.sb(st, [128, CAP], F32); Bio = Buf()
                S.dma("sp", iota[:], W["iota"], reads=[Bw], writes=[Bio])
                Pe = [(K.sb(st, [128, 16, CAP], BF16), Buf()) for _ in range(1)]
                PTst = (K.sb(st, [128, CT, T], BF16), Buf())
                Xst = (K.sb(st, [128, KT, CAP], BF16), Buf())
                ptp = [(K.ps(st, [128, 1024], BF16), Buf()) for _ in range(2)]
                pgs = (K.ps(st), Buf())
                gtmp = K.sb(st, [128, 2 * CT], F32); Bgtmp = Buf()
                pga = [(K.ps(st), Buf()) for _ in range(4)]
                n = 0
                m = 0
                for ex in range(NE):
                    P, BP = Pe[0]
                    for tt in range(16):
                        eng = "dve"
                        S.op(eng, lambda e: e.tensor_scalar(P[:, tt, :], iota[:], pos[:, tt, ex:ex + 1], sel[:, tt, ex:ex + 1], ALU.is_equal, ALU.mult),
                             reads=[Bio, Br], writes=[BP])
                    for ct in range(CT):
                        for g in range(2):
                            p, Bp = ptp[n % 2]
                            n += 1
                            for k in range(8):
                                tt = g * 8 + k
                                S.op("pe", lambda e: e.transpose(p[:, k * 128:(k + 1) * 128], P[:, tt, ct * 128:(ct + 1) * 128], C["ident_b"]),
                                     reads=[BP, C["Bb"]], writes=[Bp], inc=(k == 7))
                            if g == 0:
                                S.op("act", lambda e: e.copy(PTst[0][:, ct, g * 1024:(g + 1) * 1024], p[:]), reads=[Bp], writes=[PTst[1]])
                            else:
                                S.op("dve", lambda e: e.tensor_copy(PTst[0][:, ct, g * 1024:(g + 1) * 1024], p[:]), reads=[Bp], writes=[PTst[1]])
                    S.dma("sp", PTs[:, ex * CT:(ex + 1) * CT, :], PTst[0][:], reads=[PTst[1]], writes=[BPT])
                    for ct in range(CT):
                        for tt in range(16):
                            S.op("pe", lambda e: e.matmul(pgs[0][:, ct * 2:(ct + 1) * 2], P[:, tt, ct * 128:(ct + 1) * 128], ghl[:, tt, ex, :], start=(tt == 0), stop=(tt == 15)),
                                 reads=[BP, Br], writes=[pgs[1]], inc=(tt == 15))
                    S.op("act", lambda e: e.copy(gtmp[:], pgs[0][:, 0:2 * CT]), reads=[pgs[1]], writes=[Bgtmp])
                    pv = gtmp[:].rearrange("p (a c) -> p a c", c=2)
                    S.op("dve", lambda e: e.tensor_tensor(gs[:, ex, :], pv[:, :, 0], pv[:, :, 1], ALU.add), reads=[Bgtmp], writes=[Bgs])
                    for dt in range(KT):
                        for cc in range(2):
                            p, Bp = pga[m % 4]
                            m += 1
                            for tt in range(16):
                                S.op("pe", lambda e: e.matmul(p[:, 0:CCH], xn_tm[:, tt, dt * 128:(dt + 1) * 128], P[:, tt, cc * CCH:(cc + 1) * CCH], start=(tt == 0), stop=(tt == 15)),
                                     reads=[Bxtm, BP], writes=[Bp], inc=(tt == 15))
                            if m % 2 == 0:
                                S.op("act", lambda e: e.copy(Xst[0][:, dt, cc * CCH:(cc + 1) * CCH], p[:, 0:CCH]), reads=[Bp], writes=[Xst[1]])
                            else:
                                S.op("dve", lambda e: e.tensor_copy(Xst[0][:, dt, cc * CCH:(cc + 1) * CCH], p[:, 0:CCH]), reads=[Bp], writes=[Xst[1]])
                    S.dma("sp", XeT[ex], Xst[0][:], reads=[Xst[1]], writes=[BXe])
                S.barrier()

        FC = 256
        NFC = DFF // FC
        NFT = FC // 128
        with ExitStack() as st:
            Xe = [(K.sb(st, [128, KT, CAP], BF16), Buf()) for _ in range(2)]
            yacc = K.sb(st, [128, CT, D], F32); By = [Buf() for _ in range(CT)]
            Yst = (K.sb(st, [128, CT, D], BF16), Buf())
            wgs = [(K.sb(st, [128, KT, FC], BF16), Buf()) for _ in range(2)]
            wus = [(K.sb(st, [128, KT, FC], BF16), Buf()) for _ in range(2)]
            wds = [(K.sb(st, [128, NFT, D], BF16), Buf()) for _ in range(2)]
            hts = [(K.sb(st, [128, NFT, CAP], BF16), Buf()) for _ in range(2)]
            sgs = [(K.sb(st, [128, CCH], F32), Buf()) for _ in range(2)]
            pgs = [(K.ps(st), Buf()) for _ in range(2)]
            pus = [(K.ps(st), Buf()) for _ in range(2)]
            pos_ = [(K.ps(st), Buf()) for _ in range(4)]
            seq = [(ex, fc) for ex in range(NE) for fc in range(NFC)]

            def wview(w, ex):
                return w[ex].rearrange("(k p) f -> p k f", p=128)

            def load_gu(i):
                ex, fc = seq[i]
                s = i % 2
                S.dma("pool", wgs[s][0][:], wview(W["wg"], ex)[:, :, fc * FC:(fc + 1) * FC], reads=[Bw], writes=[wgs[s][1]])
                S.dma("pool", wus[s][0][:], wview(W["wu"], ex)[:, :, fc * FC:(fc + 1) * FC], reads=[Bw], writes=[wus[s][1]])

            def load_d(i):
                ex, fc = seq[i]
                s = i % 2
                S.dma("pool", wds[s][0][:], W["wd"][ex].rearrange("(j p) d -> p j d", p=128)[:, fc * NFT:(fc + 1) * NFT, :], reads=[Bw], writes=[wds[s][1]])

            def load_x(ex):
                S.dma("sp", Xe[ex % 2][0][:], XeT[ex], reads=[BXe], writes=[Xe[ex % 2][1]])

            cnt = [0]
            ocnt = [0]

            def gateup(i):
                ex, fc = seq[i]
                s = i % 2
                x, Bx = Xe[ex % 2]
                wgt, Bwg = wgs[s]
                wut, Bwu = wus[s]
                ht, Bht = hts[s]
                for ft in range(NFT):
                    for cc in range(2):
                        j = cnt[0] % 2
                        cnt[0] += 1
                        pg, Bpg = pgs[j]
                        pu, Bpu = pus[j]
                        sg, Bsg = sgs[j]
                        csl = slice(cc * CCH, (cc + 1) * CCH)
                        for k in range(KT):
                            S.op("pe", lambda e: e.matmul(pg[:, 0:CCH], wgt[:, k, ft * 128:(ft + 1) * 128], x[:, k, csl], start=(k == 0), stop=(k == KT - 1)),
                                 reads=[Bwg, Bx], writes=[Bpg], inc=(k == KT - 1))
                        for k in range(KT):
                            S.op("pe", lambda e: e.matmul(pu[:, 0:CCH], wut[:, k, ft * 128:(ft + 1) * 128], x[:, k, csl], start=(k == 0), stop=(k == KT - 1)),
                                 reads=[Bwu, Bx], writes=[Bpu], inc=(k == KT - 1))
                        S.op("act", lambda e: e.activation(sg[:], pg[:, 0:CCH], AF.Silu), reads=[Bpg], writes=[Bsg])
                        S.op("dve", lambda e: e.tensor_tensor(ht[:, ft, csl], sg[:], pu[:, 0:CCH], ALU.mult), reads=[Bsg, Bpu], writes=[Bht])

            def down(i):
                ex, fc = seq[i]
                s = i % 2
                wdt, Bwd = wds[s]
                ht, Bht = hts[s]
                for ct in range(CT):
                    for dc in range(4):
                        j = ocnt[0] % 4
                        ocnt[0] += 1
                        po, Bpo = pos_[j]
                        dsl = slice(dc * 512, (dc + 1) * 512)
                        for ft in range(NFT):
                            S.op("pe", lambda e: e.matmul(po[:], ht[:, ft, ct * 128:(ct + 1) * 128], wdt[:, ft, dsl], start=(ft == 0), stop=(ft == NFT - 1)),
                                 reads=[Bwd, Bht], writes=[Bpo], inc=(ft == NFT - 1))
                        if fc == 0:
                            S.op("dve", lambda e: e.tensor_copy(yacc[:, ct, dsl], po[:]), reads=[Bpo], writes=[By[ct]])
                        else:
                            S.op("dve", lambda e: e.tensor_tensor(yacc[:, ct, dsl], yacc[:, ct, dsl], po[:], ALU.add), reads=[Bpo, By[ct]], writes=[By[ct]])
                if fc == NFC - 1:
                    for ct in range(CT):
                        if ct % 2 == 0:
                            S.op("act", lambda e: e.activation(Yst[0][:, ct, :], yacc[:, ct, :], AF.Copy, scale=gs[:, ex, ct:ct + 1]), reads=[By[ct], Bgs], writes=[Yst[1]])
                        else:
                            S.op("dve", lambda e: e.tensor_scalar(Yst[0][:, ct, :], yacc[:, ct, :], gs[:, ex, ct:ct + 1], None, ALU.mult), reads=[By[ct], Bgs], writes=[Yst[1]])
                    S.dma("sp", Ysd[:, ex * CT:(ex + 1) * CT, :], Yst[0][:], reads=[Yst[1]], writes=[BYs])

            NS = len(seq)
            load_x(0)
            load_gu(0)
            load_d(0)
            for i in range(NS + 1):
                if i + 1 < NS:
                    load_gu(i + 1)
                if i < NS:
                    if seq[i][1] == 0 and seq[i][0] + 1 < NE:
                        load_x(seq[i][0] + 1)
                    gateup(i)
                if i >= 1:
                    down(i - 1)
                if i + 1 < NS:
                    load_d(i + 1)
            S.barrier()

        with ExitStack() as st:
            NJ = NE * CT
            PTc = (K.sb(st, [128, NJ, 512], BF16), Buf())
            Ysc = [(K.sb(st, [128, NJ, 512], BF16), Buf()) for _ in range(2)]
            hacc = K.sb(st, [128, KT, 512], F32); Bh = [Buf() for _ in range(KT)]
            fnw = K.sb(st, [128, KT], F32); Bfn = Buf()
            S.dma("sp", fnw[:], W["fnw"], reads=[Bw], writes=[Bfn])
            tmp = norm_tmp(K, st)
            pn = (K.ps(st), Buf())
            pss = [(K.ps(st), Buf()) for _ in range(4)]
            n = 0
            q = 0
            for tc in range(T // 512):
                tsl = slice(tc * 512, (tc + 1) * 512)
                S.dma("sp", PTc[0][:], PTs[:, :, tsl], reads=[BPT], writes=[PTc[1]])
                S.dma("sp", hacc[:], hin_v[:, :, tsl], reads=[Bhin], writes=Bh)
                for dq in range(4):
                    ys, Bys = Ysc[q % 2]
                    q += 1
                    S.dma("sp", ys[:], Ysd[:, :, dq * 512:(dq + 1) * 512], reads=[BYs], writes=[Bys])
                    for dl in range(4):
                        dt = dq * 4 + dl
                        p, Bp = pss[n % 4]
                        n += 1
                        for j in range(NJ):
                            S.op("pe", lambda e: e.matmul(p[:], ys[:, j, dl * 128:(dl + 1) * 128], PTc[0][:, j, :], start=(j == 0), stop=(j == NJ - 1)),
                                 reads=[Bys, PTc[1]], writes=[Bp], inc=(j == NJ - 1))
                        S.op("dve", lambda e: e.tensor_tensor(hacc[:, dt, :], hacc[:, dt, :], p[:], ALU.add), reads=[Bp, Bh[dt]], writes=[Bh[dt]])
                Bo = Buf()
                norm_T(K, C, hacc, Bh, hacc, Bo, fnw, Bfn, 512, tmp, pn[0], pn[1])
                S.dma("sp", out_v[:, :, tsl], hacc[:], reads=[Bo] + Bh, writes=[Bout])
        S.barrier()


def out_proj(K, srcT, Bsrc, hin, Bhin, hout, Bhout, wo_d, Bw):
    S = K.S
    hin_v = hin.rearrange("(k p) t -> p k t", p=128)
    hout_v = hout.rearrange("(k p) t -> p k t", p=128)
    src_v = srcT.rearrange("(h p) t -> p h t", p=128)
    wo_v = wo_d.rearrange("(k p) f -> p k f", p=128)
    with ExitStack() as st:
        wo = K.sb(st, [128, KT, D], BF16); Bwo = Buf()
        for k0 in range(0, KT, 4):
            S.dma("pool", wo[:, k0:k0 + 4, :], wo_v[:, k0:k0 + 4, :], reads=[Bw], writes=[Bwo])
        och = [(K.sb(st, [128, KT, 512], BF16), Buf()) for _ in range(2)]
        hch = [(K.sb(st, [128, KT, 512], F32), [Buf() for _ in range(KT)]) for _ in range(2)]
        pcs = [(K.ps(st), Buf()) for _ in range(4)]
        n = 0
        for tc in range(T // 512):
            tsl = slice(tc * 512, (tc + 1) * 512)
            oc, Boc = och[tc % 2]
            hc, Bhc = hch[tc % 2]
            S.dma("sp", oc[:], src_v[:, :, tsl], reads=[Bsrc], writes=[Boc])
            S.dma("sp", hc[:], hin_v[:, :, tsl], reads=[Bhin], writes=Bhc)
            for dt in range(KT):
                p, Bp = pcs[n % 4]
                n += 1
                for k in range(KT):
                    S.op("pe", lambda e: e.matmul(p[:], wo[:, k, dt * 128:(dt + 1) * 128], oc[:, k, :], start=(k == 0), stop=(k == KT - 1)),
                         reads=[Bwo, Boc], writes=[Bp], inc=(k == KT - 1))
                S.op("dve", lambda e: e.tensor_tensor(hc[:, dt, :], hc[:, dt, :], p[:], ALU.add), reads=[Bp, Bhc[dt]], writes=[Bhc[dt]])
            S.dma("sp", hout_v[:, :, tsl], hc[:], reads=Bhc, writes=[Bhout])
    S.barrier()


NEG = -30000.0


def phase_gdn(K, C, hin, Bhin, hout, Bhout, W, Bw, stop=99):
    S = K.S
    hin_v = hin.rearrange("(k p) t -> p k t", p=128)
    win_v = W["win"].rearrange("(k p) f -> p k f", p=128)
    qkvT, zT, goT = W["qkvT"], W["zT"], W["goT"]
    Bqkv, Bz, Bgo = K.dbuf("qkvT"), K.dbuf("zT"), K.dbuf("goT")
    mask_incl = C["f"][:, 640:768]
    mask_strict = C["f"][:, 768:896]
    with ExitStack() as stg:
        sc_kbg = K.sb(stg, [128, 16, 16], F32); sc_kdec = K.sb(stg, [128, 16, 16], F32)
        nbeta = K.sb(stg, [128, 16, 16], F32); beta_tm = K.sb(stg, [128, 16, 16], F32)
        gc_tm = K.sb(stg, [128, 16, 16], F32)
        gcT = K.sb(stg, [16, T], F32)
        egl = K.sb(stg, [128, 16, 32], F32)
        stba = ExitStack()
        bT = K.sb(stba, [16, T], F32); aT = K.sb(stba, [16, T], F32); Bba = Buf()
        with ExitStack() as st:
            xn = K.sb(st, [128, KT, T], BF16); Bxn = Buf()
            lnw = K.sb(st, [128, KT], F32); Blnw = Buf()
            cw = K.sb(st, [128, 48, 4], F32)
            S.dma("sp", lnw[:], W["lnw"], reads=[Bw], writes=[Blnw])
            S.dma("sp", cw[:], W["cw"], reads=[Bw], writes=[Blnw])
            tmp = norm_tmp(K, st)
            pn = (K.ps(st), Buf())
            with ExitStack() as st2:
                hch = K.sb(st2, [128, KT, 512], F32); Bh = [Buf() for _ in range(KT)]
                for tc in range(4):
                    S.dma("sp", hch[:], hin_v[:, :, tc * 512:(tc + 1) * 512], reads=[Bhin], writes=Bh)
                    norm_T(K, C, hch, Bh, xn, Bxn, lnw, Blnw, 512, tmp, pn[0], pn[1], dcol=tc * 512)
                S.barrier()
            wb = [(K.sb(st, [128, KT, 512], BF16), Buf()) for _ in range(2)]
            wba = K.sb(st, [128, KT, 32], BF16); Bwba = Buf()
            S.dma("pool", wba[:], win_v[:, :, 8192:8224], reads=[Bw], writes=[Bwba])
            NSET = 2
            pcb = [(K.sb(st, [128, 3 + T], F32), Buf()) for _ in range(NSET)]
            cvb = [(K.sb(st, [128, T], F32), Buf()) for _ in range(NSET)]
            obb = [(K.sb(st, [128, T], BF16), Buf()) for _ in range(NSET)]
            pps = [(K.ps(st), Buf()) for _ in range(4)]
            pst = [(K.ps(st), Buf()) for _ in range(3)] + [pn]
            sq4 = K.sb(st, [128, T], F32); Bsq4 = Buf()
            rs4 = K.sb(st, [128, T], F32); Brs4 = Buf()
            onesr = K.sb(st, [128, 128], F32); Bonesr = Buf()
            S.op("dve", lambda e: e.tensor_copy(onesr[:].bitcast(F32R), C["ones_f"]), reads=[C["Bf"]], writes=[Bonesr])
            for pcx, Bpc in pcb:
                S.op("dve", lambda e: e.memset(pcx[:, 0:3], 0.0), writes=[Bpc])
            pi = [0]

            def projm(wt, Bwt, c0, m, tc, evac):
                p, Bp = pps[pi[0] % 4]
                pi[0] += 1
                for k in range(KT):
                    S.op("pe", lambda e: e.matmul(p[0:m, :], wt[:, k, c0:c0 + m], xn[:, k, tc * 512:(tc + 1) * 512], start=(k == 0), stop=(k == KT - 1)),
                         reads=[Bwt, Bxn], writes=[Bp], inc=(k == KT - 1))
                evac(p, Bp)

            for tc in range(4):
                tsl = slice(tc * 512, (tc + 1) * 512)
                projm(wba, Bwba, 0, 16, tc, lambda p, Bp: S.op("act", lambda e: e.copy(bT[:, tsl], p[0:16, :]), reads=[Bp], writes=[Bba]))
                projm(wba, Bwba, 16, 16, tc, lambda p, Bp: S.op("act", lambda e: e.copy(aT[:, tsl], p[0:16, :]), reads=[Bp], writes=[Bba]))
            S.dma("pool", wb[0][0][:], win_v[:, :, 0:512], reads=[Bw], writes=[wb[0][1]])

            def stageA(m):
                g, ml = m // 4, m % 4
                wt, Bwt = wb[g % 2]
                pcx, Bpc = pcb[m % NSET]
                cv, Bcv = cvb[m % NSET]
                if m >= 48:
                    for tc in range(4):
                        projm(wt, Bwt, ml * 128, 128, tc, lambda p, Bp: S.op("act", lambda e: e.copy(cv[:, tc * 512:(tc + 1) * 512], p[:]), reads=[Bp], writes=[Bcv]))
                    S.dma("sp", zT[(m - 48) * 128:(m - 47) * 128, :], cv[:], reads=[Bcv], writes=[Bz])
                    return
                for tc in range(4):
                    projm(wt, Bwt, ml * 128, 128, tc, lambda p, Bp: S.op("act", lambda e: e.copy(pcx[:, 3 + tc * 512:3 + (tc + 1) * 512], p[:]), reads=[Bp], writes=[Bpc]))
                S.op("dve", lambda e: e.tensor_scalar(cv[:], pcx[:, 0:T], cw[:, m, 0:1], None, ALU.mult), reads=[Bpc, Blnw], writes=[Bcv])
                for j in range(1, 4):
                    S.op("dve", lambda e: e.scalar_tensor_tensor(cv[:], pcx[:, j:j + T], cw[:, m, j:j + 1], cv[:], ALU.mult, ALU.add), reads=[Bpc, Bcv, Blnw], writes=[Bcv])
                S.op("act", lambda e: e.activation(cv[:], cv[:], AF.Silu), reads=[Bcv], writes=[Bcv])

            def stageB(m):
                if m >= 48:
                    return
                cv, Bcv = cvb[m % NSET]
                ob, Bob = obb[m % NSET]
                if m < 32:
                    sc = float(128 ** -0.5) if m < 16 else 1.0
                    for tc in range(4):
                        tsl = slice(tc * 512, (tc + 1) * 512)
                        S.op("act", lambda e: e.activation(sq4[:, tsl].bitcast(F32R), cv[:, tsl], AF.Square), reads=[Bcv], writes=[Bsq4])
                    for tc in range(4):
                        tsl = slice(tc * 512, (tc + 1) * 512)
                        pq, Bpq = pst[tc]
                        S.op("pe", lambda e: e.matmul(pq[:], onesr[:].bitcast(F32R), sq4[:, tsl].bitcast(F32R), start=True, stop=True), reads=[Bsq4, Bonesr], writes=[Bpq])
                    for tc in range(4):
                        tsl = slice(tc * 512, (tc + 1) * 512)
                        pq, Bpq = pst[tc]
                        S.op("act", lambda e: e.activation(rs4[:, tsl], pq[:], AF.Ln, bias=tmp["eps"][:, 0:1], scale=1.0), reads=[Bpq, tmp["Beps"]], writes=[Brs4])
                    S.op("act", lambda e: e.activation(rs4[:], rs4[:], AF.Exp, scale=-0.5), reads=[Brs4], writes=[Brs4])
                    S.op("dve", lambda e: e.scalar_tensor_tensor(ob[:], cv[:], sc, rs4[:], ALU.mult, ALU.mult), reads=[Bcv, Brs4], writes=[Bob])
                else:
                    S.op("dve", lambda e: e.tensor_copy(ob[:], cv[:]), reads=[Bcv], writes=[Bob])
                S.dma("sp", qkvT[m * 128:(m + 1) * 128, :], ob[:], reads=[Bob], writes=[Bqkv])

            for m in range(64):
                g = m // 4
                if m % 4 == 0 and g + 1 < 16:
                    S.dma("pool", wb[(g + 1) % 2][0][:], win_v[:, :, (g + 1) * 512:(g + 2) * 512], reads=[Bw], writes=[wb[(g + 1) % 2][1]])
                stageA(m)
                if m >= 1:
                    stageB(m - 1)
            stageB(63)
            S.barrier()
        if stop == 1:
            return

        Bg = Buf()
        selh = lambda h: C["f"][0:16, h:h + 1].to_broadcast([16, 128])
        nselh = lambda h: C["f"][0:16, 896 + h:897 + h].to_broadcast([16, 128])
        with ExitStack() as st:
            G = lambda eng, fn: S.op(eng, fn, reads=[Bg, Bba, Blnw2, C["Bf"]], writes=[Bg])
            Blnw2 = Buf()
            alog = K.sb(st, [16, 1], F32); dtb = K.sb(st, [16, 1], F32)
            S.dma("sp", alog[:], W["alog"], reads=[Bw], writes=[Blnw2])
            S.dma("sp", dtb[:], W["dtb"], reads=[Bw], writes=[Blnw2])
            betaT = K.sb(st, [16, T], F32); x = K.sb(st, [16, T], F32); y = K.sb(st, [16, T], F32)
            gT = K.sb(st, [16, T], F32); t1 = K.sb(st, [16, T], F32); glT = K.sb(st, [16, 32], F32); eglT = K.sb(st, [16, 32], F32)
            egcT = K.sb(st, [16, T], F32)
            nea = K.sb(st, [16, 1], F32)
            G("act", lambda e: e.activation(betaT[:], bT[:], AF.Sigmoid))
            G("act", lambda e: e.activation(nea[:], alog[:], AF.Exp))
            G("dve", lambda e: e.tensor_scalar(nea[:], nea[:], -1.0, None, ALU.mult))
            G("dve", lambda e: e.tensor_scalar(x[:], aT[:], dtb[:, 0:1], None, ALU.add))
            G("act", lambda e: e.activation(y[:], x[:], AF.Abs))
            G("act", lambda e: e.activation(y[:], y[:], AF.Exp, scale=-1.0))
            G("act", lambda e: e.activation(y[:], y[:], AF.Ln, bias=1.0))
            G("dve", lambda e: e.tensor_scalar(x[:], x[:], 0.0, None, ALU.max))
            G("dve", lambda e: e.tensor_tensor(x[:], x[:], y[:], ALU.add))
            G("dve", lambda e: e.tensor_scalar(gT[:], x[:], nea[:, 0:1], None, ALU.mult))
            G("dve", lambda e: e.tensor_copy(gcT[:], gT[:]))
            v3 = lambda t: t[:].rearrange("p (c i) -> p c i", i=64)
            sft = 1
            while sft < 64:
                G("dve", lambda e: e.tensor_copy(t1[:], gcT[:]))
                G("dve", lambda e: e.tensor_tensor(v3(gcT)[:, :, sft:64], v3(t1)[:, :, sft:64], v3(t1)[:, :, 0:64 - sft], ALU.add))
                sft *= 2
            G("dve", lambda e: e.tensor_copy(glT[:], v3(gcT)[:, :, 63]))
            G("act", lambda e: e.activation(eglT[:], glT[:], AF.Exp))
            G("act", lambda e: e.activation(egcT[:], gcT[:], AF.Exp))
            G("dve", lambda e: e.tensor_tensor(x[:], betaT[:], egcT[:], ALU.mult))
            G("dve", lambda e: e.tensor_tensor(v3(y), v3(gcT), glT[:].unsqueeze(2).to_broadcast([16, 32, 64]), ALU.subtract))
            G("act", lambda e: e.activation(y[:], y[:], AF.Exp, scale=-1.0))
            pt = (K.ps(st), Buf())
            for src, dst in ((x, sc_kbg), (y, sc_kdec), (betaT, beta_tm), (gcT, gc_tm)):
                for tt in range(16):
                    S.op("pe", lambda e: e.transpose(pt[0][:, tt * 16:(tt + 1) * 16], src[:, tt * 128:(tt + 1) * 128], C["f"][0:16, 0:16]),
                         reads=[Bg, C["Bf"]], writes=[pt[1]], inc=(tt == 15))
                S.op("dve", lambda e: e.tensor_copy(dst[:], pt[0][:, 0:256].rearrange("p (a c) -> p a c", c=16)), reads=[pt[1]], writes=[Bg])
            G("dve", lambda e: e.tensor_scalar(nbeta[:], beta_tm[:], -1.0, None, ALU.mult))
            for h in range(NH):
                S.op("pe", lambda e: e.matmul(pt[0][:, 0:32], selh(h), eglT[:], start=True, stop=True), reads=[Bg, C["Bf"]], writes=[pt[1]])
                S.op("dve", lambda e: e.tensor_copy(egl[:, h, :], pt[0][:, 0:32]), reads=[pt[1]], writes=[Bg])
            S.barrier()
        stba.close()
        if stop == 2:
            return

        HG = 4
        strict01 = C["f"][:, 768:896]

        def run_chains(gens, stagger=0):
            gens = list(gens)
            for k_, g_ in enumerate(list(gens)):
                for _ in range(k_ * stagger):
                    try:
                        next(g_)
                    except StopIteration:
                        gens.remove(g_)
                        break
            while gens:
                for g_ in list(gens):
                    try:
                        next(g_)
                    except StopIteration:
                        gens.remove(g_)

        with ExitStack() as st:
            qkv_v = qkvT.rearrange("(m p) t -> m p t", p=128)
            z_v = zT.rearrange("(m p) t -> m p t", p=128)
            go_v = goT.rearrange("(m p) t -> m p t", p=128)
            gnw = K.sb(st, [128, 1], F32); Bgn = Buf()
            S.dma("sp", gnw[:], W["gnw"], reads=[Bw], writes=[Bgn])
            tmp = norm_tmp(K, st)
            PH = []
            for _ in range(HG):
                d = {}
                for nm in ("qd", "negw"):
                    d[nm] = (K.sb(st, [128, T], BF16), Buf())
                for nm in ("kdec", "vb", "TT", "AT"):
                    d[nm] = (K.sb(st, [128, 16, 128], BF16), Buf())
                d["oT"] = (K.sb(st, [128, T], F32), Buf())
                d["S"] = (K.sb(st, [128, 128], F32), Buf())
                d["Sbf"] = [(K.sb(st, [128, 128], BF16), Buf()) for _ in range(2)]
                d["vn"] = [(K.sb(st, [128, 128], BF16), Buf()) for _ in range(2)]
                for t_, B_ in d["vn"]:
                    S.op("dve", lambda e: e.memset(t_[:], 0.0), writes=[B_])
                PH.append(d)
            qT = K.sb(st, [128, T], BF16); BqT = Buf()
            kT = K.sb(st, [128, T], BF16); BkT = Buf()
            vT = K.sb(st, [128, T], BF16); BvT = Buf()
            kbg = K.sb(st, [128, 16, 128], BF16); Bkbg = Buf()
            gcrow = K.sb(st, [128, 512], F32); Bgcrow = Buf()
            rs2 = [tmp["rs"], (K.sb(st, [128, 512], F32), Buf())]
            zc = [(K.sb(st, [128, 512], F32), Buf()) for _ in range(2)]
            goc = [(K.sb(st, [128, 512], BF16), Buf()) for _ in range(2)]
            CH = []
            for _ in range(4):
                d = {"dm": (K.sb(st, [128, 128], F32), Buf()), "dms": (K.sb(st, [128, 128], F32), Buf()),
                     "M": [(K.sb(st, [128, 128], F32), Buf()) for _ in range(2)], "N": [(K.sb(st, [128, 128], F32), Buf()) for _ in range(2)],
                     "RT": (K.sb(st, [128, 128], F32), Buf()), "A": (K.sb(st, [128, 128], BF16), Buf())}
                CH.append(d)
            pKK = K.ps(st); LKK = Buf()
            pQK = K.ps(st); LQK = Buf()
            pDi = K.ps(st); LDi = Buf()
            pM = K.ps(st); LM = Buf()
            pN = K.ps(st); LN = Buf()
            pR = K.ps(st); LR = Buf()
            pTr = K.ps(st); LTr = Buf()
            pTb = K.ps(st, [128, 1024], BF16); LTb = Buf()
            v2 = lambda ap: ap.rearrange("p (a c) -> p a c", c=128)

            def pre_head(h, ph):
                qd, Bqd = ph["qd"]; negw, Bnw = ph["negw"]; kdec, Bkdec = ph["kdec"]; vb, Bvb = ph["vb"]; TT, BTT = ph["TT"]; AT, BAT = ph["AT"]
                S.dma("sp", qT[:], qkv_v[h], reads=[Bqkv], writes=[BqT])
                S.dma("sp", kT[:], qkv_v[16 + h], reads=[Bqkv], writes=[BkT])
                S.dma("sp", vT[:], qkv_v[32 + h], reads=[Bqkv], writes=[BvT])
                for tc in range(4):
                    tsl = slice(tc * 512, (tc + 1) * 512)
                    eg, Beg = tmp["sq"][tc % 2]
                    S.op("pe", lambda e: e.matmul(pTr[:], selh(h), gcT[:, tsl], start=True, stop=True), reads=[Bg, C["Bf"]], writes=[LTr])
                    S.op("act", lambda e: e.activation(eg[:], pTr[:], AF.Exp), reads=[], writes=[Beg, LTr])
                    S.op("dve", lambda e: e.tensor_tensor(qd[:, tsl], qT[:, tsl], eg[:], ALU.mult), reads=[BqT, Beg], writes=[Bqd])
                for g4 in range(4):
                    gs_ = slice(g4 * 4, (g4 + 1) * 4)
                    for j in range(4):
                        tl = slice((g4 * 4 + j) * 128, (g4 * 4 + j + 1) * 128)
                        S.op("pe", lambda e: e.transpose(pTb[:, j * 128:(j + 1) * 128], kT[:, tl], C["ident_b"]), reads=[BkT, C["Bb"]], writes=[LTb], inc=False)
                        S.op("pe", lambda e: e.transpose(pTb[:, 512 + j * 128:512 + (j + 1) * 128], vT[:, tl], C["ident_b"]), reads=[BvT, C["Bb"]], writes=[LTb], inc=(j == 3))
                    bc4 = lambda t_: t_[:, gs_, h:h + 1].to_broadcast([128, 4, 128])
                    S.op("dve", lambda e: e.tensor_tensor(kbg[:, gs_, :], v2(pTb[:, 0:512]), bc4(sc_kbg), ALU.mult), reads=[Bg], writes=[Bkbg, LTb])
                    S.op("dve", lambda e: e.tensor_tensor(kdec[:, gs_, :], v2(pTb[:, 0:512]), bc4(sc_kdec), ALU.mult), reads=[Bg], writes=[Bkdec, LTb])
                    S.op("dve", lambda e: e.tensor_tensor(vb[:, gs_, :], v2(pTb[:, 512:1024]), bc4(beta_tm), ALU.mult), reads=[Bg], writes=[Bvb, LTb])

                def chain(tt, c):
                    ch = CH[c]
                    dm, Bdm = ch["dm"]; dms, Bdms = ch["dms"]; RT, BRT = ch["RT"]; Ab, BAb = ch["A"]
                    o = c * 128
                    os_ = slice(o, o + 128)
                    tl = slice(tt * 128, (tt + 1) * 128)
                    R32 = lambda ap: ap.bitcast(F32R)
                    S.op("pe", lambda e: e.matmul(pKK[:, os_], kT[:, tl], kT[:, tl], start=True, stop=True), reads=[BkT], writes=[LKK])
                    S.op("pe", lambda e: e.matmul(pQK[:, os_], qT[:, tl], kT[:, tl], start=True, stop=True), reads=[BqT, BkT], writes=[LQK])
                    S.op("dve", lambda e: e.scalar_tensor_tensor(dms[:], gcrow[:, os_], gc_tm[:, tt, h:h + 1], mask_incl, ALU.subtract, ALU.subtract),
                         reads=[Bgcrow, Bg, C["Bf"]], writes=[Bdms])
                    yield
                    S.op("act", lambda e: e.activation(dm[:], dms[:], AF.Exp, scale=-1.0), reads=[Bdms], writes=[Bdm])
                    yield
                    m0, Bm0 = ch["M"][0]
                    n0, Bn0 = ch["N"][0]
                    S.op("dve", lambda e: e.tensor_tensor(dms[:], dm[:], strict01, ALU.mult), reads=[Bdm, C["Bf"]], writes=[Bdms])
                    S.op("dve", lambda e: e.scalar_tensor_tensor(R32(m0[:]), pKK[:, os_], nbeta[:, tt, h:h + 1], dms[:], ALU.mult, ALU.mult),
                         reads=[Bg, Bdms], writes=[Bm0, LKK])
                    S.op("dve", lambda e: e.tensor_tensor(Ab[:], pQK[:, os_], dm[:], ALU.mult), reads=[Bdm], writes=[BAb, LQK])
                    yield
                    S.op("pe", lambda e: e.transpose(pTr[:, os_], m0[:], C["ident_f"]), reads=[Bm0, C["Bf"]], writes=[LTr])
                    S.op("pe", lambda e: e.transpose(pTb[:, os_], Ab[:], C["ident_b"]), reads=[BAb, C["Bb"]], writes=[LTb])
                    yield
                    S.op("act", lambda e: e.copy(R32(n0[:]), pTr[:, os_]), reads=[], writes=[Bn0, LTr])
                    S.op("act", lambda e: e.copy(AT[:, tt, :], pTb[:, os_]), reads=[], writes=[BAT, LTb])
                    yield
                    S.op("dve", lambda e: e.tensor_tensor(R32(RT[:]), n0[:], C["ident_f"], ALU.add), reads=[Bn0, C["Bf"]], writes=[BRT])
                    S.op("pe", lambda e: e.matmul(pM[:, os_], R32(n0[:]), R32(m0[:]), start=True, stop=True), reads=[Bn0, Bm0], writes=[LM])
                    S.op("pe", lambda e: e.matmul(pN[:, os_], R32(m0[:]), R32(n0[:]), start=True, stop=True), reads=[Bn0, Bm0], writes=[LN])
                    yield
                    for lvl in range(1, 6):
                        mn, Bmn = ch["M"][lvl % 2]
                        nn, Bnn = ch["N"][lvl % 2]
                        if lvl > 1:
                            S.op("dve", lambda e: e.tensor_tensor(R32(RT[:]), RT[:], pR[:, os_], ALU.add), reads=[BRT], writes=[BRT, LR])
                        S.op("act", lambda e: e.copy(R32(mn[:]), pM[:, os_]), reads=[], writes=[Bmn, LM])
                        if lvl < 5:
                            if c % 2 == 0:
                                S.op("dve", lambda e: e.tensor_copy(R32(nn[:]), pN[:, os_]), reads=[], writes=[Bnn, LN])
                            else:
                                S.op("act", lambda e: e.copy(R32(nn[:]), pN[:, os_]), reads=[], writes=[Bnn, LN])
                        yield
                        S.op("pe", lambda e: e.matmul(pR[:, os_], R32(mn[:]), R32(RT[:]), start=True, stop=True), reads=[Bmn, BRT], writes=[LR])
                        if lvl < 5:
                            S.op("pe", lambda e: e.matmul(pM[:, os_], R32(nn[:]), R32(mn[:]), start=True, stop=True), reads=[Bnn, Bmn], writes=[LM])
                        if lvl < 4:
                            S.op("pe", lambda e: e.matmul(pN[:, os_], R32(mn[:]), R32(nn[:]), start=True, stop=True), reads=[Bnn, Bmn], writes=[LN])
                        yield
                    S.op("dve", lambda e: e.tensor_tensor(R32(RT[:]), RT[:], pR[:, os_], ALU.add), reads=[BRT], writes=[BRT, LR])
                    yield
                    S.op("act", lambda e: e.copy(TT[:, tt, :], RT[:]), reads=[BRT], writes=[BTT])
                    yield
                    S.op("pe", lambda e: e.matmul(pR[:, os_], kbg[:, tt, :], TT[:, tt, :], start=True, stop=True), reads=[Bkbg, BTT], writes=[LR])
                    yield
                    S.op("dve", lambda e: e.tensor_scalar(negw[:, tl], pR[:, os_], -1.0, None, ALU.mult), reads=[], writes=[Bnw, LR])

                for rnd in range(4):
                    S.op("pe", lambda e: e.matmul(pDi[:], selh(h), gcT[:, rnd * 512:(rnd + 1) * 512], start=True, stop=True), reads=[Bg, C["Bf"]], writes=[LDi])
                    S.op("act", lambda e: e.copy(gcrow[:], pDi[:]), reads=[], writes=[Bgcrow, LDi])
                    run_chains([chain(rnd * 4 + c, c) for c in range(4)], stagger=1)

            def scan_head(h, ph, i):
                qd, Bqd = ph["qd"]; negw, Bnw = ph["negw"]; kdec, Bkdec = ph["kdec"]; vb, Bvb = ph["vb"]; TT, BTT = ph["TT"]; AT, BAT = ph["AT"]
                oT, BoT = ph["oT"]; Sst, BS = ph["S"]
                pE, LE, pF, LF, pG_, LG = pKK, LKK, pQK, LQK, pDi, LDi
                S.op("dve", lambda e: e.memset(Sst[:], 0.0), writes=[BS])
                S.op("dve", lambda e: e.memset(ph["Sbf"][0][0][:], 0.0), writes=[ph["Sbf"][0][1]])
                pv = pE[:, i * 128:(i + 1) * 128]
                ps_ = pF[:, i * 128:(i + 1) * 128]
                po = pG_[:, i * 64:(i + 1) * 64]
                for c in range(32):
                    tt, half = c // 2, c % 2
                    gsl = slice(c * 64, (c + 1) * 64)
                    msz = 64 if half == 0 else 128
                    rows = slice(0, 64) if half == 0 else slice(64, 128)
                    sb_, Bsb = ph["Sbf"][c % 2]
                    sbn, Bsbn = ph["Sbf"][(c + 1) % 2]
                    vn, Bvn = ph["vn"][tt % 2]
                    S.op("pe", lambda e: e.matmul(pv[0:msz, :], TT[:, tt, 0:msz], vb[:, tt, :], start=True, stop=False), reads=[BTT, Bvb], writes=[LE], inc=False)
                    S.op("pe", lambda e: e.matmul(pv[0:msz, :], negw[:, tt * 128: tt * 128 + msz], sb_[:], start=False, stop=True), reads=[Bnw, Bsb], writes=[LE])
                    yield
                    S.op("act", lambda e: e.copy(vn[rows, :], pv[rows, :]), reads=[], writes=[Bvn, LE])
                    yield
                    S.op("pe", lambda e: e.matmul(ps_, kdec[rows, tt, :], vn[rows, :], start=True, stop=True), reads=[Bkdec, Bvn], writes=[LF])
                    S.op("pe", lambda e: e.matmul(po, sb_[:], qd[:, gsl], start=True, stop=False), reads=[Bsb, Bqd], writes=[LG], inc=False)
                    S.op("pe", lambda e: e.matmul(po, vn[:], AT[:, tt, half * 64:(half + 1) * 64], start=False, stop=True), reads=[Bvn, BAT], writes=[LG])
                    yield
                    S.op("dve", lambda e: e.scalar_tensor_tensor(sbn[:], Sst[:], egl[:, h, c:c + 1], ps_, ALU.mult, ALU.add), reads=[BS, Bg], writes=[Bsbn, LF])
                    S.op("dve", lambda e: e.scalar_tensor_tensor(Sst[:], Sst[:], egl[:, h, c:c + 1], ps_, ALU.mult, ALU.add), reads=[BS, Bg], writes=[BS, LF])
                    S.op("act", lambda e: e.copy(oT[:, gsl], po), reads=[], writes=[BoT, LG])
                    yield

            def finish_head(h, ph):
                oT, BoT = ph["oT"]
                for half in range(2):
                    cs_ = [half * 2, half * 2 + 1]
                    for tc in cs_:
                        tsl = slice(tc * 512, (tc + 1) * 512)
                        zt, Bzt = zc[tc % 2]
                        S.dma("sp", zt[:], z_v[h][:, tsl], reads=[Bz], writes=[Bzt])
                        S.op("act", lambda e: e.activation(zt[:], zt[:], AF.Silu), reads=[Bzt], writes=[Bzt])
                    for tc in cs_:
                        tsl = slice(tc * 512, (tc + 1) * 512)
                        sq, Bsq = tmp["sq"][tc % 2]
                        S.op("act", lambda e: e.activation(sq[:], oT[:, tsl], AF.Square), reads=[BoT], writes=[Bsq])
                    for tc in cs_:
                        sq, Bsq = tmp["sq"][tc % 2]
                        pq, Lq = (pTr, LTr) if tc % 2 == 0 else (pM, LM)
                        S.op("pe", lambda e: e.matmul(pq[:], C["ones_f"], sq[:], start=True, stop=True), reads=[Bsq, C["Bf"]], writes=[Lq])
                    for tc in cs_:
                        rs, Brs = rs2[tc % 2]
                        pq, Lq = (pTr, LTr) if tc % 2 == 0 else (pM, LM)
                        S.op("act", lambda e: e.activation(rs[:], pq[:], AF.Ln, bias=tmp["eps"][:, 0:1], scale=1.0 / 128), reads=[tmp["Beps"]], writes=[Brs, Lq])
                    for tc in cs_:
                        rs, Brs = rs2[tc % 2]
                        S.op("act", lambda e: e.activation(rs[:], rs[:], AF.Exp, scale=-0.5), reads=[Brs], writes=[Brs])
                    for tc in cs_:
                        tsl = slice(tc * 512, (tc + 1) * 512)
                        rs, Brs = rs2[tc % 2]
                        zt, Bzt = zc[tc % 2]
                        go, Bgo_ = goc[tc % 2]
                        S.op("dve", lambda e: e.scalar_tensor_tensor(oT[:, tsl], oT[:, tsl], gnw[:, 0:1], rs[:], ALU.mult, ALU.mult), reads=[BoT, Bgn, Brs], writes=[BoT])
                        S.op("dve", lambda e: e.tensor_tensor(go[:], oT[:, tsl], zt[:], ALU.mult), reads=[BoT, Bzt], writes=[Bgo_])
                        S.dma("sp", go_v[h][:, tsl], go[:], reads=[Bgo_], writes=[Bgo])

            for hg in range(NH // HG):
                for i in range(HG):
                    pre_head(hg * HG + i, PH[i])
                run_chains([scan_head(hg * HG + i, PH[i], i) for i in range(HG)], stagger=1)
                for i in range(HG):
                    finish_head(hg * HG + i, PH[i])
            S.barrier()
    out_proj(K, goT, Bgo, hin, Bhin, hout, Bhout, W["wo"], Bw)


def _consts_np():
    c = np.zeros((128, 1024), np.float32)
    c[:, 0:128] = np.eye(128)
    c[:, 128:256] = 1.0
    p = np.arange(128)[:, None]
    q = np.arange(128)[None, :]
    c[:, 256:384] = np.where(q <= p, 0.0, -1e9)
    invf = (np.float32(10000.0) ** (-np.arange(0, 64, 2, dtype=np.float32) / np.float32(64))).astype(np.float32)
    c[0:64, 384] = np.concatenate([invf, invf])
    c[0:32, 385] = -1.0
    c[32:64, 385] = 1.0
    c[:, 512:640] = (p < q).astype(np.float32)
    same = (p // 64) == (q // 64)
    c[:, 640:768] = np.where(same & (q <= p), 0.0, NEG)
    c[:, 768:896] = (same & (q < p)).astype(np.float32)
    c[0:16, 896:912] = -np.eye(16)
    return c


def _pk(v):
    return np.ascontiguousarray(np.asarray(v, np.float32).reshape(-1, 128).T)


EI = "ExternalInput"
_IN_SPECS = [
    ("xT", [D, T], F32), ("posb", [64, T], I32), ("cst", [128, 1024], F32), ("iota", [128, CAP], F32),
    ("lnw_mla", [128, 16], F32), ("win", [D, 1152], F32), ("qnw", [128, 4], F32), ("wqb", [512, 4096], F32), ("kvnw", [128, 4], F32),
    ("wkvb", [512, 4096], F32), ("wo", [D, D], F32),
    ("lnw_ffn", [128, 16], F32), ("fwg", [D, DFF], F32), ("fwu", [D, DFF], F32), ("fwd", [DFF, D], F32),
    ("lnw_gdn", [128, 16], F32), ("gwin", [D, 8224], F32), ("gcw", [128, 48, 4], F32), ("alog", [16, 1], F32), ("dtb", [16, 1], F32),
    ("gnw", [128, 1], F32), ("gwo", [D, D], F32),
    ("lnw_moe", [128, 16], F32), ("rw", [128, 16, 8], F32), ("rb", [128, 8], F32), ("mwg", [NE, D, DFF], F32), ("mwu", [NE, D, DFF], F32),
    ("mwd", [NE, DFF, D], F32), ("fnw", [128, 16], F32),
]


def build_program():
    K = KB()
    A = {}
    for name, shape, dt in _IN_SPECS:
        A[name] = K.dram(name, shape, dt, kind=EI)
    outT = K.dram("outT", [D, T], F32, kind="ExternalOutput")
    h1 = K.dram("h1T", [D, T], F32)
    h2 = K.dram("h2T", [D, T], F32)
    h3 = K.dram("h3T", [D, T], F32)
    Bw = K.dbuf("cst")
    with K.st:
        C = load_consts(K, K.st, A["cst"])
        Wm = {"win": A["win"], "wqb": A["wqb"], "wkvb": A["wkvb"], "wo": A["wo"], "lnw": A["lnw_mla"], "qnw": A["qnw"], "kvnw": A["kvnw"],
              "posb": A["posb"], "oT": K.dram("oT", [D, T], BF16)}
        phase_mla(K, C, A["xT"], K.dbuf("xT"), h1, K.dbuf("h1T"), Wm, Bw)
        phase_ffn(K, C, h1, K.dbuf("h1T"), h2, K.dbuf("h2T"), A["lnw_ffn"], A["fwg"], A["fwu"], A["fwd"], Bw)
        Wg = {"win": A["gwin"], "cw": A["gcw"], "alog": A["alog"], "dtb": A["dtb"], "gnw": A["gnw"], "wo": A["gwo"], "lnw": A["lnw_gdn"],
              "qkvT": K.dram("qkvT", [6144, T], BF16), "zT": K.dram("zT", [D, T], F32), "goT": K.dram("goT", [D, T], BF16)}
        phase_gdn(K, C, h2, K.dbuf("h2T"), h3, K.dbuf("h3T"), Wg, Bw)
        We = {"rw": A["rw"], "rb": A["rb"], "lnw": A["lnw_moe"], "fnw": A["fnw"], "iota": A["iota"], "wg": A["mwg"], "wu": A["mwu"], "wd": A["mwd"],
              "XeT": K.dram("XeT", [NE, D, CAP], BF16), "PTs": K.dram("PTs", [NE * CAP, T], BF16), "Ys": K.dram("Ys", [NE * CAP, D], BF16)}
        phase_moe(K, C, h3, K.dbuf("h3T"), outT, K.dbuf("outT"), We, Bw)
        K.S._wait("sp", [K.dbuf("outT").w])
        K.S.barrier()
    return K


def _host_inputs(inp):
    g = lambda k: np.asarray(inp[k])
    w_in = g("mla_w_in")[0]
    win = np.concatenate([w_in, w_in[:, 1056:1088], w_in[:, 1024:1056]], axis=1)
    wqb = g("mla_w_qb")[0].reshape(512, 16, 192)
    wqb_aug = np.concatenate([wqb, wqb[:, :, 160:192], wqb[:, :, 128:160]], axis=2).reshape(512, 16 * 256)
    shared = {
        "cst": _consts_np(),
        "iota": np.ascontiguousarray(np.broadcast_to(np.arange(CAP, dtype=np.float32)[None, :], (128, CAP))),
        "lnw_mla": _pk(g("ln_mix_mla")[0]), "win": np.ascontiguousarray(win), "qnw": _pk(g("mla_q_norm")[0]), "wqb": np.ascontiguousarray(wqb_aug),
        "kvnw": _pk(g("mla_kv_norm")[0]), "wkvb": np.ascontiguousarray(g("mla_w_kvb")[0]), "wo": np.ascontiguousarray(g("mla_w_o")[0]),
        "lnw_ffn": _pk(g("ln_ffn_dense")[0]), "fwg": np.ascontiguousarray(g("ffn_w_gate")[0]), "fwu": np.ascontiguousarray(g("ffn_w_up")[0]),
        "fwd": np.ascontiguousarray(g("ffn_w_down")[0]),
        "lnw_gdn": _pk(g("ln_mix_gdn")[0]), "gwin": np.ascontiguousarray(g("gdn_w_in")[0]),
        "gcw": np.ascontiguousarray(g("gdn_conv_w")[0].T.reshape(48, 128, 4).transpose(1, 0, 2)),
        "alog": np.ascontiguousarray(g("gdn_a_log")[0].reshape(16, 1)), "dtb": np.ascontiguousarray(g("gdn_dt_bias")[0].reshape(16, 1)),
        "gnw": np.ascontiguousarray(g("gdn_norm")[0].reshape(128, 1)), "gwo": np.ascontiguousarray(g("gdn_w_o")[0]),
        "lnw_moe": _pk(g("ln_ffn_moe")[0]),
        "rw": np.ascontiguousarray(g("moe_router")[0].reshape(16, 128, 8).transpose(1, 0, 2)),
        "rb": np.ascontiguousarray(np.broadcast_to(g("moe_router_bias")[0][None, :], (128, 8))),
        "mwg": np.ascontiguousarray(g("moe_w_gate")[0]), "mwu": np.ascontiguousarray(g("moe_w_up")[0]), "mwd": np.ascontiguousarray(g("moe_w_down")[0]),
        "fnw": _pk(g("final_norm")),
    }
    shared = {k: (v if v.dtype != np.float64 else v.astype(np.float32)) for k, v in shared.items()}
    x = g("x")
    pos = g("positions")
    maps = []
    for b in range(x.shape[0]):
        m = dict(shared)
        m["xT"] = np.ascontiguousarray(x[b].T)
        m["posb"] = np.ascontiguousarray(np.broadcast_to(pos[b][None, :], (64, T))).astype(np.int32)
        maps.append(m)
    return maps


def kernel(**inputs):
    maps = _host_inputs(inputs)
    K = build_program()
    res = run_bass_kernel_spmd(K.nc, maps, core_ids=list(range(len(maps))))
    out = np.stack([np.ascontiguousarray(r["outT"].T) for r in res.results], axis=0)
    return out.astype(np.float32)
```
